# Optimizing a Trainium2 kernel written in Bass

```python
import jax, jax.numpy as jnp
from jax import lax
import numpy as np

D_MODEL = 1024
BATCH = 32
SEQ = 2048
DEPTH = 2

CHUNK = 64
HEAD_DIM = 64
ROPE_THETA = 10000.0
EPS = 1e-6
A_Q_HEADS = (D_MODEL // 2) // HEAD_DIM
A_KV_HEADS = A_Q_HEADS // 4
A_WINDOW = 128
A_PREV_CHUNKS = A_WINDOW // CHUNK
B_KEY_DIM = 128
B_VAL_DIM = 128
B_HEADS = (D_MODEL // 2) // B_VAL_DIM
B_BLOCK = 32
C_HEADS = D_MODEL // HEAD_DIM
C_PREV_CHUNKS = 8
REL_CLIP = 128
D_FF = 2816
CONV_WIDTH = 3
A_Q_DIM = A_Q_HEADS * HEAD_DIM
A_KV_DIM = A_KV_HEADS * HEAD_DIM
B_QK_DIM = B_HEADS * B_KEY_DIM
B_V_DIM = B_HEADS * B_VAL_DIM
EVEN_IN = A_Q_DIM + 2 * A_KV_DIM + 2 * B_QK_DIM + 2 * B_V_DIM
EVEN_MIX = A_Q_DIM + B_V_DIM
EVEN_SPLITS = (A_Q_DIM,
               A_Q_DIM + A_KV_DIM,
               A_Q_DIM + 2 * A_KV_DIM,
               A_Q_DIM + 2 * A_KV_DIM + B_QK_DIM,
               A_Q_DIM + 2 * A_KV_DIM + 2 * B_QK_DIM,
               A_Q_DIM + 2 * A_KV_DIM + 2 * B_QK_DIM + B_V_DIM)
C_DIM = C_HEADS * HEAD_DIM
ODD_IN = 3 * C_DIM
N_EVEN = (DEPTH + 1) // 2
N_ODD = DEPTH // 2

kernel_name = "chunk_causal_hybrid_swa_hgrn2_relpos_convffn"

F32 = jnp.float32


def rms_norm(x, w):
    xf = x.astype(F32)
    y = xf * lax.rsqrt(jnp.mean(xf * xf, axis=-1, keepdims=True) + EPS)
    return (y * w.astype(F32)).astype(x.dtype)


def rope(x):
    S, D = x.shape[1], x.shape[3]
    inv_freq = ROPE_THETA ** (-jnp.arange(0, D, 2, dtype=F32) / D)
    ang = jnp.arange(S, dtype=F32)[:, None] * inv_freq[None, :]
    cos = jnp.cos(ang)[None, :, None, :]
    sin = jnp.sin(ang)[None, :, None, :]
    xf = x.astype(F32)
    x1, x2 = xf[..., : D // 2], xf[..., D // 2:]
    return jnp.concatenate([x1 * cos - x2 * sin, x2 * cos + x1 * sin], axis=-1).astype(x.dtype)


def chunk_band_attention(q, k, v, n_prev, bias, sinks):
    B, S, Hq, D = q.shape
    Hkv = k.shape[2]
    G = Hq // Hkv
    n_chunks = S // CHUNK
    pad = n_prev * CHUNK
    band = pad + CHUNK
    scale = D ** -0.5
    k_pad = jnp.pad(k, ((0, 0), (pad, 0), (0, 0), (0, 0)))
    v_pad = jnp.pad(v, ((0, 0), (pad, 0), (0, 0), (0, 0)))
    qc = jnp.moveaxis(q.reshape(B, n_chunks, CHUNK, Hkv, G, D), 1, 0)
    key_idx = jnp.arange(band)

    def one_chunk(args):
        c, q_c = args
        start = c * CHUNK
        k_c = lax.dynamic_slice_in_dim(k_pad, start, band, axis=1)
        v_c = lax.dynamic_slice_in_dim(v_pad, start, band, axis=1)
        s = jnp.einsum('bqhgd,bjhd->bhgqj', q_c, k_c).astype(F32) * scale
        if bias is not None:
            s = s + bias.reshape(Hkv, G, CHUNK, band).astype(F32)
        valid = key_idx >= pad - start
        s = jnp.where(valid, s, -jnp.inf)
        if sinks is not None:
            sk = sinks.reshape(Hkv, G).astype(F32)[None, :, :, None, None]
            m = jnp.maximum(jnp.max(s, axis=-1, keepdims=True), sk)
            p = jnp.exp(s - m)
            p = p / (jnp.sum(p, axis=-1, keepdims=True) + jnp.exp(sk - m))
        else:
            p = jax.nn.softmax(s, axis=-1)
        o = jnp.einsum('bhgqj,bjhd->bqhgd', p.astype(v.dtype), v_c)
        return o.reshape(B, CHUNK, Hq, D)

    out = lax.map(one_chunk, (jnp.arange(n_chunks), qc))
    return jnp.moveaxis(out, 0, 1).reshape(B, S, Hq, D)


def hgrn2(q, f_logit, i, g, lb, norm_w):
    B, S, H, DK = q.shape
    DV = i.shape[-1]
    N = S // B_BLOCK
    lbh = lb.reshape(H, DK).astype(F32)
    f = lbh + (1.0 - lbh) * jax.nn.sigmoid(f_logit.astype(F32))
    log_f = jnp.log(f)
    k = 1.0 - f

    def blk(t):
        return t.reshape(B, N, B_BLOCK, H, t.shape[-1])

    qf, kf, vf, lf = blk(q.astype(F32)), blk(k), blk(i.astype(F32)), blk(log_f)
    cum = jnp.cumsum(lf, axis=2)
    last = cum[:, :, -1:]
    q_dec = qf * jnp.exp(cum)
    k_dec = kf * jnp.exp(-cum)
    k_end = kf * jnp.exp(last - cum)
    blk_decay = jnp.exp(last[:, :, 0])
    causal = jnp.tril(jnp.ones((B_BLOCK, B_BLOCK), dtype=bool))
    s = jnp.einsum('bnlhd,bnmhd->bnhlm', q_dec, k_dec)
    s = jnp.where(causal, s, 0.0)
    o_intra = jnp.einsum('bnhlm,bnmhe->bnlhe', s, vf)

    def step(state, xs):
        qd, ke, vv, dec = xs
        o = jnp.einsum('blhd,bhde->blhe', qd, state)
        state = dec[..., None] * state + jnp.einsum('blhd,blhe->bhde', ke, vv)
        return state, o

    state0 = jnp.zeros((B, H, DK, DV), F32)
    xs = (jnp.moveaxis(q_dec, 1, 0), jnp.moveaxis(k_end, 1, 0),
          jnp.moveaxis(vf, 1, 0), jnp.moveaxis(blk_decay, 1, 0))
    _, o_inter = lax.scan(step, state0, xs)
    o = (o_intra + jnp.moveaxis(o_inter, 0, 1)).reshape(B, S, H, DV)
    o = o * lax.rsqrt(jnp.mean(o * o, axis=-1, keepdims=True) + EPS) * norm_w.reshape(H, DV).astype(F32)
    return (o * jax.nn.silu(g.astype(F32))).astype(i.dtype)


def even_mixer(y, w_in, w_out, sinks, lb, gnorm_w):
    B, S, _ = y.shape
    proj = y @ w_in
    qa, ka, va, qb, fb, ib, gb = jnp.split(proj, EVEN_SPLITS, axis=-1)
    qa = rope(qa.reshape(B, S, A_Q_HEADS, HEAD_DIM))
    ka = rope(ka.reshape(B, S, A_KV_HEADS, HEAD_DIM))
    va = va.reshape(B, S, A_KV_HEADS, HEAD_DIM)
    oa = chunk_band_attention(qa, ka, va, A_PREV_CHUNKS, None, sinks)
    ob = hgrn2(qb.reshape(B, S, B_HEADS, B_KEY_DIM), fb.reshape(B, S, B_HEADS, B_KEY_DIM),
               ib.reshape(B, S, B_HEADS, B_VAL_DIM), gb.reshape(B, S, B_HEADS, B_VAL_DIM),
               lb, gnorm_w)
    o = jnp.concatenate([oa.reshape(B, S, A_Q_DIM), ob.reshape(B, S, B_V_DIM)], axis=-1)
    return o @ w_out


def odd_mixer(y, w_in, w_out, rel_table):
    B, S, _ = y.shape
    proj = y @ w_in
    q, k, v = jnp.split(proj, (C_DIM, 2 * C_DIM), axis=-1)
    q = q.reshape(B, S, C_HEADS, HEAD_DIM)
    k = k.reshape(B, S, C_HEADS, HEAD_DIM)
    v = v.reshape(B, S, C_HEADS, HEAD_DIM)
    pad = C_PREV_CHUNKS * CHUNK
    band = pad + CHUNK
    rel = jnp.arange(CHUNK)[:, None] + pad - jnp.arange(band)[None, :]
    idx = jnp.clip(rel, -REL_CLIP, REL_CLIP) + REL_CLIP
    bias = rel_table[:, idx]
    o = chunk_band_attention(q, k, v, C_PREV_CHUNKS, bias, None)
    return o.reshape(B, S, C_DIM) @ w_out


def conv_ffn(y, w_up, conv_w, conv_b, w_down):
    S = y.shape[1]
    up = y @ w_up
    u, v = jnp.split(up, 2, axis=-1)
    u_pad = jnp.pad(u, ((0, 0), (CONV_WIDTH - 1, 0), (0, 0)))
    c = conv_b
    for j in range(CONV_WIDTH):
        c = c + conv_w[j] * u_pad[:, j:j + S]
    return (jax.nn.gelu(c, approximate=False) * v) @ w_down


def setup_inputs(seed: int = 0) -> dict:
    key = jax.random.key(seed)
    ks = jax.random.split(key, 16)
    nrm = jax.random.normal
    x = nrm(ks[0], (BATCH, SEQ, D_MODEL), F32)
    even_w_in = nrm(ks[1], (N_EVEN, D_MODEL, EVEN_IN), F32) * D_MODEL ** -0.5
    even_w_out = nrm(ks[2], (N_EVEN, EVEN_MIX, D_MODEL), F32) * EVEN_MIX ** -0.5
    even_sinks = nrm(ks[3], (N_EVEN, A_Q_HEADS), F32) * 0.5
    hgrn_lb_logits = nrm(ks[4], (N_EVEN + 1, B_QK_DIM), F32) * 0.1
    hgrn_norm_w = 1.0 + 0.02 * nrm(ks[5], (N_EVEN, B_V_DIM), F32)
    odd_w_in = nrm(ks[6], (N_ODD, D_MODEL, ODD_IN), F32) * D_MODEL ** -0.5
    odd_w_out = nrm(ks[7], (N_ODD, C_DIM, D_MODEL), F32) * C_DIM ** -0.5
    odd_rel_bias = nrm(ks[8], (N_ODD, C_HEADS, 2 * REL_CLIP + 1), F32) * 0.1
    ffn_w_up = nrm(ks[9], (DEPTH, D_MODEL, 2 * D_FF), F32) * D_MODEL ** -0.5
    ffn_conv_w = nrm(ks[10], (DEPTH, CONV_WIDTH, D_FF), F32) * CONV_WIDTH ** -0.5
    ffn_conv_b = nrm(ks[11], (DEPTH, D_FF), F32) * 0.02
    ffn_w_down = nrm(ks[12], (DEPTH, D_FF, D_MODEL), F32) * D_FF ** -0.5
    norm_w = 1.0 + 0.02 * nrm(ks[13], (DEPTH, 4, D_MODEL), F32)
    return {"x": x, "even_w_in": even_w_in, "even_w_out": even_w_out, "even_sinks": even_sinks,
            "hgrn_lb_logits": hgrn_lb_logits, "hgrn_norm_w": hgrn_norm_w,
            "odd_w_in": odd_w_in, "odd_w_out": odd_w_out, "odd_rel_bias": odd_rel_bias,
            "ffn_w_up": ffn_w_up, "ffn_conv_w": ffn_conv_w, "ffn_conv_b": ffn_conv_b,
            "ffn_w_down": ffn_w_down, "norm_w": norm_w}


def reference(x, even_w_in, even_w_out, even_sinks, hgrn_lb_logits, hgrn_norm_w,
              odd_w_in, odd_w_out, odd_rel_bias, ffn_w_up, ffn_conv_w, ffn_conv_b,
              ffn_w_down, norm_w):
    lb_all = jnp.cumsum(jax.nn.softmax(hgrn_lb_logits.astype(F32), axis=0), axis=0)
    h = x
    for layer in range(DEPTH):
        nw = norm_w[layer]
        j = layer // 2
        y = rms_norm(h, nw[0])
        if layer % 2 == 0:
            y = even_mixer(y, even_w_in[j], even_w_out[j], even_sinks[j], lb_all[j], hgrn_norm_w[j])
        else:
            y = odd_mixer(y, odd_w_in[j], odd_w_out[j], odd_rel_bias[j])
        h = h + rms_norm(y, nw[1])
        y = conv_ffn(rms_norm(h, nw[2]), ffn_w_up[layer], ffn_conv_w[layer], ffn_conv_b[layer], ffn_w_down[layer])
        h = h + rms_norm(y, nw[3])
    return h
```

```python
import numpy as np
import concourse.bass as bass
import concourse.mybir as mybir
from concourse.bass_utils import run_bass_kernel_spmd

F32 = mybir.dt.float32
BF16 = mybir.dt.bfloat16
AF = mybir.ActivationFunctionType
ALU = mybir.AluOpType
AX = mybir.AxisListType

ENG_ATTR = {"pe": "tensor", "act": "scalar", "dve": "vector", "pool": "gpsimd", "sp": "sync"}


class Buf:
    __slots__ = ("name", "w", "r", "excl")

    def __init__(self, name, excl=False):
        self.name = name
        self.w = None
        self.r = []
        self.excl = excl


class Sched:
    def __init__(self, nc, same_engine_sync=True):
        self.nc = nc
        self.same = same_engine_sync
        self.ops = {k: [] for k in ENG_ATTR}
        self.cnt = {k: 0 for k in ENG_ATTR}
        self.seen = {k: {} for k in ENG_ATTR}
        self.sems = {}
        self.dma_total = {}
        for k in ENG_ATTR:
            self.sems[k] = nc.alloc_semaphore("s_" + k)

    def dma_sem(self, key):
        if key not in self.sems:
            self.sems[key] = self.nc.alloc_semaphore("d_" + key)
            self.dma_total[key] = 0
        return key

    def _deps(self, eng, reads, writes):
        deps = {}
        for b in reads:
            if b.w is not None:
                k, v = b.w
                deps[k] = max(deps.get(k, 0), v)
            if b.excl:
                for (k, v) in b.r:
                    if k != eng:
                        deps[k] = max(deps.get(k, 0), v)
        for b in writes:
            if b.w is not None:
                k, v = b.w
                deps[k] = max(deps.get(k, 0), v)
            for (k, v) in b.r:
                deps[k] = max(deps.get(k, 0), v)
        seen = self.seen[eng]
        for k, v in deps.items():
            if k == eng and (eng == "pe" or not self.same):
                continue
            if seen.get(k, 0) < v:
                seen[k] = v
                sem = self.sems[k]
                self.ops[eng].append(lambda e, sem=sem, v=v: e.wait_ge(sem, v))

    def op(self, eng, fn, reads=(), writes=(), inc=True):
        self._deps(eng, reads, writes)
        if inc:
            self.cnt[eng] += 1
            tok = (eng, self.cnt[eng])
            sem = self.sems[eng]
            self.ops[eng].append(lambda e, fn=fn, sem=sem: fn(e).then_inc(sem, 1))
        else:
            tok = (eng, self.cnt[eng] + 1)
            self.ops[eng].append(lambda e, fn=fn: fn(e))
        for b in reads:
            b.r.append(tok)
        for b in writes:
            b.w = tok
            b.r = []
        return tok

    def dma(self, eng, semkey, fn, reads=(), writes=()):
        self.dma_sem(semkey)
        self._deps(eng, reads, writes)
        self.dma_total[semkey] += 16
        tok = (semkey, self.dma_total[semkey])
        sem = self.sems[semkey]
        self.ops[eng].append(lambda e, fn=fn, sem=sem: fn(e).then_inc(sem, 16))
        for b in reads:
            b.r.append(tok)
        for b in writes:
            b.w = tok
            b.r = []
        return tok

    def wait_all(self, eng, bufs):
        self._deps(eng, (), bufs)

    def emit(self):
        nc = self.nc
        with nc.Block() as block:
            for k, attr in ENG_ATTR.items():
                ops = self.ops[k]
                if not ops:
                    continue

                def body(e, ops=ops):
                    for f in ops:
                        f(e)
                getattr(block, attr)(body)


D = 1024
KC = 8
DFF = 2816
FC = 22
G = 512
TPG = 4
EPS = 1e-6
SLOT = 5632
NSLOT = 3
NEG = -30000.0


def ffn_piece_list():
    out = [("up%d" % j, 4096) for j in range(11)]
    out += [("dn%d_%d" % (cg, hf), 5632) for cg in range(2) for hf in range(2)]
    return out


def ffn_pieces_host(w_up, w_down):
    res = []
    wu = w_up.reshape(KC, 128, 2 * DFF)
    for j in range(11):
        cols = np.concatenate([np.arange(2 * j * 128, (2 * j + 2) * 128),
                               DFF + np.arange(2 * j * 128, (2 * j + 2) * 128)])
        pc = wu[:, :, cols].transpose(1, 0, 2).reshape(128, KC * 512)
        res.append(("up%d" % j, pc))
    wd = w_down.reshape(FC, 128, D)
    for cg in range(2):
        for hf in range(2):
            pc = wd[hf * 11:(hf + 1) * 11, :, cg * 512:(cg + 1) * 512].transpose(1, 0, 2).reshape(128, 11 * 512)
            res.append(("dn%d_%d" % (cg, hf), pc))
    return res


def const_layout():
    lay = {}
    off = 0

    def add(name, w):
        nonlocal off
        lay[name] = (off, w)
        off += w
    for L in range(2):
        add("nwT%d_0" % L, KC)
        add("nwT%d_2" % L, KC)
        add("nwR%d_1" % L, D)
        add("nwR%d_3" % L, D)
        add("cw%d" % L, FC * 3)
        add("cb%d" % L, FC)
    add("ident", 128)
    add("lbl", 8)
    add("sinks", 8)
    add("gw", 512)
    add("mc", 128)
    add("crep", 16)
    add("m0", 128)
    add("m4", 128)
    lay["_total"] = (off, 0)
    return lay


def consts_host(inp):
    lay = const_layout()
    c = np.zeros((128, lay["_total"][0]), np.float32)

    def put(name, arr):
        o, w = lay[name]
        c[:, o:o + w] = arr.reshape(128, w) if arr.shape[0] == 128 else np.broadcast_to(arr.reshape(1, w), (128, w))
    nw = inp["norm_w"]
    for L in range(2):
        put("nwT%d_0" % L, nw[L, 0].reshape(KC, 128).T)
        put("nwT%d_2" % L, nw[L, 2].reshape(KC, 128).T)
        put("nwR%d_1" % L, nw[L, 1])
        put("nwR%d_3" % L, nw[L, 3])
        cw = inp["ffn_conv_w"][L]
        put("cw%d" % L, cw.reshape(3, FC, 128).transpose(2, 1, 0).reshape(128, FC * 3))
        put("cb%d" % L, inp["ffn_conv_b"][L].reshape(FC, 128).T)
    put("ident", np.eye(128, dtype=np.float32))
    put("lbl", inp["hgrn_lb_logits"][0:2].reshape(2, 4, 128).transpose(2, 0, 1).reshape(128, 8))
    put("sinks", inp["even_sinks"][0])
    put("gw", inp["hgrn_norm_w"][0])
    put("mc", (np.arange(128)[:, None] <= np.arange(128)[None, :]).astype(np.float32))
    put("crep", inp["odd_rel_bias"][0][:, 256])
    jj = np.arange(128)[:, None]
    qq = np.arange(128)[None, :]
    put("m0", np.where((jj >= 64) & (qq < 64), NEG, 0.0).astype(np.float32))
    put("m4", np.where((jj < 64) & (qq >= 64), NEG, 0.0).astype(np.float32))
    return c


def relb_host(inp):
    tab = inp["odd_rel_bias"][0]
    jj = np.arange(128)[:, None]
    qq = np.arange(128)[None, :]
    i0 = (qq - jj) + 128
    i1 = np.minimum(128 + qq - jj, 128) + 128
    b0 = tab[:, i0]
    b1 = tab[:, i1]
    r = np.stack([b0, b1], axis=1)
    return np.ascontiguousarray(r.transpose(2, 0, 1, 3).reshape(128, 16 * 2 * 128).astype(np.float32))


def odd_piece_list():
    return [(n, 4096) for n in ("ik0", "ik1", "iv0", "iv1", "iq0", "iq1", "ow0", "ow1")]


def odd_pieces_host(w_in, w_out):
    wi = w_in.reshape(KC, 128, 3 * D)
    wo = w_out.reshape(KC, 128, D)
    res = []

    def pc(w, c0):
        return w[:, :, c0:c0 + 512].transpose(1, 0, 2).reshape(128, KC * 512)
    for i in range(2):
        res.append(("ik%d" % i, pc(wi, D + i * 512)))
    for i in range(2):
        res.append(("iv%d" % i, pc(wi, 2 * D + i * 512)))
    for i in range(2):
        res.append(("iq%d" % i, pc(wi, i * 512)))
    for i in range(2):
        res.append(("ow%d" % i, pc(wo, i * 512)))
    return res


def layer_piece_list(L):
    if L == 1:
        return odd_piece_list() + ffn_piece_list()
    return even_piece_list() + ffn_piece_list()


EVEN_FM = [["ka", "rka", "qa0", "rqa0"], ["qa1", "rqa1", "qa2", "rqa2"], ["qa3", "rqa3", "qb0", "fb0"],
           ["qb1", "fb1", "qb2", "fb2"], ["qb3", "fb3"]]


def even_piece_list():
    out = [("e5", KC * 640), ("e6", KC * 512)]
    out += [("e%d" % i, KC * 128 * len(ch)) for i, ch in enumerate(EVEN_FM)]
    out += [("ow0", 4096), ("ow1", 4096)]
    return out


def even_cols():
    rot = (np.arange(64) + 32) % 64
    cols = {}
    cols["ka"] = 512 + np.arange(128)
    cols["rka"] = 512 + np.concatenate([rot, 64 + rot])
    for c in range(4):
        h0, h1 = c, 4 + c
        cols["qa%d" % c] = np.concatenate([h0 * 64 + np.arange(64), h1 * 64 + np.arange(64)])
        cols["rqa%d" % c] = np.concatenate([h0 * 64 + rot, h1 * 64 + rot])
        cols["qb%d" % c] = 768 + c * 128 + np.arange(128)
        cols["fb%d" % c] = 1280 + c * 128 + np.arange(128)
    return cols


def even_pieces_host(inp):
    wi = inp["even_w_in"][0].reshape(KC, 128, 2816)
    wo = inp["even_w_out"][0].reshape(KC, 128, D)
    cols = even_cols()
    res = []
    cc = np.concatenate([640 + np.arange(128), 1792 + np.arange(512)])
    res.append(("e5", wi[:, :, cc].transpose(1, 0, 2).reshape(128, KC * 640)))
    cc = 2304 + np.arange(512)
    res.append(("e6", wi[:, :, cc].transpose(1, 0, 2).reshape(128, KC * 512)))
    for i, ch in enumerate(EVEN_FM):
        cc = np.concatenate([cols[n] for n in ch])
        res.append(("e%d" % i, wi[:, :, cc].transpose(1, 0, 2).reshape(128, KC * len(cc))))
    for i in range(2):
        res.append(("ow%d" % i, wo[:, :, i * 512:(i + 1) * 512].transpose(1, 0, 2).reshape(128, KC * 512)))
    return res


def rope_host(seq):
    inv = (10000.0 ** (-np.arange(0, 64, 2, dtype=np.float32) / 64)).astype(np.float32)
    ang = np.arange(seq, dtype=np.float32)[None, :] * inv[:, None]
    cos = np.cos(ang).astype(np.float32)
    sin = np.sin(ang).astype(np.float32)
    c64 = np.concatenate([cos, cos], 0)
    s64 = np.concatenate([-sin, sin], 0)
    t = np.stack([np.concatenate([c64, c64], 0), np.concatenate([s64, s64], 0)], axis=1)
    return np.ascontiguousarray(t.astype(np.float32))


def weights_host(inp):
    chunks = []
    for L in range(2):
        pcs = (odd_pieces_host(inp["odd_w_in"][0], inp["odd_w_out"][0]) if L == 1 else even_pieces_host(inp))
        pcs = pcs + ffn_pieces_host(inp["ffn_w_up"][L], inp["ffn_w_down"][L])
        want = layer_piece_list(L)
        assert [p[0] for p in pcs] == [w[0] for w in want]
        for (n, a), (_, nco) in zip(pcs, want):
            assert a.shape == (128, nco), (n, a.shape, nco)
            chunks.append(np.ascontiguousarray(a, dtype=np.float32).reshape(-1))
    return np.concatenate(chunks)


class K:
    pass


def build(nseq, seq, layers=(0, 1), parts=("mixer", "ffn")):
    nc = bass.Bass("TRN2", target_bir_lowering=False)
    NG = seq // G
    NTOK = nseq * seq
    lay = const_layout()
    NCST = lay["_total"][0]
    ptab = {}
    off = 0
    for L in range(2):
        for (n, nco) in layer_piece_list(L):
            ptab[(L, n)] = (off, nco)
            off += 128 * nco
    WTOT = off

    x_d = nc.dram_tensor("x", [NTOK, D], F32, kind="ExternalInput").ap()
    out_d = nc.dram_tensor("out", [NTOK, D], F32, kind="ExternalOutput").ap()
    wsrc_d = nc.dram_tensor("wsrc", [WTOT // 2048, 2048], F32, kind="ExternalInput").ap()
    cst_d = nc.dram_tensor("cst", [128, NCST], F32, kind="ExternalInput").ap()
    wbf_d = nc.dram_tensor("wbf", [WTOT // 2048, 2048], BF16, kind="Internal").ap()
    relb_d = nc.dram_tensor("relb", [128, 4096], F32, kind="ExternalInput").ap()
    rope_d = nc.dram_tensor("rope", [128, 2, seq], F32, kind="ExternalInput").ap()

    S = Sched(nc)
    k = K()
    k.nc, k.S = nc, S

    def sb(name, shape, dt):
        return nc.alloc_sbuf_tensor("s_" + name, shape, dt)

    cst = sb("cst", [128, NCST], F32)
    cstB = Buf("cst")
    ident = sb("ident", [128, 128], BF16)
    identB = Buf("ident")
    NH = 1
    h = [sb("h%d" % i, [128, TPG, D], F32) for i in range(NH)]
    hB = [[Buf("h%d_%d" % (i, t)) for t in range(TPG)] for i in range(NH)]
    ring = [sb("ring%d" % i, [128, SLOT], BF16) for i in range(NSLOT)]
    ringB = [Buf("ring%d" % i) for i in range(NSLOT)]
    yn = [sb("yn%d" % i, [128, D], BF16) for i in range(2)]
    ynB = [Buf("yn%d" % i) for i in range(2)]
    yT = sb("yT", [128, KC, G], BF16)
    yTB = [Buf("yT%d" % t) for t in range(TPG)]
    arena = sb("arena", [128, FC * G], BF16)
    gT = arena[:].rearrange("p (f t) -> p f t", f=FC)
    gTB = Buf("gT")
    qT = arena[:, 0:4096].rearrange("p (k t) -> p k t", k=KC)
    qTB = Buf("qT")
    otok = arena[:, 4096:8192].rearrange("p (t d) -> p t d", t=TPG)
    otokB = Buf("otok")
    PT = [arena[:, 8192 + i * 1280:8192 + (i + 1) * 1280] for i in range(2)]
    PTB = [Buf("PT%d" % i) for i in range(2)]
    mixBs = [qTB, otokB] + PTB

    def alias_fence(dsts, srcs):
        for d_ in dsts:
            for s_ in srcs:
                if s_.w is not None:
                    d_.r.append(s_.w)
                d_.r.extend(s_.r)
    kT = sb("kT", [128, KC, 2 * G], BF16)
    kTB = [Buf("kT%d" % i) for i in range(2)]
    Vr = sb("Vr", [128, 8, 16, 65], BF16)
    VB = [Buf("V%d" % i) for i in range(2)]
    bt = sb("bt", [128, 16, 2, 128], BF16)
    btB = Buf("bt")
    m4 = sb("m4", [128, 128], BF16)
    m4B = Buf("m4")
    rden = sb("rden", [128, 8], F32)
    rdenB = Buf("rden")
    qaT = arena[:, 0:2048].rearrange("p (k t) -> p k t", k=4)
    iTok = arena[:, 2048:4096].rearrange("p (t d) -> p t d", t=TPG)
    iTokB = Buf("iTok")
    PTa = [arena[:, 8192 + i * 512:8192 + (i + 1) * 512] for i in range(2)]
    sg = arena[:, 9216:11264].rearrange("p (t d) -> p t d", t=TPG)
    sgB = Buf("sg")
    mixBs.extend([iTokB, sgB])
    ropeT = sb("ropeT", [128, 2, G], F32)
    ropeB = Buf("rope")
    kaT = sb("kaT", [128, 2 * G], BF16)
    kaTB = [Buf("kaT%d" % i) for i in range(2)]
    Va = sb("Va", [128, 8, 2, 65], BF16)
    VaB = [Buf("Va%d" % i) for i in range(2)]
    esink = sb("esink", [128, 8], F32)
    lbv = sb("lbv", [128, 8], F32)
    evB = Buf("evconst")
    mcb = sb("mcb", [128, 128], BF16)
    m0b = sb("m0b", [128, 128], BF16)
    ones = sb("ones", [128, 128], F32)
    mfull = sb("mfull", [128, 2, 2, 128], BF16)
    qdT = [sb("qdT%d" % i, [128, G], BF16) for i in range(2)]
    qdTB = [Buf("qdT%d" % i) for i in range(2)]
    kdT = [sb("kdT%d" % i, [128, G], BF16) for i in range(2)]
    kdTB = [Buf("kdT%d" % i) for i in range(2)]
    kdTok = [sb("kdTok%d" % i, [128, TPG, 128], BF16) for i in range(2)]
    kdTokB = [Buf("kdTok%d" % i) for i in range(2)]
    sTm = [sb("sTm%d" % i, [128, 128], BF16) for i in range(4)]
    sTmB = [Buf("sTm%d" % i) for i in range(4)]
    state = sb("state", [128, 4, 128], F32)
    stS = sb("stS", [128, 4, 128], BF16)
    stateB = [Buf("state%d" % i) for i in range(4)]
    stSB = [Buf("stS%d" % i) for i in range(4)]
    tmpd = [sb("tmpd%d" % i, [128, 128], F32) for i in range(4)]
    tmpdB = [Buf("tmpd%d" % i) for i in range(4)]
    hvec = [sb("hvec%d" % i, [128, 4, 4], F32) for i in range(2)]
    hvecB = [Buf("hvec%d" % i) for i in range(2)]
    ssh = sb("ssh", [128, 16], F32)
    sshB = [Buf("ssh%d" % i) for i in range(16)]
    rsh = sb("rsh", [128, 16], F32)
    rshB = Buf("rsh")
    tmpo = sb("tmpo", [128, TPG, D], F32)
    tmpoB = [Buf("tmpo%d" % t) for t in range(TPG)]
    junks = [sb("junk%d" % i, [128, D], BF16) for i in range(2)]
    junkBs = [Buf("junk%d" % i) for i in range(2)]
    k.junk_i = 0

    def next_junk():
        k.junk_i += 1
        return junks[k.junk_i % 2], junkBs[k.junk_i % 2]
    csb = [sb("csb%d" % i, [128, G], F32) for i in range(2)]
    csbB = [Buf("csb%d" % i) for i in range(2)]
    gsb = [sb("gsb%d" % i, [128, G], F32) for i in range(2)]
    gsbB = [Buf("gsb%d" % i) for i in range(2)]
    carry = [sb("carry%d" % L, [128, FC, 2], F32) for L in range(2)]
    carryB = [Buf("carry%d" % L) for L in range(2)]
    ss4 = sb("ss4", [128, 4], F32)
    ss4B = [Buf("ss4_%d" % t) for t in range(TPG)]
    ssp = sb("ssp", [128, 4, 2], F32)
    sspB = [Buf("ssp%d" % t) for t in range(TPG)]
    rstd = sb("rstd", [128, 4], F32)
    rstdB = [Buf("rstd%d" % t) for t in range(TPG)]
    psum = [nc.alloc_psum_tensor("ps%d" % i, [128, 512], F32) for i in range(8)]
    psumB = [Buf("ps%d" % i, excl=True) for i in range(8)]
    k.bank_i = 0
    k.bank_n = 8

    def bank():
        i = k.bank_i % k.bank_n
        k.bank_i += 1
        return psum[i], psumB[i]

    def cs(name, a=None, b=None):
        o, w = lay[name]
        if a is None:
            return cst[:, o:o + w]
        return cst[:, o + a:o + b]

    S.dma("sp", "cst", lambda e: e.dma_start(out=cst[:], in_=cst_d), writes=[cstB])
    S.op("dve", lambda e: e.tensor_copy(out=ident[:], in_=cs("ident")), reads=[cstB], writes=[identB])
    if 1 in layers and "mixer" in parts:
        S.dma("sp", "relb", lambda e: e.dma_start(out=tmpo[:].rearrange("p t d -> p (t d)"), in_=relb_d), writes=tmpoB)
        S.op("pool", lambda e: e.memset(Vr[:], 1.0), writes=VB)
        S.op("act", lambda e: e.activation(out=m4[:], in_=cs("m4"), func=AF.Exp), reads=[cstB], writes=[m4B])
        rb = tmpo[:].rearrange("p t d -> p (t d)").rearrange("p (h k q) -> p h k q", h=16, k=2)
        for hh in range(16):
            S.op("dve", lambda e, hh=hh: e.scalar_tensor_tensor(out=rb[:, hh, 0, :], in0=rb[:, hh, 0, :], scalar=cs("crep", hh, hh + 1), op0=ALU.subtract, in1=cs("m0"), op1=ALU.add),
                 reads=tmpoB + [cstB], writes=tmpoB)
            S.op("dve", lambda e, hh=hh: e.tensor_scalar(out=rb[:, hh, 1, :], in0=rb[:, hh, 1, :], scalar1=cs("crep", hh, hh + 1), scalar2=None, op0=ALU.subtract),
                 reads=tmpoB + [cstB], writes=tmpoB)
            S.op("act", lambda e, hh=hh: e.activation(out=bt[:, hh, :, :], in_=rb[:, hh, :, :], func=AF.Exp), reads=tmpoB, writes=[btB])
    if 0 in layers and "mixer" in parts:
        S.op("pool", lambda e: e.memset(Va[:], 1.0), writes=VaB)
        S.op("pool", lambda e: e.memset(ones[:], 1.0), writes=[evB])
        S.op("dve", lambda e: e.tensor_copy(out=mcb[:], in_=cs("mc")), reads=[cstB], writes=[evB])
        if not (1 in layers and "mixer" in parts):
            S.op("act", lambda e: e.activation(out=m4[:], in_=cs("m4"), func=AF.Exp), reads=[cstB], writes=[m4B])
        S.op("act", lambda e: e.activation(out=m0b[:], in_=cs("m0"), func=AF.Exp), reads=[cstB], writes=[evB])
        for e_ in range(2):
            S.op("dve", lambda e, e_=e_: e.tensor_copy(out=mfull[:, e_, 0, :], in_=m4[:]), reads=[m4B], writes=[evB])
            S.op("dve", lambda e, e_=e_: e.tensor_copy(out=mfull[:, e_, 1, :], in_=m0b[:]), reads=[evB], writes=[evB])
        S.op("act", lambda e: e.activation(out=esink[:], in_=cs("sinks"), func=AF.Exp), reads=[cstB], writes=[evB])
        S.op("dve", lambda e: e.tensor_tensor(out=lbv[:, 4:8], in0=cs("lbl", 0, 4), in1=cs("lbl", 4, 8), op=ALU.subtract), reads=[cstB], writes=[evB])
        S.op("act", lambda e: e.activation(out=lbv[:, 0:4], in_=lbv[:, 4:8], func=AF.Sigmoid), reads=[evB], writes=[evB])
        S.op("dve", lambda e: e.tensor_scalar(out=lbv[:, 4:8], in0=lbv[:, 0:4], scalar1=-1.0, scalar2=1.0, op0=ALU.mult, op1=ALU.add), reads=[evB], writes=[evB])
    cvB = {}
    for L in layers:
        for (n, nco) in layer_piece_list(L):
            sec = n[:2]
            key = "cv%d%s" % (L, sec)
            cvB.setdefault(key, Buf(key))
            o, _ = ptab[(L, n)]
            r0, r1 = o // 2048, (o + 128 * nco) // 2048
            S.dma("pool", key, lambda e, r0=r0, r1=r1: e.dma_start(out=wbf_d[r0:r1, :], in_=wsrc_d[r0:r1, :]),
                  writes=[cvB[key]])

    order = []
    for s in range(nseq):
        for g in range(NG):
            for L in layers:
                for (n, nco) in layer_piece_list(L):
                    if (n[:2] in ("up", "dn")) and "ffn" not in parts:
                        continue
                    if (n[:2] not in ("up", "dn")) and "mixer" not in parts:
                        continue
                    order.append((L, n))
    k.w_issue = 0
    k.w_use = 0

    def w_issue_upto(i):
        while k.w_issue <= i and k.w_issue < len(order):
            j = k.w_issue
            L, n = order[j]
            o, nco = ptab[(L, n)]
            slot = j % NSLOT
            key = "cv%d%s" % (L, n[:2])
            src = wbf_d.rearrange("r c -> (r c)")[o:o + 128 * nco].rearrange("(p c) -> p c", p=128)
            S.dma("sp", "w%d" % slot, lambda e, slot=slot, nco=nco, src=src: e.dma_start(out=ring[slot][:, 0:nco], in_=src),
                  reads=[cvB[key]], writes=[ringB[slot]])
            k.w_issue += 1

    def w_get(L, n):
        i = k.w_use
        assert order[i] == (L, n), (order[i], L, n)
        w_issue_upto(i + NSLOT - 1)
        k.w_use += 1
        return ring[i % NSLOT], ringB[i % NSLOT]

    def rstd_tile(t):
        S.op("act", lambda e: e.activation(out=rstd[:, t:t + 1], in_=ss4[:, t:t + 1], func=AF.Sqrt, scale=1.0 / D, bias=EPS),
             reads=[ss4B[t]], writes=[rstdB[t]])
        S.op("dve", lambda e: e.reciprocal(out=rstd[:, t:t + 1], in_=rstd[:, t:t + 1]), reads=[rstdB[t]], writes=[rstdB[t]])

    def norm_tile(hb, hbB, nwT, t):
        junk, junkB = next_junk()
        S.op("act", lambda e: e.activation(out=junk[:], in_=hb[:, t, :], func=AF.Square, accum_out=ss4[:, t:t + 1]),
             reads=[hbB[t]], writes=[ss4B[t], junkB])
        rstd_tile(t)
        y, yB = yn[t % 2], ynB[t % 2]
        S.op("dve", lambda e: e.tensor_scalar(out=y[:], in0=hb[:, t, :], scalar1=rstd[:, t:t + 1], scalar2=None, op0=ALU.mult),
             reads=[hbB[t], rstdB[t]], writes=[yB])
        p, pB = bank()
        pb = p[:].bitcast(BF16)
        for kc in range(KC):
            S.op("pe", lambda e, kc=kc: e.transpose(out=pb[:, kc * 128:(kc + 1) * 128], in_=y[:, kc * 128:(kc + 1) * 128], identity=ident[:]),
                 reads=[yB, identB], writes=[pB], inc=(kc == KC - 1))
        S.op("dve", lambda e: e.tensor_tensor(out=yT[:, :, t * 128:(t + 1) * 128], in0=pb.rearrange("p (k t) -> p k t", k=KC),
                                              in1=nwT.unsqueeze(2).broadcast_to([128, KC, 128]), op=ALU.mult),
             reads=[pB, cstB], writes=[yTB[t]])

    def norm_transpose(hb, hbB, nwT):
        for t in range(TPG):
            norm_tile(hb, hbB, nwT, t)

    def post_tile(hb, hbB, t):
        S.op("dve", lambda e: e.tensor_tensor(out=ss4[:, t:t + 1], in0=ssp[:, t, 0:1], in1=ssp[:, t, 1:2], op=ALU.add), reads=[sspB[t]], writes=[ss4B[t]])
        rstd_tile(t)
        S.op("dve", lambda e: e.scalar_tensor_tensor(out=hb[:, t, :], in0=tmpo[:, t, :], scalar=rstd[:, t:t + 1], op0=ALU.mult, in1=hb[:, t, :], op1=ALU.add),
             reads=[tmpoB[t], rstdB[t], hbB[t]], writes=[hbB[t]])

    def post_norm_residual(hb, hbB, nwR_name, produce, stage=9):
        for cg in range(2):
            banks = produce(cg)
            for t in range(TPG):
                p, pB = banks[t]
                junk, junkB = next_junk()
                S.op("act", lambda e, t=t, cg=cg, p=p, junk=junk: e.activation(out=junk[:, 0:512], in_=p[:], func=AF.Square, accum_out=ssp[:, t, cg:cg + 1]),
                     reads=[pB], writes=[sspB[t], junkB])
                S.op("dve", lambda e, t=t, cg=cg, p=p: e.tensor_tensor(out=tmpo[:, t, cg * 512:(cg + 1) * 512], in0=p[:], in1=cs(nwR_name, cg * 512, (cg + 1) * 512), op=ALU.mult),
                     reads=[pB, cstB], writes=[tmpoB[t]])
                if cg == 1:
                    post_tile(hb, hbB, t)

    def evac(i, out, in_, reads, writes, scale=None):
        if i % 2 == 0:
            if scale is None:
                S.op("act", lambda e: e.activation(out=out, in_=in_, func=AF.Identity), reads=reads, writes=writes)
            else:
                S.op("act", lambda e: e.activation(out=out, in_=in_, func=AF.Identity, scale=scale), reads=reads, writes=writes)
        else:
            if scale is None:
                S.op("dve", lambda e: e.tensor_copy(out=out, in_=in_), reads=reads, writes=writes)
            else:
                S.op("dve", lambda e: e.tensor_scalar(out=out, in0=in_, scalar1=scale, scalar2=None, op0=ALU.mult), reads=reads, writes=writes)

    def proj_fm(wv, wB, c, dst, dstB, ei, scale=None):
        p, pB = bank()
        for kc in range(KC):
            S.op("pe", lambda e, kc=kc: e.matmul(p[:], lhsT=wv[:, kc, c * 128:(c + 1) * 128], rhs=yT[:, kc, :], start=(kc == 0), stop=(kc == KC - 1)),
                 reads=[wB] + yTB, writes=[pB], inc=(kc == KC - 1))
        evac(ei, dst, p[:], [pB], [dstB], scale)

    def otok_to_yT(qt):
        p, pB = bank()
        pb = p[:].bitcast(BF16)
        for kc in range(KC):
            S.op("pe", lambda e, kc=kc: e.transpose(out=pb[:, kc * 128:(kc + 1) * 128], in_=otok[:, qt, kc * 128:(kc + 1) * 128], identity=ident[:]),
                 reads=[otokB, identB], writes=[pB], inc=(kc == KC - 1))
        evac(qt, yT[:, :, qt * 128:(qt + 1) * 128], pb.rearrange("p (k t) -> p k t", k=KC), [pB], [yTB[qt]])

    def out_proj_post(L, hb, hbB, nwR_name):
        def produce(cg):
            wt, wB = w_get(L, "ow%d" % cg)
            wv = wt[:, 0:4096].rearrange("p (k c) -> p k c", k=KC)
            banks = [bank() for _ in range(TPG)]
            for t in range(TPG):
                p, pB = banks[t]
                for kc in range(KC):
                    S.op("pe", lambda e, t=t, kc=kc, p=p: e.matmul(p[:], lhsT=yT[:, kc, t * 128:(t + 1) * 128], rhs=wv[:, kc, :], start=(kc == 0), stop=(kc == KC - 1)),
                         reads=[wB, yTB[t]], writes=[pB], inc=(kc == KC - 1))
            return banks
        post_norm_residual(hb, hbB, nwR_name, produce)

    def odd_mixer(L, hb, hbB, g):
        alias_fence(mixBs, [gTB])
        norm_transpose(hb, hbB, cs("nwT%d_0" % L))
        ks = g % 2
        ei = 0
        for i in range(2):
            wt, wB = w_get(L, "ik%d" % i)
            wv = wt[:, 0:4096].rearrange("p (k c) -> p k c", k=KC)
            for c in range(4):
                proj_fm(wv, wB, c, kT[:, 4 * i + c, ks * G:(ks + 1) * G], kTB[ks], ei)
                ei += 1
        for i in range(2):
            wt, wB = w_get(L, "iv%d" % i)
            wv = wt[:, 0:4096].rearrange("p (k c) -> p k c", k=KC)
            for t in range(TPG):
                p, pB = bank()
                for kc in range(KC):
                    S.op("pe", lambda e, t=t, kc=kc, p=p, wv=wv: e.matmul(p[:], lhsT=yT[:, kc, t * 128:(t + 1) * 128], rhs=wv[:, kc, :], start=(kc == 0), stop=(kc == KC - 1)),
                         reads=[wB, yTB[t]], writes=[pB], inc=(kc == KC - 1))
                evac(ei, Vr[:, ks * 4 + t, 8 * i:8 * i + 8, 0:64], p[:].rearrange("p (h d) -> p h d", d=64), [pB], [VB[ks]])
                ei += 1
        for i in range(2):
            wt, wB = w_get(L, "iq%d" % i)
            wv = wt[:, 0:4096].rearrange("p (k c) -> p k c", k=KC)
            for c in range(4):
                proj_fm(wv, wB, c, qT[:, 4 * i + c, :], qTB, ei, scale=0.125)
                ei += 1
        k.bank_n = 5
        unit = 0
        for qt in range(TPG):
            T = g * 4 + qt
            kts = [kt for kt in range(T - 4, T + 1) if kt >= 0]
            jmin = 5 - len(kts)
            for hp in range(8):
                sbk = [bank(), bank(), bank()]
                pt, ptB = PT[unit % 2], PTB[unit % 2]
                unit += 1
                stops = []
                fixes = []
                l0 = [(kt, 0) for kt in kts]
                l0 = l0[-1:] + l0[:-1]
                l1 = [(kt, 1) for kt in kts]
                seq_ = [(kt, 0) for kt in kts] + [(None, None)] + [(kt, 1) for kt in kts]
                for (kt, e_) in seq_:
                    if kt is None:
                        stops.append((lambda e, sbk=sbk: e.matmul(sbk[2][0][:, 256:258], lhsT=ident[:], rhs=ident[:, 0:2], start=True, stop=True),
                                      [identB], [sbk[2][1]]))
                        continue
                    if True:
                        hh = 2 * hp + e_
                        lo, hi = e_ * 64, (e_ + 1) * 64
                        j = kt - (T - 4)
                        if j < 4:
                            reg, regB = sbk[e_][0][:, j * 128:(j + 1) * 128], sbk[e_][1]
                            pcol = e_ * 512 + j * 128
                        elif len(kts) == 1 and e_ == 1:
                            reg, regB = sbk[1][0][:, 0:128], sbk[1][1]
                            pcol = 1024 + e_ * 128
                        else:
                            reg, regB = sbk[2][0][:, e_ * 128:(e_ + 1) * 128], sbk[2][1]
                            pcol = 1024 + e_ * 128
                        kslot = (kt // 4) % 2
                        k0 = kslot * G + (kt % 4) * 128
                        delta = T - kt
                        stops.append((lambda e, reg=reg, lo=lo, hi=hi, k0=k0, hp=hp, qt=qt: e.matmul(reg, lhsT=kT[lo:hi, hp, k0:k0 + 128], rhs=qT[lo:hi, hp, qt * 128:(qt + 1) * 128], start=True, stop=True),
                                      [kTB[kslot], qTB], [regB]))
                        if delta in (0, 1, 4):
                            fac = bt[:, hh, 0, :] if delta == 0 else (bt[:, hh, 1, :] if delta == 1 else m4[:])
                            fixes.append((pcol, fac))
                for si, (fn_, rd_, wr_) in enumerate(stops):
                    S.op("pe", fn_, reads=rd_, writes=wr_, inc=(si == len(stops) - 1))
                c0 = jmin * 128
                if jmin < 4:
                    for e_ in range(2):
                        S.op("act", lambda e, e_=e_, c0=c0, pt=pt, sbk=sbk: e.activation(out=pt[:, e_ * 512 + c0:(e_ + 1) * 512], in_=sbk[e_][0][:, c0:512], func=AF.Exp),
                             reads=[sbk[e_][1]], writes=[ptB])
                if len(kts) == 1:
                    S.op("act", lambda e, pt=pt, sbk=sbk: e.activation(out=pt[:, 1024:1152], in_=sbk[2][0][:, 0:128], func=AF.Exp),
                         reads=[sbk[2][1]], writes=[ptB])
                    S.op("act", lambda e, pt=pt, sbk=sbk: e.activation(out=pt[:, 1152:1280], in_=sbk[1][0][:, 0:128], func=AF.Exp),
                         reads=[sbk[1][1]], writes=[ptB])
                else:
                    S.op("act", lambda e, pt=pt, sbk=sbk: e.activation(out=pt[:, 1024:1280], in_=sbk[2][0][:, 0:256], func=AF.Exp),
                         reads=[sbk[2][1]], writes=[ptB])
                for (pcol, fac) in fixes:
                    S.op("dve", lambda e, pt=pt, pcol=pcol, fac=fac: e.tensor_tensor(out=pt[:, pcol:pcol + 128], in0=pt[:, pcol:pcol + 128], in1=fac, op=ALU.mult),
                         reads=[ptB, btB, m4B], writes=[ptB])
                for e_ in range(2):
                    hh = 2 * hp + e_
                    ob, obB = psum[5 + hh // 6], psumB[5 + hh // 6]
                    col = (hh % 6) * 65
                    for idx, kt in enumerate(kts):
                        j = kt - (T - 4)
                        pc = pt[:, e_ * 512 + j * 128:e_ * 512 + (j + 1) * 128] if j < 4 else pt[:, 1024 + e_ * 128:1024 + (e_ + 1) * 128]
                        last = (idx == len(kts) - 1)
                        S.op("pe", lambda e, pc=pc, kt=kt, hh=hh, idx=idx, last=last, ob=ob, col=col: e.matmul(ob[:, col:col + 65], lhsT=pc, rhs=Vr[:, kt % 8, hh, :], start=(idx == 0), stop=last),
                             reads=[ptB, VB[(kt // 4) % 2]], writes=[obB], inc=(last and e_ == 1))
            for b in range(3):
                nh = 6 if b < 2 else 4
                ob, obB = psum[5 + b], psumB[5 + b]
                view = ob[:, 0:nh * 65].rearrange("p (h c) -> p h c", c=65)
                S.op("dve", lambda e, view=view, nh=nh: e.reciprocal(out=rden[:, 0:nh], in_=view[:, :, 64]), reads=[obB], writes=[rdenB])
                S.op("dve", lambda e, view=view, nh=nh, b=b, qt=qt: e.tensor_tensor(out=otok[:, qt, b * 384:b * 384 + nh * 64].rearrange("p (h d) -> p h d", d=64),
                                                                            in0=view[:, :, 0:64], in1=rden[:, 0:nh].unsqueeze(2).broadcast_to([128, nh, 64]), op=ALU.mult),
                     reads=[obB, rdenB], writes=[otokB])
            otok_to_yT(qt)
        k.bank_n = 8
        out_proj_post(L, hb, hbB, "nwR%d_1" % L)

    def mm_group(p, pB, lhs_fn, rhs_fn, reads, n=KC):
        for kc in range(n):
            S.op("pe", lambda e, kc=kc: e.matmul(p, lhsT=lhs_fn(kc), rhs=rhs_fn(kc), start=(kc == 0), stop=(kc == n - 1)),
                 reads=reads, writes=[pB], inc=(kc == n - 1))

    def rope_chunk(wv, wB, c, dst, dstB):
        pa, paB = bank()
        mm_group(pa[:], paB, lambda kc: wv[:, kc, c * 128:(c + 1) * 128], lambda kc: yT[:, kc, :], [wB] + yTB)
        pr, prB = bank()
        mm_group(pr[:], prB, lambda kc: wv[:, kc, (c + 1) * 128:(c + 2) * 128], lambda kc: yT[:, kc, :], [wB] + yTB)
        t1, t1B = csb[0], csbB[0]
        t2, t2B = csb[1], csbB[1]
        S.op("dve", lambda e: e.tensor_tensor(out=t1[:], in0=pa[:], in1=ropeT[:, 0, :], op=ALU.mult), reads=[paB, ropeB], writes=[t1B])
        S.op("dve", lambda e: e.tensor_tensor(out=t2[:], in0=pr[:], in1=ropeT[:, 1, :], op=ALU.mult), reads=[prB, ropeB], writes=[t2B])
        S.op("pool", lambda e: e.tensor_tensor(out=dst, in0=t1[:], in1=t2[:], op=ALU.add), reads=[t1B, t2B], writes=[dstB])

    def hgrn_head(hd, wvq, wBq, cq, wvf, wBf, cf, g, first):
        i2 = hd % 2
        pq, pqB = bank()
        mm_group(pq[:], pqB, lambda kc: wvq[:, kc, cq * 128:(cq + 1) * 128], lambda kc: yT[:, kc, :], [wBq] + yTB)
        pf, pfB = bank()
        mm_group(pf[:], pfB, lambda kc: wvf[:, kc, cf * 128:(cf + 1) * 128], lambda kc: yT[:, kc, :], [wBf] + yTB)
        fa, faB = csb[0], csbB[0]
        lc, lcB = csb[1], csbB[1]
        e1, e1B = gsb[0], gsbB[0]
        e2, e2B = gsb[1], gsbB[1]
        hv, hvB = hvec[i2], hvecB[i2]
        S.op("act", lambda e: e.activation(out=fa[:], in_=pf[:], func=AF.Sigmoid), reads=[pfB], writes=[faB])
        S.op("dve", lambda e: e.tensor_scalar(out=fa[:], in0=fa[:], scalar1=lbv[:, 4 + hd:5 + hd], scalar2=lbv[:, hd:hd + 1], op0=ALU.mult, op1=ALU.add),
             reads=[faB, evB], writes=[faB])
        S.op("act", lambda e: e.activation(out=e1[:], in_=fa[:], func=AF.Ln), reads=[faB], writes=[e1B])
        for t in range(TPG):
            S.op("dve", lambda e, t=t: e.tensor_tensor_scan(out=lc[:, t * 128:(t + 1) * 128], data0=ones[:], data1=e1[:, t * 128:(t + 1) * 128], initial=0.0, op0=ALU.mult, op1=ALU.add),
                 reads=[e1B, evB], writes=[lcB])
        lc3 = lc[:].rearrange("p (t m) -> p t m", t=TPG)
        S.op("dve", lambda e: e.tensor_copy(out=hv[:, :, 0], in_=lc3[:, :, 63]), reads=[lcB], writes=[hvB])
        S.op("act", lambda e: e.activation(out=hv[:, :, 1], in_=lc3[:, :, 63], func=AF.Exp), reads=[lcB], writes=[hvB])
        S.op("act", lambda e: e.activation(out=hv[:, :, 2], in_=lc3[:, :, 127], func=AF.Exp), reads=[lcB], writes=[hvB])
        S.op("dve", lambda e: e.tensor_tensor(out=lc3, in0=lc3, in1=hv[:, :, 0:1].broadcast_to([128, TPG, 128]), op=ALU.subtract), reads=[lcB, hvB], writes=[lcB])
        S.op("act", lambda e: e.activation(out=hv[:, :, 3], in_=lc3[:, :, 127], func=AF.Exp), reads=[lcB], writes=[hvB])
        S.op("act", lambda e: e.activation(out=e1[:], in_=lc[:], func=AF.Exp), reads=[lcB], writes=[e1B])
        S.op("act", lambda e: e.activation(out=e2[:], in_=lc[:], func=AF.Exp, scale=-1.0), reads=[lcB], writes=[e2B])
        qd, qdB = qdT[i2], qdTB[i2]
        kd, kdB = kdT[i2], kdTB[i2]
        S.op("dve", lambda e: e.tensor_tensor(out=qd[:], in0=pq[:], in1=e1[:], op=ALU.mult), reads=[pqB, e1B], writes=[qdB])
        S.op("act", lambda e: e.activation(out=fa[:], in_=pf[:], func=AF.Sigmoid, scale=-1.0), reads=[pfB, faB], writes=[faB])
        S.op("dve", lambda e: e.scalar_tensor_tensor(out=kd[:], in0=fa[:], scalar=lbv[:, 4 + hd:5 + hd], op0=ALU.mult, in1=e2[:], op1=ALU.mult), reads=[faB, e2B, evB], writes=[kdB])
        kk, kkB = kdTok[i2], kdTokB[i2]
        pt_, ptB_ = bank()
        ptb = pt_[:].bitcast(BF16)
        for t in range(TPG):
            S.op("pe", lambda e, t=t: e.transpose(out=ptb[:, t * 128:(t + 1) * 128], in_=kd[:, t * 128:(t + 1) * 128], identity=ident[:]),
                 reads=[kdB, identB], writes=[ptB_], inc=(t == TPG - 1))
        evac(hd, kk[:].rearrange("p t d -> p (t d)"), ptb[:, 0:512], [ptB_], [kkB])

    def hgrn_tile(hd, t, g, first):
        i2 = hd % 2
        hv, hvB = hvec[i2], hvecB[i2]
        qd, qdB = qdT[i2], qdTB[i2]
        kd, kdB = kdT[i2], kdTB[i2]
        kk, kkB = kdTok[i2], kdTokB[i2]
        tile0 = first and t == 0
        sl = slice(t * 128, (t + 1) * 128)
        vv = iTok[:, t, hd * 128:(hd + 1) * 128]
        bi = i2 * 2 + (t % 2)
        ps_, psB_ = bank()
        S.op("pe", lambda e: e.matmul(ps_[:, 0:128], lhsT=kd[:, sl], rhs=qd[:, sl], start=True, stop=True), reads=[kdB, qdB], writes=[psB_])
        sm, smB = sTm[bi], sTmB[bi]
        S.op("dve", lambda e: e.tensor_tensor(out=sm[:], in0=ps_[:, 0:128], in1=mcb[:], op=ALU.mult), reads=[psB_, evB], writes=[smB])
        if not tile0:
            S.op("act", lambda e: e.activation(out=stS[:, hd, :], in_=state[:, hd, :], func=AF.Identity, scale=hv[:, t, 1:2]),
                 reads=[stateB[hd], hvB], writes=[stSB[hd]])
        po, poB = bank()
        S.op("pe", lambda e: e.matmul(po[:, 0:128], lhsT=sm[:], rhs=vv, start=True, stop=tile0), reads=[smB, iTokB], writes=[poB], inc=tile0)
        if not tile0:
            S.op("pe", lambda e: e.matmul(po[:, 0:128], lhsT=qd[:, sl], rhs=stS[:, hd, :], start=False, stop=True), reads=[qdB, stSB[hd]], writes=[poB])
        if not (g == NG - 1 and t == TPG - 1):
            pd, pdB = bank()
            S.op("pe", lambda e: e.matmul(pd[:, 0:128], lhsT=kk[:, t, :], rhs=vv, start=True, stop=True), reads=[kkB, iTokB], writes=[pdB])
            if tile0:
                S.op("dve", lambda e: e.tensor_scalar(out=state[:, hd, :], in0=pd[:, 0:128], scalar1=hv[:, t, 3:4], scalar2=None, op0=ALU.mult),
                     reads=[pdB, hvB], writes=[stateB[hd]])
            else:
                td_, tdB_ = tmpd[bi], tmpdB[bi]
                S.op("dve", lambda e: e.tensor_scalar(out=td_[:], in0=pd[:, 0:128], scalar1=hv[:, t, 3:4], scalar2=None, op0=ALU.mult),
                     reads=[pdB, hvB], writes=[tdB_])
                S.op("dve", lambda e: e.scalar_tensor_tensor(out=state[:, hd, :], in0=state[:, hd, :], scalar=hv[:, t, 2:3], op0=ALU.mult, in1=td_[:], op1=ALU.add),
                     reads=[tdB_, hvB, stateB[hd]], writes=[stateB[hd]])
        junk, junkB = next_junk()
        S.op("act", lambda e: e.activation(out=junk[:, 0:128], in_=po[:, 0:128], func=AF.Square, accum_out=ssh[:, t * 4 + hd:t * 4 + hd + 1]),
             reads=[poB], writes=[sshB[t * 4 + hd], junkB])
        S.op("dve", lambda e: e.tensor_copy(out=tmpo[:, t, hd * 128:(hd + 1) * 128], in_=po[:, 0:128]), reads=[poB], writes=[tmpoB[t]])

    def even_mixer(L, hb, hbB, g):
        first = (g == 0)
        alias_fence(mixBs, [gTB])
        r0 = g * G
        S.dma("sp", "rope", lambda e: e.dma_start(out=ropeT[:], in_=rope_d[:, :, r0:r0 + G]), writes=[ropeB])
        norm_transpose(hb, hbB, cs("nwT%d_0" % L))
        ks = g % 2
        def piece(i):
            wt, wB = w_get(L, "e%d" % i)
            n = len(EVEN_FM[i])
            return wt[:, 0:KC * 128 * n].rearrange("p (k c) -> p k c", k=KC), wB
        wt5, wB5 = w_get(L, "e5")
        w5 = wt5[:, 0:KC * 640].rearrange("p (k c) -> p k c", k=KC)
        for t in range(TPG):
            p, pB = bank()
            mm_group(p[:, 0:128], pB, lambda kc, t=t: yT[:, kc, t * 128:(t + 1) * 128], lambda kc: w5[:, kc, 0:128], [wB5, yTB[t]])
            evac(t, Va[:, ks * 4 + t, :, 0:64], p[:, 0:128].rearrange("p (h d) -> p h d", d=64), [pB], [VaB[ks]])
            p, pB = bank()
            mm_group(p[:], pB, lambda kc, t=t: yT[:, kc, t * 128:(t + 1) * 128], lambda kc: w5[:, kc, 128:640], [wB5, yTB[t]])
            evac(t + 1, iTok[:, t, :], p[:], [pB], [iTokB])
        wt6, wB6 = w_get(L, "e6")
        w6 = wt6[:, 0:KC * 512].rearrange("p (k c) -> p k c", k=KC)
        for t in range(TPG):
            p, pB = bank()
            mm_group(p[:], pB, lambda kc, t=t: yT[:, kc, t * 128:(t + 1) * 128], lambda kc: w6[:, kc, :], [wB6, yTB[t]])
            S.op("act", lambda e, p=p, t=t: e.activation(out=sg[:, t, :], in_=p[:], func=AF.Silu), reads=[pB], writes=[sgB])
            S.op("pool", lambda e, t=t: e.tensor_tensor(out=sg[:, t, :], in0=sg[:, t, :], in1=cs("gw"), op=ALU.mult), reads=[sgB, cstB], writes=[sgB])
        wv, wB = piece(0)
        rope_chunk(wv, wB, 0, kaT[:, ks * G:(ks + 1) * G], kaTB[ks])
        rope_chunk(wv, wB, 2, qaT[:, 0, :], qTB)
        wv, wB = piece(1)
        rope_chunk(wv, wB, 0, qaT[:, 1, :], qTB)
        rope_chunk(wv, wB, 2, qaT[:, 2, :], qTB)
        wv2, wB2 = piece(2)
        rope_chunk(wv2, wB2, 0, qaT[:, 3, :], qTB)
        k.bank_n = 6
        unit = 0
        for qt in range(TPG):
            T = g * 4 + qt
            kts = [kt for kt in (T - 1, T) if kt >= 0]
            for c in range(4):
                sb2 = [bank(), bank()]
                pt, ptB = PTa[unit % 2], PTB[unit % 2]
                unit += 1
                ops_ = []
                for e_ in range(2):
                    if e_ == 1:
                        ops_.append((lambda e, sb2=sb2: e.matmul(sb2[1][0][:, 256:258], lhsT=ident[:], rhs=ident[:, 0:2], start=True, stop=True),
                                     [identB], [sb2[1][1]]))
                    for kt in kts:
                        lo, hi = e_ * 64, (e_ + 1) * 64
                        j = kt - (T - 1)
                        reg = sb2[e_][0][:, j * 128:(j + 1) * 128]
                        kslot = (kt // 4) % 2
                        k0 = kslot * G + (kt % 4) * 128
                        ops_.append((lambda e, reg=reg, lo=lo, hi=hi, k0=k0, c=c, qt=qt: e.matmul(reg, lhsT=kaT[lo:hi, k0:k0 + 128], rhs=qaT[lo:hi, c, qt * 128:(qt + 1) * 128], start=True, stop=True),
                                     [kaTB[kslot], qTB], [sb2[e_][1]]))
                for si, (fn_, rd_, wr_) in enumerate(ops_):
                    S.op("pe", fn_, reads=rd_, writes=wr_, inc=(si == len(ops_) - 1))
                j0 = 2 - len(kts)
                for e_ in range(2):
                    S.op("act", lambda e, e_=e_, sb2=sb2, pt=pt, j0=j0: e.activation(out=pt[:, e_ * 256 + j0 * 128:(e_ + 1) * 256],
                                                                                    in_=sb2[e_][0][:, j0 * 128:256], func=AF.Exp, scale=0.125),
                         reads=[sb2[e_][1]], writes=[ptB])
                S.op("dve", lambda e, pt=pt, j0=j0: e.tensor_tensor(out=pt.rearrange("p (e j q) -> p e j q", e=2, j=2)[:, :, j0:2, :],
                                                                    in0=pt.rearrange("p (e j q) -> p e j q", e=2, j=2)[:, :, j0:2, :], in1=mfull[:, :, j0:2, :], op=ALU.mult),
                     reads=[ptB, evB], writes=[ptB])
                for e_ in range(2):
                    hh = 4 * e_ + c
                    ob, obB = psum[6 + e_], psumB[6 + e_]
                    col = c * 65
                    for idx, kt in enumerate(kts):
                        j = kt - (T - 1)
                        pc = pt[:, e_ * 256 + j * 128:e_ * 256 + (j + 1) * 128]
                        last = (idx == len(kts) - 1)
                        S.op("pe", lambda e, pc=pc, kt=kt, e_=e_, idx=idx, last=last, ob=ob, col=col: e.matmul(ob[:, col:col + 65], lhsT=pc, rhs=Va[:, kt % 8, e_, :], start=(idx == 0), stop=last),
                             reads=[ptB, VaB[(kt // 4) % 2]], writes=[obB], inc=(last and e_ == 1))
            for b in range(2):
                ob, obB = psum[6 + b], psumB[6 + b]
                view = ob[:, 0:260].rearrange("p (h c) -> p h c", c=65)
                S.op("dve", lambda e, view=view, b=b: e.tensor_tensor(out=rden[:, 0:4], in0=view[:, :, 64], in1=esink[:, 4 * b:4 * b + 4], op=ALU.add), reads=[obB, evB], writes=[rdenB])
                S.op("dve", lambda e: e.reciprocal(out=rden[:, 0:4], in_=rden[:, 0:4]), reads=[rdenB], writes=[rdenB])
                S.op("dve", lambda e, view=view, b=b, qt=qt: e.tensor_tensor(out=otok[:, qt, b * 256:(b + 1) * 256].rearrange("p (h d) -> p h d", d=64),
                                                                            in0=view[:, :, 0:64], in1=rden[:, 0:4].unsqueeze(2).broadcast_to([128, 4, 64]), op=ALU.mult),
                     reads=[obB, rdenB], writes=[otokB])
        k.bank_n = 8
        hgrn_head(0, wv2, wB2, 2, wv2, wB2, 3, g, first)
        wv3, wB3 = piece(3)
        hgrn_head(1, wv3, wB3, 0, wv3, wB3, 1, g, first)
        for t in range(TPG):
            hgrn_tile(0, t, g, first)
            hgrn_tile(1, t, g, first)
        hgrn_head(2, wv3, wB3, 2, wv3, wB3, 3, g, first)
        wv4, wB4 = piece(4)
        hgrn_head(3, wv4, wB4, 0, wv4, wB4, 1, g, first)
        for t in range(TPG):
            hgrn_tile(2, t, g, first)
            hgrn_tile(3, t, g, first)
        S.op("act", lambda e: e.activation(out=rsh[:], in_=ssh[:], func=AF.Sqrt, scale=1.0 / 128, bias=EPS), reads=sshB, writes=[rshB])
        S.op("dve", lambda e: e.reciprocal(out=rsh[:], in_=rsh[:]), reads=[rshB], writes=[rshB])
        for t in range(TPG):
            o3 = tmpo[:, t, 0:512].rearrange("p (h d) -> p h d", h=4)
            S.op("dve", lambda e, t=t, o3=o3: e.tensor_tensor(out=o3, in0=o3, in1=rsh[:, t * 4:(t + 1) * 4].unsqueeze(2).broadcast_to([128, 4, 128]), op=ALU.mult),
                 reads=[tmpoB[t], rshB], writes=[tmpoB[t]])
            S.op("dve", lambda e, t=t: e.tensor_tensor(out=otok[:, t, 512:1024], in0=tmpo[:, t, 0:512], in1=sg[:, t, :], op=ALU.mult), reads=[tmpoB[t], sgB], writes=[otokB])
        for qt in range(TPG):
            otok_to_yT(qt)
        out_proj_post(L, hb, hbB, "nwR%d_1" % L)

    def ffn_block(L, hb, hbB, first, last):
        import os
        stage = int(os.environ.get("FFN_STAGE", "9"))
        alias_fence([gTB], mixBs)
        norm_transpose(hb, hbB, cs("nwT%d_2" % L))
        if stage < 2:
            return
        cw = cs("cw%d" % L).rearrange("p (f j) -> p f j", j=3)
        cb = cs("cb%d" % L)
        cidx = 0
        for j in range(11):
            wt, wB = w_get(L, "up%d" % j)
            wv = wt[:, 0:4096].rearrange("p (k c) -> p k c", k=KC)
            for i in range(2):
                fc = 2 * j + i
                pu, puB = bank()
                for kc in range(KC):
                    S.op("pe", lambda e, kc=kc, i=i, pu=pu, wv=wv: e.matmul(pu[:], lhsT=wv[:, kc, i * 128:(i + 1) * 128], rhs=yT[:, kc, :], start=(kc == 0), stop=(kc == KC - 1)),
                         reads=[wB] + yTB, writes=[puB], inc=(kc == KC - 1))
                pv, pvB = bank()
                for kc in range(KC):
                    S.op("pe", lambda e, kc=kc, i=i, pv=pv, wv=wv: e.matmul(pv[:], lhsT=wv[:, kc, 256 + i * 128:256 + (i + 1) * 128], rhs=yT[:, kc, :], start=(kc == 0), stop=(kc == KC - 1)),
                         reads=[wB] + yTB, writes=[pvB], inc=(kc == KC - 1))
                c, cB = csb[cidx % 2], csbB[cidx % 2]
                gg, gB = gsb[cidx % 2], gsbB[cidx % 2]
                cidx += 1
                if stage < 3:
                    continue
                S.op("act", lambda e, fc=fc, c=c, pu=pu: e.activation(out=c[:], in_=pu[:], func=AF.Identity, scale=cw[:, fc, 2:3], bias=cb[:, fc:fc + 1]),
                     reads=[puB, cstB], writes=[cB])
                S.op("dve", lambda e, fc=fc, c=c, pu=pu: e.scalar_tensor_tensor(out=c[:, 1:G], in0=pu[:, 0:G - 1], scalar=cw[:, fc, 1:2], op0=ALU.mult, in1=c[:, 1:G], op1=ALU.add),
                     reads=[puB, cstB, cB], writes=[cB])
                S.op("dve", lambda e, fc=fc, c=c, pu=pu: e.scalar_tensor_tensor(out=c[:, 2:G], in0=pu[:, 0:G - 2], scalar=cw[:, fc, 0:1], op0=ALU.mult, in1=c[:, 2:G], op1=ALU.add),
                     reads=[puB, cstB, cB], writes=[cB])
                if stage < 4:
                    continue
                if not first:
                    S.op("dve", lambda e, fc=fc, c=c: e.scalar_tensor_tensor(out=c[:, 0:1], in0=carry[L][:, fc, 1:2], scalar=cw[:, fc, 1:2], op0=ALU.mult, in1=c[:, 0:1], op1=ALU.add),
                         reads=[carryB[L], cstB, cB], writes=[cB])
                    S.op("dve", lambda e, fc=fc, c=c: e.scalar_tensor_tensor(out=c[:, 0:2], in0=carry[L][:, fc, 0:2], scalar=cw[:, fc, 0:1], op0=ALU.mult, in1=c[:, 0:2], op1=ALU.add),
                         reads=[carryB[L], cstB, cB], writes=[cB])
                if not last:
                    S.op("dve", lambda e, fc=fc, pu=pu: e.tensor_copy(out=carry[L][:, fc, :], in_=pu[:, G - 2:G]),
                         reads=[puB], writes=[carryB[L]])
                S.op("act", lambda e, c=c, gg=gg: e.activation(out=gg[:], in_=c[:], func=AF.Gelu), reads=[cB], writes=[gB])
                S.op("dve", lambda e, fc=fc, gg=gg, pv=pv: e.tensor_tensor(out=gT[:, fc, :], in0=pv[:], in1=gg[:], op=ALU.mult),
                     reads=[pvB, gB], writes=[gTB])

        if stage < 5:
            for _ in range(4):
                w_get(L, order[k.w_use][1])
            return

        def produce(cg):
            banks = [bank() for _ in range(TPG)]
            for hf in range(2):
                wt, wB = w_get(L, "dn%d_%d" % (cg, hf))
                wv = wt[:, 0:5632].rearrange("p (f c) -> p f c", f=11)
                for t in range(TPG):
                    p, pB = banks[t]
                    for f in range(11):
                        fc = hf * 11 + f
                        S.op("pe", lambda e, t=t, f=f, fc=fc, p=p, wv=wv: e.matmul(p[:], lhsT=gT[:, fc, t * 128:(t + 1) * 128], rhs=wv[:, f, :], start=(fc == 0), stop=(fc == FC - 1)),
                             reads=[wB, gTB], writes=[pB], inc=(f == 10))
            return banks
        post_norm_residual(hb, hbB, "nwR%d_3" % L, produce)

    k.ffn_block = ffn_block
    k.extra = {}

    gi = 0
    groups = [(s_, g_) for s_ in range(nseq) for g_ in range(NG)]

    def load_tile(gidx, t):
        s_, g_ = groups[gidx]
        r0 = s_ * seq + g_ * G + t * 128
        hb_, hbB_ = h[gidx % NH], hB[gidx % NH]
        S.dma("pool", "ldx%d_%d" % (gidx % NH, t), lambda e: e.dma_start(out=hb_[:, t, :], in_=x_d[r0:r0 + 128, :]), writes=[hbB_[t]])

    def store_tile(gidx, t):
        s_, g_ = groups[gidx]
        r0 = s_ * seq + g_ * G + t * 128
        hb_, hbB_ = h[gidx % NH], hB[gidx % NH]
        S.dma("pool", "stx%d_%d" % (gidx % NH, t), lambda e: e.dma_start(out=out_d[r0:r0 + 128, :], in_=hb_[:, t, :]), reads=[hbB_[t]])

    for t in range(TPG):
        load_tile(0, t)
    for gi, (s, g) in enumerate(groups):
        hb, hbB = h[gi % NH], hB[gi % NH]
        for L in layers:
            if "mixer" in parts:
                if L == 1:
                    odd_mixer(L, hb, hbB, g)
                else:
                    even_mixer(L, hb, hbB, g)
            if "ffn" in parts:
                ffn_block(L, hb, hbB, first=(g == 0), last=(g == NG - 1))
        for t in range(TPG):
            store_tile(gi, t)
            if gi + 1 < len(groups):
                load_tile(gi + 1, t)
    S.wait_all("pool", [b for hl in hB for b in hl])
    S.emit()
    return nc


def host_prep(inp):
    return {"wsrc": weights_host(inp).reshape(-1, 2048), "cst": consts_host(inp), "relb": relb_host(inp),
            "rope": rope_host(int(inp["x"].shape[1]))}


_NC_CACHE = {}


def kernel(**inputs):
    inp = {k_: np.asarray(v) for k_, v in inputs.items()}
    x = inp["x"]
    B, Sq, _ = x.shape
    ncores = 8
    nseq = B // ncores
    key = (nseq, Sq)
    if key not in _NC_CACHE:
        _NC_CACHE[key] = build(nseq, Sq)
    nc = _NC_CACHE[key]
    host = host_prep(inp)
    in_maps = []
    for c in range(ncores):
        m = dict(host)
        m["x"] = np.ascontiguousarray(x[c * nseq:(c + 1) * nseq].reshape(nseq * Sq, D))
        in_maps.append(m)
    res = run_bass_kernel_spmd(nc, in_maps, core_ids=list(range(ncores)))
    out = np.stack([np.asarray(r["out"]).reshape(nseq, Sq, D) for r in res.results])
    return out.reshape(B, Sq, D).astype(np.float32)
```

```python
import numpy as np
import concourse.bass as bass
import concourse.mybir as mybir
from concourse.bass_utils import run_bass_kernel_spmd

F32 = mybir.dt.float32
BF16 = mybir.dt.bfloat16
AF = mybir.ActivationFunctionType
ALU = mybir.AluOpType
AX = mybir.AxisListType

ENG_ATTR = {"pe": "tensor", "act": "scalar", "dve": "vector", "pool": "gpsimd", "sp": "sync"}


class Buf:
    __slots__ = ("name", "w", "r", "excl")

    def __init__(self, name, excl=False):
        self.name = name
        self.w = None
        self.r = []
        self.excl = excl


class Sched:
    def __init__(self, nc, same_engine_sync=True):
        self.nc = nc
        self.same = same_engine_sync
        self.ops = {k: [] for k in ENG_ATTR}
        self.cnt = {k: 0 for k in ENG_ATTR}
        self.seen = {k: {} for k in ENG_ATTR}
        self.sems = {}
        self.dma_total = {}
        for k in ENG_ATTR:
            self.sems[k] = nc.alloc_semaphore("s_" + k)

    def dma_sem(self, key):
        if key not in self.sems:
            self.sems[key] = self.nc.alloc_semaphore("d_" + key)
            self.dma_total[key] = 0
        return key

    def _deps(self, eng, reads, writes):
        deps = {}
        for b in reads:
            if b.w is not None:
                k, v = b.w
                deps[k] = max(deps.get(k, 0), v)
            if b.excl:
                for (k, v) in b.r:
                    if k != eng:
                        deps[k] = max(deps.get(k, 0), v)
        for b in writes:
            if b.w is not None:
                k, v = b.w
                deps[k] = max(deps.get(k, 0), v)
            for (k, v) in b.r:
                deps[k] = max(deps.get(k, 0), v)
        seen = self.seen[eng]
        for k, v in deps.items():
            if k == eng and (eng == "pe" or not self.same):
                continue
            if seen.get(k, 0) < v:
                seen[k] = v
                sem = self.sems[k]
                self.ops[eng].append(lambda e, sem=sem, v=v: e.wait_ge(sem, v))

    def op(self, eng, fn, reads=(), writes=(), inc=True):
        self._deps(eng, reads, writes)
        if inc:
            self.cnt[eng] += 1
            tok = (eng, self.cnt[eng])
            sem = self.sems[eng]
            self.ops[eng].append(lambda e, fn=fn, sem=sem: fn(e).then_inc(sem, 1))
        else:
            tok = (eng, self.cnt[eng] + 1)
            self.ops[eng].append(lambda e, fn=fn: fn(e))
        for b in reads:
            b.r.append(tok)
        for b in writes:
            b.w = tok
            b.r = []
        return tok

    def dma(self, eng, semkey, fn, reads=(), writes=()):
        self.dma_sem(semkey)
        self._deps(eng, reads, writes)
        self.dma_total[semkey] += 16
        tok = (semkey, self.dma_total[semkey])
        sem = self.sems[semkey]
        self.ops[eng].append(lambda e, fn=fn, sem=sem: fn(e).then_inc(sem, 16))
        for b in reads:
            b.r.append(tok)
        for b in writes:
            b.w = tok
            b.r = []
        return tok

    def wait_all(self, eng, bufs):
        self._deps(eng, (), bufs)

    def emit(self):
        nc = self.nc
        with nc.Block() as block:
            for k, attr in ENG_ATTR.items():
                ops = self.ops[k]
                if not ops:
                    continue

                def body(e, ops=ops):
                    for f in ops:
                        f(e)
                getattr(block, attr)(body)


D = 1024
KC = 8
DFF = 2816
FC = 22
G = 512
TPG = 4
EPS = 1e-6
SLOT = 5632
NSLOT = 3
NEG = -30000.0


def ffn_piece_list():
    out = [("up%d" % j, 4096) for j in range(11)]
    out += [("dn%d_%d" % (cg, hf), 5632) for cg in range(2) for hf in range(2)]
    return out


def ffn_pieces_host(w_up, w_down):
    res = []
    wu = w_up.reshape(KC, 128, 2 * DFF)
    for j in range(11):
        cols = np.concatenate([np.arange(2 * j * 128, (2 * j + 2) * 128),
                               DFF + np.arange(2 * j * 128, (2 * j + 2) * 128)])
        pc = wu[:, :, cols].transpose(1, 0, 2).reshape(128, KC * 512)
        res.append(("up%d" % j, pc))
    wd = w_down.reshape(FC, 128, D)
    for cg in range(2):
        for hf in range(2):
            pc = wd[hf * 11:(hf + 1) * 11, :, cg * 512:(cg + 1) * 512].transpose(1, 0, 2).reshape(128, 11 * 512)
            res.append(("dn%d_%d" % (cg, hf), pc))
    return res


def const_layout():
    lay = {}
    off = 0

    def add(name, w):
        nonlocal off
        lay[name] = (off, w)
        off += w
    for L in range(2):
        add("nwT%d_0" % L, KC)
        add("nwT%d_2" % L, KC)
        add("nwR%d_1" % L, D)
        add("nwR%d_3" % L, D)
        add("cw%d" % L, FC * 3)
        add("cb%d" % L, FC)
    add("ident", 128)
    add("lbl", 8)
    add("sinks", 8)
    add("gw", 512)
    add("mc", 128)
    add("crep", 16)
    add("m0", 128)
    add("m4", 128)
    lay["_total"] = (off, 0)
    return lay


def consts_host(inp):
    lay = const_layout()
    c = np.zeros((128, lay["_total"][0]), np.float32)

    def put(name, arr):
        o, w = lay[name]
        c[:, o:o + w] = arr.reshape(128, w) if arr.shape[0] == 128 else np.broadcast_to(arr.reshape(1, w), (128, w))
    nw = inp["norm_w"]
    for L in range(2):
        put("nwT%d_0" % L, nw[L, 0].reshape(KC, 128).T)
        put("nwT%d_2" % L, nw[L, 2].reshape(KC, 128).T)
        put("nwR%d_1" % L, nw[L, 1])
        put("nwR%d_3" % L, nw[L, 3])
        cw = inp["ffn_conv_w"][L]
        put("cw%d" % L, cw.reshape(3, FC, 128).transpose(2, 1, 0).reshape(128, FC * 3))
        put("cb%d" % L, inp["ffn_conv_b"][L].reshape(FC, 128).T)
    put("ident", np.eye(128, dtype=np.float32))
    put("lbl", inp["hgrn_lb_logits"][0:2].reshape(2, 4, 128).transpose(2, 0, 1).reshape(128, 8))
    put("sinks", inp["even_sinks"][0])
    put("gw", inp["hgrn_norm_w"][0])
    put("mc", (np.arange(128)[:, None] <= np.arange(128)[None, :]).astype(np.float32))
    put("crep", inp["odd_rel_bias"][0][:, 256])
    jj = np.arange(128)[:, None]
    qq = np.arange(128)[None, :]
    put("m0", np.where((jj >= 64) & (qq < 64), NEG, 0.0).astype(np.float32))
    put("m4", np.where((jj < 64) & (qq >= 64), NEG, 0.0).astype(np.float32))
    return c


def relb_host(inp):
    tab = inp["odd_rel_bias"][0]
    jj = np.arange(128)[:, None]
    qq = np.arange(128)[None, :]
    i0 = (qq - jj) + 128
    i1 = np.minimum(128 + qq - jj, 128) + 128
    b0 = tab[:, i0]
    b1 = tab[:, i1]
    r = np.stack([b0, b1], axis=1)
    return np.ascontiguousarray(r.transpose(2, 0, 1, 3).reshape(128, 16 * 2 * 128).astype(np.float32))


def odd_piece_list():
    return [(n, 4096) for n in ("ik0", "ik1", "iv0", "iv1", "iq0", "iq1", "ow0", "ow1")]


def odd_pieces_host(w_in, w_out):
    wi = w_in.reshape(KC, 128, 3 * D)
    wo = w_out.reshape(KC, 128, D)
    res = []

    def pc(w, c0):
        return w[:, :, c0:c0 + 512].transpose(1, 0, 2).reshape(128, KC * 512)
    for i in range(2):
        res.append(("ik%d" % i, pc(wi, D + i * 512)))
    for i in range(2):
        res.append(("iv%d" % i, pc(wi, 2 * D + i * 512)))
    for i in range(2):
        res.append(("iq%d" % i, pc(wi, i * 512)))
    for i in range(2):
        res.append(("ow%d" % i, pc(wo, i * 512)))
    return res


def layer_piece_list(L):
    if L == 1:
        return odd_piece_list() + ffn_piece_list()
    return even_piece_list() + ffn_piece_list()


EVEN_FM = [["ka", "rka", "qa0", "rqa0"], ["qa1", "rqa1", "qa2", "rqa2"], ["qa3", "rqa3", "qb0", "fb0"],
           ["qb1", "fb1", "qb2", "fb2"], ["qb3", "fb3"]]


def even_piece_list():
    out = [("e5", KC * 640), ("e6", KC * 512)]
    out += [("e%d" % i, KC * 128 * len(ch)) for i, ch in enumerate(EVEN_FM)]
    out += [("ow0", 4096), ("ow1", 4096)]
    return out


def even_cols():
    rot = (np.arange(64) + 32) % 64
    cols = {}
    cols["ka"] = 512 + np.arange(128)
    cols["rka"] = 512 + np.concatenate([rot, 64 + rot])
    for c in range(4):
        h0, h1 = c, 4 + c
        cols["qa%d" % c] = np.concatenate([h0 * 64 + np.arange(64), h1 * 64 + np.arange(64)])
        cols["rqa%d" % c] = np.concatenate([h0 * 64 + rot, h1 * 64 + rot])
        cols["qb%d" % c] = 768 + c * 128 + np.arange(128)
        cols["fb%d" % c] = 1280 + c * 128 + np.arange(128)
    return cols


def even_pieces_host(inp):
    wi = inp["even_w_in"][0].reshape(KC, 128, 2816)
    wo = inp["even_w_out"][0].reshape(KC, 128, D)
    cols = even_cols()
    res = []
    cc = np.concatenate([640 + np.arange(128), 1792 + np.arange(512)])
    res.append(("e5", wi[:, :, cc].transpose(1, 0, 2).reshape(128, KC * 640)))
    cc = 2304 + np.arange(512)
    res.append(("e6", wi[:, :, cc].transpose(1, 0, 2).reshape(128, KC * 512)))
    for i, ch in enumerate(EVEN_FM):
        cc = np.concatenate([cols[n] for n in ch])
        res.append(("e%d" % i, wi[:, :, cc].transpose(1, 0, 2).reshape(128, KC * len(cc))))
    for i in range(2):
        res.append(("ow%d" % i, wo[:, :, i * 512:(i + 1) * 512].transpose(1, 0, 2).reshape(128, KC * 512)))
    return res


def rope_host(seq):
    inv = (10000.0 ** (-np.arange(0, 64, 2, dtype=np.float32) / 64)).astype(np.float32)
    ang = np.arange(seq, dtype=np.float32)[None, :] * inv[:, None]
    cos = np.cos(ang).astype(np.float32)
    sin = np.sin(ang).astype(np.float32)
    c64 = np.concatenate([cos, cos], 0)
    s64 = np.concatenate([-sin, sin], 0)
    t = np.stack([np.concatenate([c64, c64], 0), np.concatenate([s64, s64], 0)], axis=1)
    return np.ascontiguousarray(t.astype(np.float32))


def weights_host(inp):
    chunks = []
    for L in range(2):
        pcs = (odd_pieces_host(inp["odd_w_in"][0], inp["odd_w_out"][0]) if L == 1 else even_pieces_host(inp))
        pcs = pcs + ffn_pieces_host(inp["ffn_w_up"][L], inp["ffn_w_down"][L])
        want = layer_piece_list(L)
        assert [p[0] for p in pcs] == [w[0] for w in want]
        for (n, a), (_, nco) in zip(pcs, want):
            assert a.shape == (128, nco), (n, a.shape, nco)
            chunks.append(np.ascontiguousarray(a, dtype=np.float32).reshape(-1))
    return np.concatenate(chunks)


class K:
    pass


def build(nseq, seq, layers=(0, 1), parts=("mixer", "ffn")):
    nc = bass.Bass("TRN2", target_bir_lowering=False)
    NG = seq // G
    NTOK = nseq * seq
    lay = const_layout()
    NCST = lay["_total"][0]
    ptab = {}
    off = 0
    for L in range(2):
        for (n, nco) in layer_piece_list(L):
            ptab[(L, n)] = (off, nco)
            off += 128 * nco
    WTOT = off

    x_d = nc.dram_tensor("x", [NTOK, D], F32, kind="ExternalInput").ap()
    out_d = nc.dram_tensor("out", [NTOK, D], F32, kind="ExternalOutput").ap()
    wsrc_d = nc.dram_tensor("wsrc", [WTOT // 2048, 2048], F32, kind="ExternalInput").ap()
    cst_d = nc.dram_tensor("cst", [128, NCST], F32, kind="ExternalInput").ap()
    wbf_d = nc.dram_tensor("wbf", [WTOT // 2048, 2048], BF16, kind="Internal").ap()
    relb_d = nc.dram_tensor("relb", [128, 4096], F32, kind="ExternalInput").ap()
    rope_d = nc.dram_tensor("rope", [128, 2, seq], F32, kind="ExternalInput").ap()

    S = Sched(nc)
    k = K()
    k.nc, k.S = nc, S

    def sb(name, shape, dt):
        return nc.alloc_sbuf_tensor("s_" + name, shape, dt)

    cst = sb("cst", [128, NCST], F32)
    cstB = Buf("cst")
    ident = sb("ident", [128, 128], BF16)
    identB = Buf("ident")
    NH = 1
    h = [sb("h%d" % i, [128, TPG, D], F32) for i in range(NH)]
    hB = [[Buf("h%d_%d" % (i, t)) for t in range(TPG)] for i in range(NH)]
    ring = [sb("ring%d" % i, [128, SLOT], BF16) for i in range(NSLOT)]
    ringB = [Buf("ring%d" % i) for i in range(NSLOT)]
    yn = [sb("yn%d" % i, [128, D], BF16) for i in range(2)]
    ynB = [Buf("yn%d" % i) for i in range(2)]
    yT = sb("yT", [128, KC, G], BF16)
    yTB = [Buf("yT%d" % t) for t in range(TPG)]
    arena = sb("arena", [128, FC * G], BF16)
    gT = arena[:].rearrange("p (f t) -> p f t", f=FC)
    gTB = Buf("gT")
    qT = arena[:, 0:4096].rearrange("p (k t) -> p k t", k=KC)
    qTB = Buf("qT")
    otok = arena[:, 4096:8192].rearrange("p (t d) -> p t d", t=TPG)
    otokB = Buf("otok")
    PT = [arena[:, 8192 + i * 1280:8192 + (i + 1) * 1280] for i in range(2)]
    PTB = [Buf("PT%d" % i) for i in range(2)]
    PTB3 = [[Buf("PT%d_%d" % (i, j)) for j in range(3)] for i in range(2)]
    PTB3f = [b for l in PTB3 for b in l]
    mixBs = [qTB, otokB] + PTB + PTB3f

    def alias_fence(dsts, srcs):
        for d_ in dsts:
            for s_ in srcs:
                if s_.w is not None:
                    d_.r.append(s_.w)
                d_.r.extend(s_.r)
    kT = sb("kT", [128, KC, 2 * G], BF16)
    kTB = [Buf("kT%d" % i) for i in range(2)]
    Vr = sb("Vr", [128, 8, 16, 65], BF16)
    VB = [Buf("V%d" % i) for i in range(2)]
    bt = sb("bt", [128, 16, 2, 128], BF16)
    btB = Buf("bt")
    m4 = sb("m4", [128, 128], BF16)
    m4B = Buf("m4")
    rden = sb("rden", [128, 8], F32)
    rdenB = Buf("rden")
    qaT = arena[:, 0:2048].rearrange("p (k t) -> p k t", k=4)
    iTok = arena[:, 2048:4096].rearrange("p (t d) -> p t d", t=TPG)
    iTokB = Buf("iTok")
    PTa = [arena[:, 8192 + i * 512:8192 + (i + 1) * 512] for i in range(2)]
    sg = arena[:, 9216:11264].rearrange("p (t d) -> p t d", t=TPG)
    sgB = Buf("sg")
    mixBs.extend([iTokB, sgB])
    ropeT = sb("ropeT", [128, 2, G], F32)
    ropeB = Buf("rope")
    kaT = sb("kaT", [128, 2 * G], BF16)
    kaTB = [Buf("kaT%d" % i) for i in range(2)]
    Va = sb("Va", [128, 8, 2, 65], BF16)
    VaB = [Buf("Va%d" % i) for i in range(2)]
    esink = sb("esink", [128, 8], F32)
    lbv = sb("lbv", [128, 8], F32)
    evB = Buf("evconst")
    mcb = sb("mcb", [128, 128], BF16)
    m0b = sb("m0b", [128, 128], BF16)
    ones = sb("ones", [128, 128], F32)
    mfull = sb("mfull", [128, 2, 2, 128], BF16)
    qdT = [sb("qdT%d" % i, [128, G], BF16) for i in range(2)]
    qdTB = [Buf("qdT%d" % i) for i in range(2)]
    kdT = [sb("kdT%d" % i, [128, G], BF16) for i in range(2)]
    kdTB = [Buf("kdT%d" % i) for i in range(2)]
    kdTok = [sb("kdTok%d" % i, [128, TPG, 128], BF16) for i in range(2)]
    kdTokB = [Buf("kdTok%d" % i) for i in range(2)]
    sTm = [sb("sTm%d" % i, [128, 128], BF16) for i in range(4)]
    sTmB = [Buf("sTm%d" % i) for i in range(4)]
    state = sb("state", [128, 4, 128], F32)
    stS = sb("stS", [128, 4, 128], BF16)
    stateB = [Buf("state%d" % i) for i in range(4)]
    stSB = [Buf("stS%d" % i) for i in range(4)]
    tmpd = [sb("tmpd%d" % i, [128, 128], F32) for i in range(4)]
    tmpdB = [Buf("tmpd%d" % i) for i in range(4)]
    hvec = [sb("hvec%d" % i, [128, 4, 4], F32) for i in range(2)]
    hvecB = [Buf("hvec%d" % i) for i in range(2)]
    ssh = sb("ssh", [128, 16], F32)
    sshB = [Buf("ssh%d" % i) for i in range(16)]
    rsh = sb("rsh", [128, 16], F32)
    rshB = Buf("rsh")
    tmpo = sb("tmpo", [128, TPG, D], F32)
    tmpoB = [Buf("tmpo%d" % t) for t in range(TPG)]
    junks = [sb("junk%d" % i, [128, D], BF16) for i in range(2)]
    junkBs = [Buf("junk%d" % i) for i in range(2)]
    k.junk_i = 0

    def next_junk():
        k.junk_i += 1
        return junks[k.junk_i % 2], junkBs[k.junk_i % 2]
    csb = [sb("csb%d" % i, [128, G], F32) for i in range(2)]
    csbB = [Buf("csb%d" % i) for i in range(2)]
    gsb = [sb("gsb%d" % i, [128, G], F32) for i in range(2)]
    gsbB = [Buf("gsb%d" % i) for i in range(2)]
    carry = [sb("carry%d" % L, [128, FC, 2], F32) for L in range(2)]
    carryB = [Buf("carry%d" % L) for L in range(2)]
    ss4 = sb("ss4", [128, 4], F32)
    ss4B = [Buf("ss4_%d" % t) for t in range(TPG)]
    ssp = sb("ssp", [128, 4, 2], F32)
    sspB = [Buf("ssp%d" % t) for t in range(TPG)]
    rstd = sb("rstd", [128, 4], F32)
    rstdB = [Buf("rstd%d" % t) for t in range(TPG)]
    psum = [nc.alloc_psum_tensor("ps%d" % i, [128, 512], F32) for i in range(8)]
    psumB = [Buf("ps%d" % i, excl=True) for i in range(8)]
    k.bank_i = 0
    k.bank_n = 8

    def bank():
        i = k.bank_i % k.bank_n
        k.bank_i += 1
        return psum[i], psumB[i]

    def cs(name, a=None, b=None):
        o, w = lay[name]
        if a is None:
            return cst[:, o:o + w]
        return cst[:, o + a:o + b]

    S.dma("sp", "cst", lambda e: e.dma_start(out=cst[:], in_=cst_d), writes=[cstB])
    S.op("dve", lambda e: e.tensor_copy(out=ident[:], in_=cs("ident")), reads=[cstB], writes=[identB])
    if 1 in layers and "mixer" in parts:
        S.dma("sp", "relb", lambda e: e.dma_start(out=tmpo[:].rearrange("p t d -> p (t d)"), in_=relb_d), writes=tmpoB)
        S.op("pool", lambda e: e.memset(Vr[:], 1.0), writes=VB)
        S.op("act", lambda e: e.activation(out=m4[:], in_=cs("m4"), func=AF.Exp), reads=[cstB], writes=[m4B])
        rb = tmpo[:].rearrange("p t d -> p (t d)").rearrange("p (h k q) -> p h k q", h=16, k=2)
        for hh in range(16):
            S.op("dve", lambda e, hh=hh: e.scalar_tensor_tensor(out=rb[:, hh, 0, :], in0=rb[:, hh, 0, :], scalar=cs("crep", hh, hh + 1), op0=ALU.subtract, in1=cs("m0"), op1=ALU.add),
                 reads=tmpoB + [cstB], writes=tmpoB)
            S.op("dve", lambda e, hh=hh: e.tensor_scalar(out=rb[:, hh, 1, :], in0=rb[:, hh, 1, :], scalar1=cs("crep", hh, hh + 1), scalar2=None, op0=ALU.subtract),
                 reads=tmpoB + [cstB], writes=tmpoB)
            S.op("act", lambda e, hh=hh: e.activation(out=bt[:, hh, :, :], in_=rb[:, hh, :, :], func=AF.Exp), reads=tmpoB, writes=[btB])
    if 0 in layers and "mixer" in parts:
        S.op("pool", lambda e: e.memset(Va[:], 1.0), writes=VaB)
        S.op("pool", lambda e: e.memset(ones[:], 1.0), writes=[evB])
        S.op("dve", lambda e: e.tensor_copy(out=mcb[:], in_=cs("mc")), reads=[cstB], writes=[evB])
        if not (1 in layers and "mixer" in parts):
            S.op("act", lambda e: e.activation(out=m4[:], in_=cs("m4"), func=AF.Exp), reads=[cstB], writes=[m4B])
        S.op("act", lambda e: e.activation(out=m0b[:], in_=cs("m0"), func=AF.Exp), reads=[cstB], writes=[evB])
        for e_ in range(2):
            S.op("dve", lambda e, e_=e_: e.tensor_copy(out=mfull[:, e_, 0, :], in_=m4[:]), reads=[m4B], writes=[evB])
            S.op("dve", lambda e, e_=e_: e.tensor_copy(out=mfull[:, e_, 1, :], in_=m0b[:]), reads=[evB], writes=[evB])
        S.op("act", lambda e: e.activation(out=esink[:], in_=cs("sinks"), func=AF.Exp), reads=[cstB], writes=[evB])
        S.op("dve", lambda e: e.tensor_tensor(out=lbv[:, 4:8], in0=cs("lbl", 0, 4), in1=cs("lbl", 4, 8), op=ALU.subtract), reads=[cstB], writes=[evB])
        S.op("act", lambda e: e.activation(out=lbv[:, 0:4], in_=lbv[:, 4:8], func=AF.Sigmoid), reads=[evB], writes=[evB])
        S.op("dve", lambda e: e.tensor_scalar(out=lbv[:, 4:8], in0=lbv[:, 0:4], scalar1=-1.0, scalar2=1.0, op0=ALU.mult, op1=ALU.add), reads=[evB], writes=[evB])
    cvB = {}
    for L in layers:
        for (n, nco) in layer_piece_list(L):
            sec = n[:2]
            key = "cv%d%s" % (L, sec)
            cvB.setdefault(key, Buf(key))
            o, _ = ptab[(L, n)]
            r0, r1 = o // 2048, (o + 128 * nco) // 2048
            S.dma("pool", key, lambda e, r0=r0, r1=r1: e.dma_start(out=wbf_d[r0:r1, :], in_=wsrc_d[r0:r1, :]),
                  writes=[cvB[key]])

    order = []
    for s in range(nseq):
        for g in range(NG):
            for L in layers:
                for (n, nco) in layer_piece_list(L):
                    if (n[:2] in ("up", "dn")) and "ffn" not in parts:
                        continue
                    if (n[:2] not in ("up", "dn")) and "mixer" not in parts:
                        continue
                    order.append((L, n))
    k.w_issue = 0
    k.w_use = 0

    def w_issue_upto(i):
        while k.w_issue <= i and k.w_issue < len(order):
            j = k.w_issue
            L, n = order[j]
            o, nco = ptab[(L, n)]
            slot = j % NSLOT
            key = "cv%d%s" % (L, n[:2])
            src = wbf_d.rearrange("r c -> (r c)")[o:o + 128 * nco].rearrange("(p c) -> p c", p=128)
            S.dma("sp", "w%d" % slot, lambda e, slot=slot, nco=nco, src=src: e.dma_start(out=ring[slot][:, 0:nco], in_=src),
                  reads=[cvB[key]], writes=[ringB[slot]])
            k.w_issue += 1

    def w_get(L, n):
        i = k.w_use
        assert order[i] == (L, n), (order[i], L, n)
        w_issue_upto(i + NSLOT - 1)
        k.w_use += 1
        return ring[i % NSLOT], ringB[i % NSLOT]

    def rstd_tile(t):
        S.op("act", lambda e: e.activation(out=rstd[:, t:t + 1], in_=ss4[:, t:t + 1], func=AF.Sqrt, scale=1.0 / D, bias=EPS),
             reads=[ss4B[t]], writes=[rstdB[t]])
        S.op("dve", lambda e: e.reciprocal(out=rstd[:, t:t + 1], in_=rstd[:, t:t + 1]), reads=[rstdB[t]], writes=[rstdB[t]])

    def norm_tile(hb, hbB, nwT, t):
        junk, junkB = next_junk()
        S.op("act", lambda e: e.activation(out=junk[:], in_=hb[:, t, :], func=AF.Square, accum_out=ss4[:, t:t + 1]),
             reads=[hbB[t]], writes=[ss4B[t], junkB])
        rstd_tile(t)
        y, yB = yn[t % 2], ynB[t % 2]
        S.op("dve", lambda e: e.tensor_scalar(out=y[:], in0=hb[:, t, :], scalar1=rstd[:, t:t + 1], scalar2=None, op0=ALU.mult),
             reads=[hbB[t], rstdB[t]], writes=[yB])
        p, pB = bank()
        pb = p[:].bitcast(BF16)
        for kc in range(KC):
            S.op("pe", lambda e, kc=kc: e.transpose(out=pb[:, kc * 128:(kc + 1) * 128], in_=y[:, kc * 128:(kc + 1) * 128], identity=ident[:]),
                 reads=[yB, identB], writes=[pB], inc=(kc == KC - 1))
        S.op("dve", lambda e: e.tensor_tensor(out=yT[:, :, t * 128:(t + 1) * 128], in0=pb.rearrange("p (k t) -> p k t", k=KC),
                                              in1=nwT.unsqueeze(2).broadcast_to([128, KC, 128]), op=ALU.mult),
             reads=[pB, cstB], writes=[yTB[t]])

    def norm_transpose(hb, hbB, nwT):
        for t in range(TPG):
            norm_tile(hb, hbB, nwT, t)

    def post_tile(hb, hbB, t):
        S.op("dve", lambda e: e.tensor_tensor(out=ss4[:, t:t + 1], in0=ssp[:, t, 0:1], in1=ssp[:, t, 1:2], op=ALU.add), reads=[sspB[t]], writes=[ss4B[t]])
        rstd_tile(t)
        S.op("dve", lambda e: e.scalar_tensor_tensor(out=hb[:, t, :], in0=tmpo[:, t, :], scalar=rstd[:, t:t + 1], op0=ALU.mult, in1=hb[:, t, :], op1=ALU.add),
             reads=[tmpoB[t], rstdB[t], hbB[t]], writes=[hbB[t]])

    def post_norm_residual(hb, hbB, nwR_name, produce, stage=9):
        for cg in range(2):
            banks = produce(cg)
            for t in range(TPG):
                p, pB = banks[t]
                junk, junkB = next_junk()
                S.op("act", lambda e, t=t, cg=cg, p=p, junk=junk: e.activation(out=junk[:, 0:512], in_=p[:], func=AF.Square, accum_out=ssp[:, t, cg:cg + 1]),
                     reads=[pB], writes=[sspB[t], junkB])
                S.op("dve", lambda e, t=t, cg=cg, p=p: e.tensor_tensor(out=tmpo[:, t, cg * 512:(cg + 1) * 512], in0=p[:], in1=cs(nwR_name, cg * 512, (cg + 1) * 512), op=ALU.mult),
                     reads=[pB, cstB], writes=[tmpoB[t]])
                if cg == 1:
                    post_tile(hb, hbB, t)

    def evac(i, out, in_, reads, writes, scale=None):
        if i % 2 == 0:
            if scale is None:
                S.op("act", lambda e: e.activation(out=out, in_=in_, func=AF.Identity), reads=reads, writes=writes)
            else:
                S.op("act", lambda e: e.activation(out=out, in_=in_, func=AF.Identity, scale=scale), reads=reads, writes=writes)
        else:
            if scale is None:
                S.op("dve", lambda e: e.tensor_copy(out=out, in_=in_), reads=reads, writes=writes)
            else:
                S.op("dve", lambda e: e.tensor_scalar(out=out, in0=in_, scalar1=scale, scalar2=None, op0=ALU.mult), reads=reads, writes=writes)

    def proj_fm(wv, wB, c, dst, dstB, ei, scale=None):
        p, pB = bank()
        for kc in range(KC):
            S.op("pe", lambda e, kc=kc: e.matmul(p[:], lhsT=wv[:, kc, c * 128:(c + 1) * 128], rhs=yT[:, kc, :], start=(kc == 0), stop=(kc == KC - 1)),
                 reads=[wB] + yTB, writes=[pB], inc=(kc == KC - 1))
        evac(ei, dst, p[:], [pB], [dstB], scale)

    def otok_to_yT(qt):
        p, pB = bank()
        pb = p[:].bitcast(BF16)
        for kc in range(KC):
            S.op("pe", lambda e, kc=kc: e.transpose(out=pb[:, kc * 128:(kc + 1) * 128], in_=otok[:, qt, kc * 128:(kc + 1) * 128], identity=ident[:]),
                 reads=[otokB, identB], writes=[pB], inc=(kc == KC - 1))
        evac(qt, yT[:, :, qt * 128:(qt + 1) * 128], pb.rearrange("p (k t) -> p k t", k=KC), [pB], [yTB[qt]])

    def out_proj_post(L, hb, hbB, nwR_name):
        def produce(cg):
            wt, wB = w_get(L, "ow%d" % cg)
            wv = wt[:, 0:4096].rearrange("p (k c) -> p k c", k=KC)
            banks = [bank() for _ in range(TPG)]
            for t in range(TPG):
                p, pB = banks[t]
                for kc in range(KC):
                    S.op("pe", lambda e, t=t, kc=kc, p=p: e.matmul(p[:], lhsT=yT[:, kc, t * 128:(t + 1) * 128], rhs=wv[:, kc, :], start=(kc == 0), stop=(kc == KC - 1)),
                         reads=[wB, yTB[t]], writes=[pB], inc=(kc == KC - 1))
            return banks
        post_norm_residual(hb, hbB, nwR_name, produce)

    def odd_mixer(L, hb, hbB, g):
        alias_fence(mixBs, [gTB])
        norm_transpose(hb, hbB, cs("nwT%d_0" % L))
        ks = g % 2
        ei = 0
        for i in range(2):
            wt, wB = w_get(L, "ik%d" % i)
            wv = wt[:, 0:4096].rearrange("p (k c) -> p k c", k=KC)
            for c in range(4):
                proj_fm(wv, wB, c, kT[:, 4 * i + c, ks * G:(ks + 1) * G], kTB[ks], ei)
                ei += 1
        for i in range(2):
            wt, wB = w_get(L, "iv%d" % i)
            wv = wt[:, 0:4096].rearrange("p (k c) -> p k c", k=KC)
            for t in range(TPG):
                p, pB = bank()
                for kc in range(KC):
                    S.op("pe", lambda e, t=t, kc=kc, p=p, wv=wv: e.matmul(p[:], lhsT=yT[:, kc, t * 128:(t + 1) * 128], rhs=wv[:, kc, :], start=(kc == 0), stop=(kc == KC - 1)),
                         reads=[wB, yTB[t]], writes=[pB], inc=(kc == KC - 1))
                evac(ei, Vr[:, ks * 4 + t, 8 * i:8 * i + 8, 0:64], p[:].rearrange("p (h d) -> p h d", d=64), [pB], [VB[ks]])
                ei += 1
        for i in range(2):
            wt, wB = w_get(L, "iq%d" % i)
            wv = wt[:, 0:4096].rearrange("p (k c) -> p k c", k=KC)
            for c in range(4):
                proj_fm(wv, wB, c, qT[:, 4 * i + c, :], qTB, ei, scale=0.125)
                ei += 1
        k.bank_n = 5
        alias_fence(PTB3f, PTB)
        units = [(qt, hp) for qt in range(TPG) for hp in range(8)]
        U = {}

        def st_phase(u):
            qt, hp = units[u]
            T = g * 4 + qt
            kts = [kt for kt in range(T - 4, T + 1) if kt >= 0]
            sbk = [bank(), bank(), bank()]
            pt, ptBs = PT[u % 2], PTB3[u % 2]
            stops = []
            fixes = []
            seq_ = [(kt, 0) for kt in kts] + [(None, None)] + [(kt, 1) for kt in kts]
            for (kt, e_) in seq_:
                if kt is None:
                    stops.append((lambda e, sbk=sbk: e.matmul(sbk[2][0][:, 256:258], lhsT=ident[:], rhs=ident[:, 0:2], start=True, stop=True),
                                  [identB], [sbk[2][1]]))
                    continue
                hh = 2 * hp + e_
                lo, hi = e_ * 64, (e_ + 1) * 64
                j = kt - (T - 4)
                if j < 4:
                    reg, regB = sbk[e_][0][:, j * 128:(j + 1) * 128], sbk[e_][1]
                    pcol, part = e_ * 512 + j * 128, e_
                elif len(kts) == 1 and e_ == 1:
                    reg, regB = sbk[1][0][:, 0:128], sbk[1][1]
                    pcol, part = 1024 + e_ * 128, 2
                else:
                    reg, regB = sbk[2][0][:, e_ * 128:(e_ + 1) * 128], sbk[2][1]
                    pcol, part = 1024 + e_ * 128, 2
                kslot = (kt // 4) % 2
                k0 = kslot * G + (kt % 4) * 128
                delta = T - kt
                stops.append((lambda e, reg=reg, lo=lo, hi=hi, k0=k0, hp=hp, qt=qt: e.matmul(reg, lhsT=kT[lo:hi, hp, k0:k0 + 128], rhs=qT[lo:hi, hp, qt * 128:(qt + 1) * 128], start=True, stop=True),
                              [kTB[kslot], qTB], [regB]))
                if delta in (0, 1, 4):
                    fac = bt[:, hh, 0, :] if delta == 0 else (bt[:, hh, 1, :] if delta == 1 else m4[:])
                    fixes.append((pcol, part, fac))
            for si, (fn_, rd_, wr_) in enumerate(stops):
                S.op("pe", fn_, reads=rd_, writes=wr_, inc=(si == len(stops) - 1))
            U[u] = dict(sbk=sbk, pt=pt, ptBs=ptBs, fixes=fixes, kts=kts, T=T)


        def ex_phase(u):
            d = U[u]
            sbk, pt, ptBs, kts = d["sbk"], d["pt"], d["ptBs"], d["kts"]
            c0 = (5 - len(kts)) * 128
            if c0 < 512:
                for e_ in range(2):
                    S.op("act", lambda e, e_=e_: e.activation(out=pt[:, e_ * 512 + c0:(e_ + 1) * 512], in_=sbk[e_][0][:, c0:512], func=AF.Exp),
                         reads=[sbk[e_][1]], writes=[ptBs[e_]])
            if len(kts) == 1:
                S.op("act", lambda e: e.activation(out=pt[:, 1024:1152], in_=sbk[2][0][:, 0:128], func=AF.Exp), reads=[sbk[2][1]], writes=[ptBs[2]])
                S.op("act", lambda e: e.activation(out=pt[:, 1152:1280], in_=sbk[1][0][:, 0:128], func=AF.Exp), reads=[sbk[1][1]], writes=[ptBs[2]])
            else:
                S.op("act", lambda e: e.activation(out=pt[:, 1024:1280], in_=sbk[2][0][:, 0:256], func=AF.Exp), reads=[sbk[2][1]], writes=[ptBs[2]])
            for (pcol, part, fac) in d["fixes"]:
                S.op("dve", lambda e, pcol=pcol, fac=fac: e.tensor_tensor(out=pt[:, pcol:pcol + 128], in0=pt[:, pcol:pcol + 128], in1=fac, op=ALU.mult),
                     reads=[ptBs[part], btB, m4B], writes=[ptBs[part]])

        def pv_phase(u):
            d = U[u]
            pt, ptBs, kts, T = d["pt"], d["ptBs"], d["kts"], d["T"]
            qt, hp = units[u]
            for e_ in range(2):
                hh = 2 * hp + e_
                ob, obB = psum[5 + hh // 6], psumB[5 + hh // 6]
                col = (hh % 6) * 65
                for idx, kt in enumerate(kts):
                    j = kt - (T - 4)
                    pc = pt[:, e_ * 512 + j * 128:e_ * 512 + (j + 1) * 128] if j < 4 else pt[:, 1024 + e_ * 128:1024 + (e_ + 1) * 128]
                    last = (idx == len(kts) - 1)
                    S.op("pe", lambda e, pc=pc, kt=kt, hh=hh, idx=idx, last=last, ob=ob, col=col: e.matmul(ob[:, col:col + 65], lhsT=pc, rhs=Vr[:, kt % 8, hh, :], start=(idx == 0), stop=last),
                         reads=[ptBs[e_], ptBs[2], VB[(kt // 4) % 2]], writes=[obB], inc=(last and e_ == 1))

        def fin_qt(qt):
            for b in range(3):
                nh = 6 if b < 2 else 4
                ob, obB = psum[5 + b], psumB[5 + b]
                view = ob[:, 0:nh * 65].rearrange("p (h c) -> p h c", c=65)
                S.op("dve", lambda e, view=view, nh=nh: e.reciprocal(out=rden[:, 0:nh], in_=view[:, :, 64]), reads=[obB], writes=[rdenB])
                S.op("dve", lambda e, view=view, nh=nh, b=b: e.tensor_tensor(out=otok[:, qt, b * 384:b * 384 + nh * 64].rearrange("p (h d) -> p h d", d=64),
                                                                            in0=view[:, :, 0:64], in1=rden[:, 0:nh].unsqueeze(2).broadcast_to([128, nh, 64]), op=ALU.mult),
                     reads=[obB, rdenB], writes=[otokB])
            otok_to_yT(qt)

        st_phase(0)
        for u in range(len(units)):
            ex_phase(u)
            if u + 1 < len(units):
                st_phase(u + 1)
            pv_phase(u)
            if units[u][1] == 7:
                fin_qt(units[u][0])
        k.bank_n = 8
        out_proj_post(L, hb, hbB, "nwR%d_1" % L)

    def mm_group(p, pB, lhs_fn, rhs_fn, reads, n=KC):
        for kc in range(n):
            S.op("pe", lambda e, kc=kc: e.matmul(p, lhsT=lhs_fn(kc), rhs=rhs_fn(kc), start=(kc == 0), stop=(kc == n - 1)),
                 reads=reads, writes=[pB], inc=(kc == n - 1))

    def rope_chunk(wv, wB, c, dst, dstB):
        pa, paB = bank()
        mm_group(pa[:], paB, lambda kc: wv[:, kc, c * 128:(c + 1) * 128], lambda kc: yT[:, kc, :], [wB] + yTB)
        pr, prB = bank()
        mm_group(pr[:], prB, lambda kc: wv[:, kc, (c + 1) * 128:(c + 2) * 128], lambda kc: yT[:, kc, :], [wB] + yTB)
        t1, t1B = csb[0], csbB[0]
        t2, t2B = csb[1], csbB[1]
        S.op("dve", lambda e: e.tensor_tensor(out=t1[:], in0=pa[:], in1=ropeT[:, 0, :], op=ALU.mult), reads=[paB, ropeB], writes=[t1B])
        S.op("dve", lambda e: e.tensor_tensor(out=t2[:], in0=pr[:], in1=ropeT[:, 1, :], op=ALU.mult), reads=[prB, ropeB], writes=[t2B])
        S.op("pool", lambda e: e.tensor_tensor(out=dst, in0=t1[:], in1=t2[:], op=ALU.add), reads=[t1B, t2B], writes=[dstB])

    def hgrn_head(hd, wvq, wBq, cq, wvf, wBf, cf, g, first):
        i2 = hd % 2
        pq, pqB = bank()
        mm_group(pq[:], pqB, lambda kc: wvq[:, kc, cq * 128:(cq + 1) * 128], lambda kc: yT[:, kc, :], [wBq] + yTB)
        pf, pfB = bank()
        mm_group(pf[:], pfB, lambda kc: wvf[:, kc, cf * 128:(cf + 1) * 128], lambda kc: yT[:, kc, :], [wBf] + yTB)
        fa, faB = csb[0], csbB[0]
        lc, lcB = csb[1], csbB[1]
        e1, e1B = gsb[0], gsbB[0]
        e2, e2B = gsb[1], gsbB[1]
        hv, hvB = hvec[i2], hvecB[i2]
        S.op("act", lambda e: e.activation(out=fa[:], in_=pf[:], func=AF.Sigmoid), reads=[pfB], writes=[faB])
        S.op("dve", lambda e: e.tensor_scalar(out=fa[:], in0=fa[:], scalar1=lbv[:, 4 + hd:5 + hd], scalar2=lbv[:, hd:hd + 1], op0=ALU.mult, op1=ALU.add),
             reads=[faB, evB], writes=[faB])
        S.op("act", lambda e: e.activation(out=e1[:], in_=fa[:], func=AF.Ln), reads=[faB], writes=[e1B])
        for t in range(TPG):
            S.op("dve", lambda e, t=t: e.tensor_tensor_scan(out=lc[:, t * 128:(t + 1) * 128], data0=ones[:], data1=e1[:, t * 128:(t + 1) * 128], initial=0.0, op0=ALU.mult, op1=ALU.add),
                 reads=[e1B, evB], writes=[lcB])
        lc3 = lc[:].rearrange("p (t m) -> p t m", t=TPG)
        S.op("dve", lambda e: e.tensor_copy(out=hv[:, :, 0], in_=lc3[:, :, 63]), reads=[lcB], writes=[hvB])
        S.op("act", lambda e: e.activation(out=hv[:, :, 1], in_=lc3[:, :, 63], func=AF.Exp), reads=[lcB], writes=[hvB])
        S.op("act", lambda e: e.activation(out=hv[:, :, 2], in_=lc3[:, :, 127], func=AF.Exp), reads=[lcB], writes=[hvB])
        S.op("dve", lambda e: e.tensor_tensor(out=lc3, in0=lc3, in1=hv[:, :, 0:1].broadcast_to([128, TPG, 128]), op=ALU.subtract), reads=[lcB, hvB], writes=[lcB])
        S.op("act", lambda e: e.activation(out=hv[:, :, 3], in_=lc3[:, :, 127], func=AF.Exp), reads=[lcB], writes=[hvB])
        S.op("act", lambda e: e.activation(out=e1[:], in_=lc[:], func=AF.Exp), reads=[lcB], writes=[e1B])
        S.op("act", lambda e: e.activation(out=e2[:], in_=lc[:], func=AF.Exp, scale=-1.0), reads=[lcB], writes=[e2B])
        qd, qdB = qdT[i2], qdTB[i2]
        kd, kdB = kdT[i2], kdTB[i2]
        S.op("dve", lambda e: e.tensor_tensor(out=qd[:], in0=pq[:], in1=e1[:], op=ALU.mult), reads=[pqB, e1B], writes=[qdB])
        S.op("act", lambda e: e.activation(out=fa[:], in_=pf[:], func=AF.Sigmoid, scale=-1.0), reads=[pfB, faB], writes=[faB])
        S.op("dve", lambda e: e.scalar_tensor_tensor(out=kd[:], in0=fa[:], scalar=lbv[:, 4 + hd:5 + hd], op0=ALU.mult, in1=e2[:], op1=ALU.mult), reads=[faB, e2B, evB], writes=[kdB])
        kk, kkB = kdTok[i2], kdTokB[i2]
        pt_, ptB_ = bank()
        ptb = pt_[:].bitcast(BF16)
        for t in range(TPG):
            S.op("pe", lambda e, t=t: e.transpose(out=ptb[:, t * 128:(t + 1) * 128], in_=kd[:, t * 128:(t + 1) * 128], identity=ident[:]),
                 reads=[kdB, identB], writes=[ptB_], inc=(t == TPG - 1))
        evac(hd, kk[:].rearrange("p t d -> p (t d)"), ptb[:, 0:512], [ptB_], [kkB])

    def hgrn_tile(hd, t, g, first):
        i2 = hd % 2
        hv, hvB = hvec[i2], hvecB[i2]
        qd, qdB = qdT[i2], qdTB[i2]
        kd, kdB = kdT[i2], kdTB[i2]
        kk, kkB = kdTok[i2], kdTokB[i2]
        tile0 = first and t == 0
        sl = slice(t * 128, (t + 1) * 128)
        vv = iTok[:, t, hd * 128:(hd + 1) * 128]
        bi = i2 * 2 + (t % 2)
        ps_, psB_ = bank()
        S.op("pe", lambda e: e.matmul(ps_[:, 0:128], lhsT=kd[:, sl], rhs=qd[:, sl], start=True, stop=True), reads=[kdB, qdB], writes=[psB_])
        sm, smB = sTm[bi], sTmB[bi]
        S.op("dve", lambda e: e.tensor_tensor(out=sm[:], in0=ps_[:, 0:128], in1=mcb[:], op=ALU.mult), reads=[psB_, evB], writes=[smB])
        if not tile0:
            S.op("act", lambda e: e.activation(out=stS[:, hd, :], in_=state[:, hd, :], func=AF.Identity, scale=hv[:, t, 1:2]),
                 reads=[stateB[hd], hvB], writes=[stSB[hd]])
        po, poB = bank()
        S.op("pe", lambda e: e.matmul(po[:, 0:128], lhsT=sm[:], rhs=vv, start=True, stop=tile0), reads=[smB, iTokB], writes=[poB], inc=tile0)
        if not tile0:
            S.op("pe", lambda e: e.matmul(po[:, 0:128], lhsT=qd[:, sl], rhs=stS[:, hd, :], start=False, stop=True), reads=[qdB, stSB[hd]], writes=[poB])
        if not (g == NG - 1 and t == TPG - 1):
            pd, pdB = bank()
            S.op("pe", lambda e: e.matmul(pd[:, 0:128], lhsT=kk[:, t, :], rhs=vv, start=True, stop=True), reads=[kkB, iTokB], writes=[pdB])
            if tile0:
                S.op("dve", lambda e: e.tensor_scalar(out=state[:, hd, :], in0=pd[:, 0:128], scalar1=hv[:, t, 3:4], scalar2=None, op0=ALU.mult),
                     reads=[pdB, hvB], writes=[stateB[hd]])
            else:
                td_, tdB_ = tmpd[bi], tmpdB[bi]
                S.op("dve", lambda e: e.tensor_scalar(out=td_[:], in0=pd[:, 0:128], scalar1=hv[:, t, 3:4], scalar2=None, op0=ALU.mult),
                     reads=[pdB, hvB], writes=[tdB_])
                S.op("dve", lambda e: e.scalar_tensor_tensor(out=state[:, hd, :], in0=state[:, hd, :], scalar=hv[:, t, 2:3], op0=ALU.mult, in1=td_[:], op1=ALU.add),
                     reads=[tdB_, hvB, stateB[hd]], writes=[stateB[hd]])
        junk, junkB = next_junk()
        S.op("act", lambda e: e.activation(out=junk[:, 0:128], in_=po[:, 0:128], func=AF.Square, accum_out=ssh[:, t * 4 + hd:t * 4 + hd + 1]),
             reads=[poB], writes=[sshB[t * 4 + hd], junkB])
        S.op("dve", lambda e: e.tensor_copy(out=tmpo[:, t, hd * 128:(hd + 1) * 128], in_=po[:, 0:128]), reads=[poB], writes=[tmpoB[t]])

    def even_mixer(L, hb, hbB, g):
        first = (g == 0)
        alias_fence(mixBs, [gTB])
        r0 = g * G
        S.dma("sp", "rope", lambda e: e.dma_start(out=ropeT[:], in_=rope_d[:, :, r0:r0 + G]), writes=[ropeB])
        norm_transpose(hb, hbB, cs("nwT%d_0" % L))
        ks = g % 2
        def piece(i):
            wt, wB = w_get(L, "e%d" % i)
            n = len(EVEN_FM[i])
            return wt[:, 0:KC * 128 * n].rearrange("p (k c) -> p k c", k=KC), wB
        wt5, wB5 = w_get(L, "e5")
        w5 = wt5[:, 0:KC * 640].rearrange("p (k c) -> p k c", k=KC)
        for t in range(TPG):
            p, pB = bank()
            mm_group(p[:, 0:128], pB, lambda kc, t=t: yT[:, kc, t * 128:(t + 1) * 128], lambda kc: w5[:, kc, 0:128], [wB5, yTB[t]])
            evac(t, Va[:, ks * 4 + t, :, 0:64], p[:, 0:128].rearrange("p (h d) -> p h d", d=64), [pB], [VaB[ks]])
            p, pB = bank()
            mm_group(p[:], pB, lambda kc, t=t: yT[:, kc, t * 128:(t + 1) * 128], lambda kc: w5[:, kc, 128:640], [wB5, yTB[t]])
            evac(t + 1, iTok[:, t, :], p[:], [pB], [iTokB])
        wt6, wB6 = w_get(L, "e6")
        w6 = wt6[:, 0:KC * 512].rearrange("p (k c) -> p k c", k=KC)
        for t in range(TPG):
            p, pB = bank()
            mm_group(p[:], pB, lambda kc, t=t: yT[:, kc, t * 128:(t + 1) * 128], lambda kc: w6[:, kc, :], [wB6, yTB[t]])
            S.op("act", lambda e, p=p, t=t: e.activation(out=sg[:, t, :], in_=p[:], func=AF.Silu), reads=[pB], writes=[sgB])
            S.op("pool", lambda e, t=t: e.tensor_tensor(out=sg[:, t, :], in0=sg[:, t, :], in1=cs("gw"), op=ALU.mult), reads=[sgB, cstB], writes=[sgB])
        wv, wB = piece(0)
        rope_chunk(wv, wB, 0, kaT[:, ks * G:(ks + 1) * G], kaTB[ks])
        rope_chunk(wv, wB, 2, qaT[:, 0, :], qTB)
        wv, wB = piece(1)
        rope_chunk(wv, wB, 0, qaT[:, 1, :], qTB)
        rope_chunk(wv, wB, 2, qaT[:, 2, :], qTB)
        wv2, wB2 = piece(2)
        rope_chunk(wv2, wB2, 0, qaT[:, 3, :], qTB)
        k.bank_n = 6
        alias_fence(PTB, PTB3f)
        units = [(qt, c) for qt in range(TPG) for c in range(4)]
        U = {}

        def st_phase(u):
            qt, c = units[u]
            T = g * 4 + qt
            kts = [kt for kt in (T - 1, T) if kt >= 0]
            sb2 = [bank(), bank()]
            ops_ = []
            for e_ in range(2):
                if e_ == 1:
                    ops_.append((lambda e, sb2=sb2: e.matmul(sb2[1][0][:, 256:258], lhsT=ident[:], rhs=ident[:, 0:2], start=True, stop=True),
                                 [identB], [sb2[1][1]]))
                for kt in kts:
                    lo, hi = e_ * 64, (e_ + 1) * 64
                    j = kt - (T - 1)
                    reg = sb2[e_][0][:, j * 128:(j + 1) * 128]
                    kslot = (kt // 4) % 2
                    k0 = kslot * G + (kt % 4) * 128
                    ops_.append((lambda e, reg=reg, lo=lo, hi=hi, k0=k0, c=c, qt=qt: e.matmul(reg, lhsT=kaT[lo:hi, k0:k0 + 128], rhs=qaT[lo:hi, c, qt * 128:(qt + 1) * 128], start=True, stop=True),
                                 [kaTB[kslot], qTB], [sb2[e_][1]]))
            for si, (fn_, rd_, wr_) in enumerate(ops_):
                S.op("pe", fn_, reads=rd_, writes=wr_, inc=(si == len(ops_) - 1))
            U[u] = dict(sb2=sb2, kts=kts, T=T)

        def ex_phase(u):
            d = U[u]
            sb2, kts = d["sb2"], d["kts"]
            pt, ptB = PTa[u % 2], PTB[u % 2]
            j0 = 2 - len(kts)
            for e_ in range(2):
                S.op("act", lambda e, e_=e_: e.activation(out=pt[:, e_ * 256 + j0 * 128:(e_ + 1) * 256], in_=sb2[e_][0][:, j0 * 128:256], func=AF.Exp, scale=0.125),
                     reads=[sb2[e_][1]], writes=[ptB])
            S.op("dve", lambda e: e.tensor_tensor(out=pt.rearrange("p (e j q) -> p e j q", e=2, j=2)[:, :, j0:2, :],
                                                  in0=pt.rearrange("p (e j q) -> p e j q", e=2, j=2)[:, :, j0:2, :], in1=mfull[:, :, j0:2, :], op=ALU.mult),
                 reads=[ptB, evB], writes=[ptB])

        def pv_phase(u):
            d = U[u]
            kts, T = d["kts"], d["T"]
            qt, c = units[u]
            pt, ptB = PTa[u % 2], PTB[u % 2]
            for e_ in range(2):
                ob, obB = psum[6 + e_], psumB[6 + e_]
                col = c * 65
                for idx, kt in enumerate(kts):
                    j = kt - (T - 1)
                    pc = pt[:, e_ * 256 + j * 128:e_ * 256 + (j + 1) * 128]
                    last = (idx == len(kts) - 1)
                    S.op("pe", lambda e, pc=pc, kt=kt, e_=e_, idx=idx, last=last, ob=ob, col=col: e.matmul(ob[:, col:col + 65], lhsT=pc, rhs=Va[:, kt % 8, e_, :], start=(idx == 0), stop=last),
                         reads=[ptB, VaB[(kt // 4) % 2]], writes=[obB], inc=(last and e_ == 1))

        def fin_qt(qt):
            for b in range(2):
                ob, obB = psum[6 + b], psumB[6 + b]
                view = ob[:, 0:260].rearrange("p (h c) -> p h c", c=65)
                S.op("dve", lambda e, view=view, b=b: e.tensor_tensor(out=rden[:, 0:4], in0=view[:, :, 64], in1=esink[:, 4 * b:4 * b + 4], op=ALU.add), reads=[obB, evB], writes=[rdenB])
                S.op("dve", lambda e: e.reciprocal(out=rden[:, 0:4], in_=rden[:, 0:4]), reads=[rdenB], writes=[rdenB])
                S.op("dve", lambda e, view=view, b=b: e.tensor_tensor(out=otok[:, qt, b * 256:(b + 1) * 256].rearrange("p (h d) -> p h d", d=64),
                                                                      in0=view[:, :, 0:64], in1=rden[:, 0:4].unsqueeze(2).broadcast_to([128, 4, 64]), op=ALU.mult),
                     reads=[obB, rdenB], writes=[otokB])

        st_phase(0)
        for u in range(len(units)):
            ex_phase(u)
            if u + 1 < len(units):
                st_phase(u + 1)
            pv_phase(u)
            if units[u][1] == 3:
                fin_qt(units[u][0])
        k.bank_n = 8
        hgrn_head(0, wv2, wB2, 2, wv2, wB2, 3, g, first)
        wv3, wB3 = piece(3)
        hgrn_head(1, wv3, wB3, 0, wv3, wB3, 1, g, first)
        for t in range(TPG):
            hgrn_tile(0, t, g, first)
            hgrn_tile(1, t, g, first)
        hgrn_head(2, wv3, wB3, 2, wv3, wB3, 3, g, first)
        wv4, wB4 = piece(4)
        hgrn_head(3, wv4, wB4, 0, wv4, wB4, 1, g, first)
        for t in range(TPG):
            hgrn_tile(2, t, g, first)
            hgrn_tile(3, t, g, first)
        S.op("act", lambda e: e.activation(out=rsh[:], in_=ssh[:], func=AF.Sqrt, scale=1.0 / 128, bias=EPS), reads=sshB, writes=[rshB])
        S.op("dve", lambda e: e.reciprocal(out=rsh[:], in_=rsh[:]), reads=[rshB], writes=[rshB])
        for t in range(TPG):
            o3 = tmpo[:, t, 0:512].rearrange("p (h d) -> p h d", h=4)
            S.op("dve", lambda e, t=t, o3=o3: e.tensor_tensor(out=o3, in0=o3, in1=rsh[:, t * 4:(t + 1) * 4].unsqueeze(2).broadcast_to([128, 4, 128]), op=ALU.mult),
                 reads=[tmpoB[t], rshB], writes=[tmpoB[t]])
            S.op("dve", lambda e, t=t: e.tensor_tensor(out=otok[:, t, 512:1024], in0=tmpo[:, t, 0:512], in1=sg[:, t, :], op=ALU.mult), reads=[tmpoB[t], sgB], writes=[otokB])
        for qt in range(TPG):
            otok_to_yT(qt)
        out_proj_post(L, hb, hbB, "nwR%d_1" % L)

    def ffn_block(L, hb, hbB, first, last):
        import os
        stage = int(os.environ.get("FFN_STAGE", "9"))
        alias_fence([gTB], mixBs)
        norm_transpose(hb, hbB, cs("nwT%d_2" % L))
        if stage < 2:
            return
        cw = cs("cw%d" % L).rearrange("p (f j) -> p f j", j=3)
        cb = cs("cb%d" % L)
        cidx = 0
        for j in range(11):
            wt, wB = w_get(L, "up%d" % j)
            wv = wt[:, 0:4096].rearrange("p (k c) -> p k c", k=KC)
            for i in range(2):
                fc = 2 * j + i
                pu, puB = bank()
                for kc in range(KC):
                    S.op("pe", lambda e, kc=kc, i=i, pu=pu, wv=wv: e.matmul(pu[:], lhsT=wv[:, kc, i * 128:(i + 1) * 128], rhs=yT[:, kc, :], start=(kc == 0), stop=(kc == KC - 1)),
                         reads=[wB] + yTB, writes=[puB], inc=(kc == KC - 1))
                pv, pvB = bank()
                for kc in range(KC):
                    S.op("pe", lambda e, kc=kc, i=i, pv=pv, wv=wv: e.matmul(pv[:], lhsT=wv[:, kc, 256 + i * 128:256 + (i + 1) * 128], rhs=yT[:, kc, :], start=(kc == 0), stop=(kc == KC - 1)),
                         reads=[wB] + yTB, writes=[pvB], inc=(kc == KC - 1))
                c, cB = csb[cidx % 2], csbB[cidx % 2]
                gg, gB = gsb[cidx % 2], gsbB[cidx % 2]
                cidx += 1
                if stage < 3:
                    continue
                S.op("act", lambda e, fc=fc, c=c, pu=pu: e.activation(out=c[:], in_=pu[:], func=AF.Identity, scale=cw[:, fc, 2:3], bias=cb[:, fc:fc + 1]),
                     reads=[puB, cstB], writes=[cB])
                S.op("dve", lambda e, fc=fc, c=c, pu=pu: e.scalar_tensor_tensor(out=c[:, 1:G], in0=pu[:, 0:G - 1], scalar=cw[:, fc, 1:2], op0=ALU.mult, in1=c[:, 1:G], op1=ALU.add),
                     reads=[puB, cstB, cB], writes=[cB])
                S.op("dve", lambda e, fc=fc, c=c, pu=pu: e.scalar_tensor_tensor(out=c[:, 2:G], in0=pu[:, 0:G - 2], scalar=cw[:, fc, 0:1], op0=ALU.mult, in1=c[:, 2:G], op1=ALU.add),
                     reads=[puB, cstB, cB], writes=[cB])
                if stage < 4:
                    continue
                if not first:
                    S.op("dve", lambda e, fc=fc, c=c: e.scalar_tensor_tensor(out=c[:, 0:1], in0=carry[L][:, fc, 1:2], scalar=cw[:, fc, 1:2], op0=ALU.mult, in1=c[:, 0:1], op1=ALU.add),
                         reads=[carryB[L], cstB, cB], writes=[cB])
                    S.op("dve", lambda e, fc=fc, c=c: e.scalar_tensor_tensor(out=c[:, 0:2], in0=carry[L][:, fc, 0:2], scalar=cw[:, fc, 0:1], op0=ALU.mult, in1=c[:, 0:2], op1=ALU.add),
                         reads=[carryB[L], cstB, cB], writes=[cB])
                if not last:
                    S.op("dve", lambda e, fc=fc, pu=pu: e.tensor_copy(out=carry[L][:, fc, :], in_=pu[:, G - 2:G]),
                         reads=[puB], writes=[carryB[L]])
                S.op("act", lambda e, c=c, gg=gg: e.activation(out=gg[:], in_=c[:], func=AF.Gelu), reads=[cB], writes=[gB])
                S.op("dve", lambda e, fc=fc, gg=gg, pv=pv: e.tensor_tensor(out=gT[:, fc, :], in0=pv[:], in1=gg[:], op=ALU.mult),
                     reads=[pvB, gB], writes=[gTB])

        if stage < 5:
            for _ in range(4):
                w_get(L, order[k.w_use][1])
            return

        def produce(cg):
            banks = [bank() for _ in range(TPG)]
            for hf in range(2):
                wt, wB = w_get(L, "dn%d_%d" % (cg, hf))
                wv = wt[:, 0:5632].rearrange("p (f c) -> p f c", f=11)
                for t in range(TPG):
                    p, pB = banks[t]
                    for f in range(11):
                        fc = hf * 11 + f
                        S.op("pe", lambda e, t=t, f=f, fc=fc, p=p, wv=wv: e.matmul(p[:], lhsT=gT[:, fc, t * 128:(t + 1) * 128], rhs=wv[:, f, :], start=(fc == 0), stop=(fc == FC - 1)),
                             reads=[wB, gTB], writes=[pB], inc=(f == 10))
            return banks
        post_norm_residual(hb, hbB, "nwR%d_3" % L, produce)

    k.ffn_block = ffn_block
    k.extra = {}

    gi = 0
    groups = [(s_, g_) for s_ in range(nseq) for g_ in range(NG)]

    def load_tile(gidx, t):
        s_, g_ = groups[gidx]
        r0 = s_ * seq + g_ * G + t * 128
        hb_, hbB_ = h[gidx % NH], hB[gidx % NH]
        S.dma("pool", "ldx%d_%d" % (gidx % NH, t), lambda e: e.dma_start(out=hb_[:, t, :], in_=x_d[r0:r0 + 128, :]), writes=[hbB_[t]])

    def store_tile(gidx, t):
        s_, g_ = groups[gidx]
        r0 = s_ * seq + g_ * G + t * 128
        hb_, hbB_ = h[gidx % NH], hB[gidx % NH]
        S.dma("pool", "stx%d_%d" % (gidx % NH, t), lambda e: e.dma_start(out=out_d[r0:r0 + 128, :], in_=hb_[:, t, :]), reads=[hbB_[t]])

    for t in range(TPG):
        load_tile(0, t)
    for gi, (s, g) in enumerate(groups):
        hb, hbB = h[gi % NH], hB[gi % NH]
        for L in layers:
            if "mixer" in parts:
                if L == 1:
                    odd_mixer(L, hb, hbB, g)
                else:
                    even_mixer(L, hb, hbB, g)
            if "ffn" in parts:
                ffn_block(L, hb, hbB, first=(g == 0), last=(g == NG - 1))
        for t in range(TPG):
            store_tile(gi, t)
            if gi + 1 < len(groups):
                load_tile(gi + 1, t)
    S.wait_all("pool", [b for hl in hB for b in hl])
    S.emit()
    return nc


def host_prep(inp):
    return {"wsrc": weights_host(inp).reshape(-1, 2048), "cst": consts_host(inp), "relb": relb_host(inp),
            "rope": rope_host(int(inp["x"].shape[1]))}


_NC_CACHE = {}


def kernel(**inputs):
    inp = {k_: np.asarray(v) for k_, v in inputs.items()}
    x = inp["x"]
    B, Sq, _ = x.shape
    ncores = 8
    nseq = B // ncores
    key = (nseq, Sq)
    if key not in _NC_CACHE:
        _NC_CACHE[key] = build(nseq, Sq)
    nc = _NC_CACHE[key]
    host = host_prep(inp)
    in_maps = []
    for c in range(ncores):
        m = dict(host)
        m["x"] = np.ascontiguousarray(x[c * nseq:(c + 1) * nseq].reshape(nseq * Sq, D))
        in_maps.append(m)
    res = run_bass_kernel_spmd(nc, in_maps, core_ids=list(range(ncores)))
    out = np.stack([np.asarray(r["out"]).reshape(nseq, Sq, D) for r in res.results])
    return out.reshape(B, Sq, D).astype(np.float32)
```

```python
import numpy as np
import concourse.bass as bass
import concourse.mybir as mybir
from concourse.bass_utils import run_bass_kernel_spmd

F32 = mybir.dt.float32
BF16 = mybir.dt.bfloat16
AF = mybir.ActivationFunctionType
ALU = mybir.AluOpType
AX = mybir.AxisListType

ENG_ATTR = {"pe": "tensor", "act": "scalar", "dve": "vector", "pool": "gpsimd", "sp": "sync"}


class Buf:
    __slots__ = ("name", "w", "r", "excl")

    def __init__(self, name, excl=False):
        self.name = name
        self.w = None
        self.r = []
        self.excl = excl


class Sched:
    def __init__(self, nc, same_engine_sync=True):
        self.nc = nc
        self.same = same_engine_sync
        self.ops = {k: [] for k in ENG_ATTR}
        self.cnt = {k: 0 for k in ENG_ATTR}
        self.seen = {k: {} for k in ENG_ATTR}
        self.sems = {}
        self.dma_total = {}
        for k in ENG_ATTR:
            self.sems[k] = nc.alloc_semaphore("s_" + k)

    def dma_sem(self, key):
        if key not in self.sems:
            self.sems[key] = self.nc.alloc_semaphore("d_" + key)
            self.dma_total[key] = 0
        return key

    def _deps(self, eng, reads, writes):
        deps = {}
        for b in reads:
            if b.w is not None:
                k, v = b.w
                deps[k] = max(deps.get(k, 0), v)
            if b.excl:
                for (k, v) in b.r:
                    if k != eng:
                        deps[k] = max(deps.get(k, 0), v)
        for b in writes:
            if b.w is not None:
                k, v = b.w
                deps[k] = max(deps.get(k, 0), v)
            for (k, v) in b.r:
                deps[k] = max(deps.get(k, 0), v)
        seen = self.seen[eng]
        for k, v in deps.items():
            if k == eng and (eng == "pe" or not self.same):
                continue
            if seen.get(k, 0) < v:
                seen[k] = v
                sem = self.sems[k]
                self.ops[eng].append(lambda e, sem=sem, v=v: e.wait_ge(sem, v))

    def op(self, eng, fn, reads=(), writes=(), inc=True):
        self._deps(eng, reads, writes)
        if inc:
            self.cnt[eng] += 1
            tok = (eng, self.cnt[eng])
            sem = self.sems[eng]
            self.ops[eng].append(lambda e, fn=fn, sem=sem: fn(e).then_inc(sem, 1))
        else:
            tok = (eng, self.cnt[eng] + 1)
            self.ops[eng].append(lambda e, fn=fn: fn(e))
        for b in reads:
            b.r.append(tok)
        for b in writes:
            b.w = tok
            b.r = []
        return tok

    def dma(self, eng, semkey, fn, reads=(), writes=()):
        self.dma_sem(semkey)
        self._deps(eng, reads, writes)
        self.dma_total[semkey] += 16
        tok = (semkey, self.dma_total[semkey])
        sem = self.sems[semkey]
        self.ops[eng].append(lambda e, fn=fn, sem=sem: fn(e).then_inc(sem, 16))
        for b in reads:
            b.r.append(tok)
        for b in writes:
            b.w = tok
            b.r = []
        return tok

    def wait_all(self, eng, bufs):
        self._deps(eng, (), bufs)

    def emit(self):
        nc = self.nc
        with nc.Block() as block:
            for k, attr in ENG_ATTR.items():
                ops = self.ops[k]
                if not ops:
                    continue

                def body(e, ops=ops):
                    for f in ops:
                        f(e)
                getattr(block, attr)(body)


D = 1024
KC = 8
DFF = 2816
FC = 22
G = 512
TPG = 4
EPS = 1e-6
SLOT = 5632
NSLOT = 3
NEG = -30000.0


def ffn_piece_list():
    out = [("up%d" % j, 4096) for j in range(11)]
    out += [("dn%d_%d" % (cg, hf), 5632) for cg in range(2) for hf in range(2)]
    return out


def ffn_pieces_host(w_up, w_down):
    res = []
    wu = w_up.reshape(KC, 128, 2 * DFF)
    for j in range(11):
        cols = np.concatenate([np.arange(2 * j * 128, (2 * j + 2) * 128),
                               DFF + np.arange(2 * j * 128, (2 * j + 2) * 128)])
        pc = wu[:, :, cols].transpose(1, 0, 2).reshape(128, KC * 512)
        res.append(("up%d" % j, pc))
    wd = w_down.reshape(FC, 128, D)
    for cg in range(2):
        for hf in range(2):
            pc = wd[hf * 11:(hf + 1) * 11, :, cg * 512:(cg + 1) * 512].transpose(1, 0, 2).reshape(128, 11 * 512)
            res.append(("dn%d_%d" % (cg, hf), pc))
    return res


def const_layout():
    lay = {}
    off = 0

    def add(name, w):
        nonlocal off
        lay[name] = (off, w)
        off += w
    for L in range(2):
        add("nwT%d_0" % L, KC)
        add("nwT%d_2" % L, KC)
        add("nwR%d_1" % L, D)
        add("nwR%d_3" % L, D)
        add("cw%d" % L, FC * 3)
        add("cb%d" % L, FC)
    add("ident", 128)
    add("lbl", 8)
    add("sinks", 8)
    add("gw", 512)
    add("mc", 128)
    add("crep", 16)
    add("m0", 128)
    add("m4", 128)
    lay["_total"] = (off, 0)
    return lay


def consts_host(inp):
    lay = const_layout()
    c = np.zeros((128, lay["_total"][0]), np.float32)

    def put(name, arr):
        o, w = lay[name]
        c[:, o:o + w] = arr.reshape(128, w) if arr.shape[0] == 128 else np.broadcast_to(arr.reshape(1, w), (128, w))
    nw = inp["norm_w"]
    for L in range(2):
        put("nwT%d_0" % L, nw[L, 0].reshape(KC, 128).T)
        put("nwT%d_2" % L, nw[L, 2].reshape(KC, 128).T)
        put("nwR%d_1" % L, nw[L, 1])
        put("nwR%d_3" % L, nw[L, 3])
        cw = inp["ffn_conv_w"][L]
        put("cw%d" % L, cw.reshape(3, FC, 128).transpose(2, 1, 0).reshape(128, FC * 3))
        put("cb%d" % L, inp["ffn_conv_b"][L].reshape(FC, 128).T)
    put("ident", np.eye(128, dtype=np.float32))
    put("lbl", inp["hgrn_lb_logits"][0:2].reshape(2, 4, 128).transpose(2, 0, 1).reshape(128, 8))
    put("sinks", inp["even_sinks"][0])
    put("gw", inp["hgrn_norm_w"][0])
    put("mc", (np.arange(128)[:, None] <= np.arange(128)[None, :]).astype(np.float32))
    put("crep", inp["odd_rel_bias"][0][:, 256])
    jj = np.arange(128)[:, None]
    qq = np.arange(128)[None, :]
    put("m0", np.where((jj >= 64) & (qq < 64), NEG, 0.0).astype(np.float32))
    put("m4", np.where((jj < 64) & (qq >= 64), NEG, 0.0).astype(np.float32))
    return c


def relb_host(inp):
    tab = inp["odd_rel_bias"][0]
    jj = np.arange(128)[:, None]
    qq = np.arange(128)[None, :]
    i0 = (qq - jj) + 128
    i1 = np.minimum(128 + qq - jj, 128) + 128
    b0 = tab[:, i0]
    b1 = tab[:, i1]
    r = np.stack([b0, b1], axis=1)
    return np.ascontiguousarray(r.transpose(2, 0, 1, 3).reshape(128, 16 * 2 * 128).astype(np.float32))


def odd_piece_list():
    return [(n, 4096) for n in ("ik0", "ik1", "iv0", "iv1", "iq0", "iq1", "ow0", "ow1")]


def odd_pieces_host(w_in, w_out):
    wi = w_in.reshape(KC, 128, 3 * D)
    wo = w_out.reshape(KC, 128, D)
    res = []

    def pc(w, c0):
        return w[:, :, c0:c0 + 512].transpose(1, 0, 2).reshape(128, KC * 512)
    for i in range(2):
        res.append(("ik%d" % i, pc(wi, D + i * 512)))
    for i in range(2):
        res.append(("iv%d" % i, pc(wi, 2 * D + i * 512)))
    for i in range(2):
        res.append(("iq%d" % i, pc(wi, i * 512)))
    for i in range(2):
        res.append(("ow%d" % i, pc(wo, i * 512)))
    return res


def layer_piece_list(L):
    if L == 1:
        return odd_piece_list() + ffn_piece_list()
    return even_piece_list() + ffn_piece_list()


EVEN_FM = [["ka", "rka", "qa0", "rqa0"], ["qa1", "rqa1", "qa2", "rqa2"], ["qa3", "rqa3", "qb0", "fb0"],
           ["qb1", "fb1", "qb2", "fb2"], ["qb3", "fb3"]]


def even_piece_list():
    out = [("e5", KC * 640), ("e6", KC * 512)]
    out += [("e%d" % i, KC * 128 * len(ch)) for i, ch in enumerate(EVEN_FM)]
    out += [("ow0", 4096), ("ow1", 4096)]
    return out


def even_cols():
    rot = (np.arange(64) + 32) % 64
    cols = {}
    cols["ka"] = 512 + np.arange(128)
    cols["rka"] = 512 + np.concatenate([rot, 64 + rot])
    for c in range(4):
        h0, h1 = c, 4 + c
        cols["qa%d" % c] = np.concatenate([h0 * 64 + np.arange(64), h1 * 64 + np.arange(64)])
        cols["rqa%d" % c] = np.concatenate([h0 * 64 + rot, h1 * 64 + rot])
        cols["qb%d" % c] = 768 + c * 128 + np.arange(128)
        cols["fb%d" % c] = 1280 + c * 128 + np.arange(128)
    return cols


def even_pieces_host(inp):
    wi = inp["even_w_in"][0].reshape(KC, 128, 2816)
    wo = inp["even_w_out"][0].reshape(KC, 128, D)
    cols = even_cols()
    res = []
    cc = np.concatenate([640 + np.arange(128), 1792 + np.arange(512)])
    res.append(("e5", wi[:, :, cc].transpose(1, 0, 2).reshape(128, KC * 640)))
    cc = 2304 + np.arange(512)
    res.append(("e6", wi[:, :, cc].transpose(1, 0, 2).reshape(128, KC * 512)))
    for i, ch in enumerate(EVEN_FM):
        cc = np.concatenate([cols[n] for n in ch])
        res.append(("e%d" % i, wi[:, :, cc].transpose(1, 0, 2).reshape(128, KC * len(cc))))
    for i in range(2):
        res.append(("ow%d" % i, wo[:, :, i * 512:(i + 1) * 512].transpose(1, 0, 2).reshape(128, KC * 512)))
    return res


def rope_host(seq):
    inv = (10000.0 ** (-np.arange(0, 64, 2, dtype=np.float32) / 64)).astype(np.float32)
    ang = np.arange(seq, dtype=np.float32)[None, :] * inv[:, None]
    cos = np.cos(ang).astype(np.float32)
    sin = np.sin(ang).astype(np.float32)
    c64 = np.concatenate([cos, cos], 0)
    s64 = np.concatenate([-sin, sin], 0)
    t = np.stack([np.concatenate([c64, c64], 0), np.concatenate([s64, s64], 0)], axis=1)
    return np.ascontiguousarray(t.astype(np.float32))


def weights_host(inp):
    chunks = []
    for L in range(2):
        pcs = (odd_pieces_host(inp["odd_w_in"][0], inp["odd_w_out"][0]) if L == 1 else even_pieces_host(inp))
        pcs = pcs + ffn_pieces_host(inp["ffn_w_up"][L], inp["ffn_w_down"][L])
        want = layer_piece_list(L)
        assert [p[0] for p in pcs] == [w[0] for w in want]
        for (n, a), (_, nco) in zip(pcs, want):
            assert a.shape == (128, nco), (n, a.shape, nco)
            chunks.append(np.ascontiguousarray(a, dtype=np.float32).reshape(-1))
    return np.concatenate(chunks)


class K:
    pass


def build(nseq, seq, layers=(0, 1), parts=("mixer", "ffn")):
    nc = bass.Bass("TRN2", target_bir_lowering=False)
    NG = seq // G
    NTOK = nseq * seq
    lay = const_layout()
    NCST = lay["_total"][0]
    ptab = {}
    off = 0
    for L in range(2):
        for (n, nco) in layer_piece_list(L):
            ptab[(L, n)] = (off, nco)
            off += 128 * nco
    WTOT = off

    x_d = nc.dram_tensor("x", [NTOK, D], F32, kind="ExternalInput").ap()
    out_d = nc.dram_tensor("out", [NTOK, D], F32, kind="ExternalOutput").ap()
    wsrc_d = nc.dram_tensor("wsrc", [WTOT // 2048, 2048], F32, kind="ExternalInput").ap()
    cst_d = nc.dram_tensor("cst", [128, NCST], F32, kind="ExternalInput").ap()
    wbf_d = nc.dram_tensor("wbf", [WTOT // 2048, 2048], BF16, kind="Internal").ap()
    relb_d = nc.dram_tensor("relb", [128, 4096], F32, kind="ExternalInput").ap()
    rope_d = nc.dram_tensor("rope", [128, 2, seq], F32, kind="ExternalInput").ap()

    S = Sched(nc)
    k = K()
    k.nc, k.S = nc, S

    def sb(name, shape, dt):
        return nc.alloc_sbuf_tensor("s_" + name, shape, dt)

    cst = sb("cst", [128, NCST], F32)
    cstB = Buf("cst")
    ident = sb("ident", [128, 128], BF16)
    identB = Buf("ident")
    NH = 1
    h = [sb("h%d" % i, [128, TPG, D], F32) for i in range(NH)]
    hB = [[Buf("h%d_%d" % (i, t)) for t in range(TPG)] for i in range(NH)]
    ring = [sb("ring%d" % i, [128, SLOT], BF16) for i in range(NSLOT)]
    ringB = [Buf("ring%d" % i) for i in range(NSLOT)]
    yn = [sb("yn%d" % i, [128, D], BF16) for i in range(2)]
    ynB = [Buf("yn%d" % i) for i in range(2)]
    yT = sb("yT", [128, KC, G], BF16)
    yTB = [Buf("yT%d" % t) for t in range(TPG)]
    arena = sb("arena", [128, FC * G], BF16)
    gT = arena[:].rearrange("p (f t) -> p f t", f=FC)
    gTB = Buf("gT")
    qT = arena[:, 0:4096].rearrange("p (k t) -> p k t", k=KC)
    qTB = Buf("qT")
    otok = arena[:, 4096:8192].rearrange("p (t d) -> p t d", t=TPG)
    otokB = Buf("otok")
    PT = [arena[:, 8192 + i * 1280:8192 + (i + 1) * 1280] for i in range(2)]
    PTB = [Buf("PT%d" % i) for i in range(2)]
    PTB3 = [[Buf("PT%d_%d" % (i, j)) for j in range(3)] for i in range(2)]
    PTB3f = [b for l in PTB3 for b in l]
    mixBs = [qTB, otokB] + PTB + PTB3f

    def alias_fence(dsts, srcs):
        for d_ in dsts:
            for s_ in srcs:
                if s_.w is not None:
                    d_.r.append(s_.w)
                d_.r.extend(s_.r)
    kT = sb("kT", [128, KC, 2 * G], BF16)
    kTB = [Buf("kT%d" % i) for i in range(2)]
    Vr = sb("Vr", [128, 8, 16, 65], BF16)
    VB = [Buf("V%d" % i) for i in range(2)]
    bt = sb("bt", [128, 16, 2, 128], BF16)
    btB = Buf("bt")
    m4 = sb("m4", [128, 128], BF16)
    m4B = Buf("m4")
    rden = sb("rden", [128, 8], F32)
    rdenB = Buf("rden")
    qaT = arena[:, 0:2048].rearrange("p (k t) -> p k t", k=4)
    iTok = arena[:, 2048:4096].rearrange("p (t d) -> p t d", t=TPG)
    iTokB = Buf("iTok")
    PTa = [arena[:, 8192 + i * 512:8192 + (i + 1) * 512] for i in range(2)]
    sg = arena[:, 9216:11264].rearrange("p (t d) -> p t d", t=TPG)
    sgB = Buf("sg")
    mixBs.extend([iTokB, sgB])
    ropeT = sb("ropeT", [128, 2, G], F32)
    ropeB = Buf("rope")
    kaT = sb("kaT", [128, 2 * G], BF16)
    kaTB = [Buf("kaT%d" % i) for i in range(2)]
    Va = sb("Va", [128, 8, 2, 65], BF16)
    VaB = [Buf("Va%d" % i) for i in range(2)]
    esink = sb("esink", [128, 8], F32)
    lbv = sb("lbv", [128, 8], F32)
    evB = Buf("evconst")
    mcb = sb("mcb", [128, 128], BF16)
    m0b = sb("m0b", [128, 128], BF16)
    ones = sb("ones", [128, 128], F32)
    htmp = [sb("htmp%d" % i, [128, G], F32) for i in range(4)]
    htmpB = [Buf("htmp%d" % i) for i in range(4)]
    mfull = sb("mfull", [128, 2, 2, 128], BF16)
    qdT = [sb("qdT%d" % i, [128, G], BF16) for i in range(2)]
    qdTB = [Buf("qdT%d" % i) for i in range(2)]
    kdT = [sb("kdT%d" % i, [128, G], BF16) for i in range(2)]
    kdTB = [Buf("kdT%d" % i) for i in range(2)]
    kdTok = [sb("kdTok%d" % i, [128, TPG, 128], BF16) for i in range(2)]
    kdTokB = [Buf("kdTok%d" % i) for i in range(2)]
    sTm = [sb("sTm%d" % i, [128, 128], BF16) for i in range(4)]
    sTmB = [Buf("sTm%d" % i) for i in range(4)]
    state = sb("state", [128, 4, 128], F32)
    stS = sb("stS", [128, 4, 128], BF16)
    stateB = [Buf("state%d" % i) for i in range(4)]
    stSB = [Buf("stS%d" % i) for i in range(4)]
    tmpd = [sb("tmpd%d" % i, [128, 128], F32) for i in range(4)]
    tmpdB = [Buf("tmpd%d" % i) for i in range(4)]
    hvec = [sb("hvec%d" % i, [128, 4, 4], F32) for i in range(2)]
    hvecB = [Buf("hvec%d" % i) for i in range(2)]
    ssh = sb("ssh", [128, 16], F32)
    sshB = [Buf("ssh%d" % i) for i in range(16)]
    rsh = sb("rsh", [128, 16], F32)
    rshB = Buf("rsh")
    tmpo = sb("tmpo", [128, TPG, D], F32)
    tmpoB = [Buf("tmpo%d" % t) for t in range(TPG)]
    junks = [sb("junk%d" % i, [128, D], BF16) for i in range(2)]
    junkBs = [Buf("junk%d" % i) for i in range(2)]
    k.junk_i = 0

    def next_junk():
        k.junk_i += 1
        return junks[k.junk_i % 2], junkBs[k.junk_i % 2]
    csb = [sb("csb%d" % i, [128, G], F32) for i in range(2)]
    csbB = [Buf("csb%d" % i) for i in range(2)]
    gsb = [sb("gsb%d" % i, [128, G], F32) for i in range(2)]
    gsbB = [Buf("gsb%d" % i) for i in range(2)]
    carry = [sb("carry%d" % L, [128, FC, 2], F32) for L in range(2)]
    carryB = [Buf("carry%d" % L) for L in range(2)]
    ss4 = sb("ss4", [128, 4], F32)
    ss4B = [Buf("ss4_%d" % t) for t in range(TPG)]
    ssp = sb("ssp", [128, 4, 2], F32)
    sspB = [Buf("ssp%d" % t) for t in range(TPG)]
    rstd = sb("rstd", [128, 4], F32)
    rstdB = [Buf("rstd%d" % t) for t in range(TPG)]
    psum = [nc.alloc_psum_tensor("ps%d" % i, [128, 512], F32) for i in range(8)]
    psumB = [Buf("ps%d" % i, excl=True) for i in range(8)]
    k.bank_i = 0
    k.bank_n = 8

    def bank():
        i = k.bank_i % k.bank_n
        k.bank_i += 1
        return psum[i], psumB[i]

    def cs(name, a=None, b=None):
        o, w = lay[name]
        if a is None:
            return cst[:, o:o + w]
        return cst[:, o + a:o + b]

    S.dma("sp", "cst", lambda e: e.dma_start(out=cst[:], in_=cst_d), writes=[cstB])
    S.op("dve", lambda e: e.tensor_copy(out=ident[:], in_=cs("ident")), reads=[cstB], writes=[identB])
    if 1 in layers and "mixer" in parts:
        S.dma("sp", "relb", lambda e: e.dma_start(out=tmpo[:].rearrange("p t d -> p (t d)"), in_=relb_d), writes=tmpoB)
        S.op("pool", lambda e: e.memset(Vr[:], 1.0), writes=VB)
        S.op("act", lambda e: e.activation(out=m4[:], in_=cs("m4"), func=AF.Exp), reads=[cstB], writes=[m4B])
        rb = tmpo[:].rearrange("p t d -> p (t d)").rearrange("p (h k q) -> p h k q", h=16, k=2)
        for hh in range(16):
            S.op("dve", lambda e, hh=hh: e.scalar_tensor_tensor(out=rb[:, hh, 0, :], in0=rb[:, hh, 0, :], scalar=cs("crep", hh, hh + 1), op0=ALU.subtract, in1=cs("m0"), op1=ALU.add),
                 reads=tmpoB + [cstB], writes=tmpoB)
            S.op("dve", lambda e, hh=hh: e.tensor_scalar(out=rb[:, hh, 1, :], in0=rb[:, hh, 1, :], scalar1=cs("crep", hh, hh + 1), scalar2=None, op0=ALU.subtract),
                 reads=tmpoB + [cstB], writes=tmpoB)
            S.op("act", lambda e, hh=hh: e.activation(out=bt[:, hh, :, :], in_=rb[:, hh, :, :], func=AF.Exp), reads=tmpoB, writes=[btB])
    if 0 in layers and "mixer" in parts:
        S.op("pool", lambda e: e.memset(Va[:], 1.0), writes=VaB)
        S.op("pool", lambda e: e.memset(ones[:], 1.0), writes=[evB])
        S.op("dve", lambda e: e.tensor_copy(out=mcb[:], in_=cs("mc")), reads=[cstB], writes=[evB])
        if not (1 in layers and "mixer" in parts):
            S.op("act", lambda e: e.activation(out=m4[:], in_=cs("m4"), func=AF.Exp), reads=[cstB], writes=[m4B])
        S.op("act", lambda e: e.activation(out=m0b[:], in_=cs("m0"), func=AF.Exp), reads=[cstB], writes=[evB])
        for e_ in range(2):
            S.op("dve", lambda e, e_=e_: e.tensor_copy(out=mfull[:, e_, 0, :], in_=m4[:]), reads=[m4B], writes=[evB])
            S.op("dve", lambda e, e_=e_: e.tensor_copy(out=mfull[:, e_, 1, :], in_=m0b[:]), reads=[evB], writes=[evB])
        S.op("act", lambda e: e.activation(out=esink[:], in_=cs("sinks"), func=AF.Exp), reads=[cstB], writes=[evB])
        S.op("dve", lambda e: e.tensor_tensor(out=lbv[:, 4:8], in0=cs("lbl", 0, 4), in1=cs("lbl", 4, 8), op=ALU.subtract), reads=[cstB], writes=[evB])
        S.op("act", lambda e: e.activation(out=lbv[:, 0:4], in_=lbv[:, 4:8], func=AF.Sigmoid), reads=[evB], writes=[evB])
        S.op("dve", lambda e: e.tensor_scalar(out=lbv[:, 4:8], in0=lbv[:, 0:4], scalar1=-1.0, scalar2=1.0, op0=ALU.mult, op1=ALU.add), reads=[evB], writes=[evB])
    cvB = {}
    for L in layers:
        for (n, nco) in layer_piece_list(L):
            sec = n[:2]
            key = "cv%d%s" % (L, sec)
            cvB.setdefault(key, Buf(key))
            o, _ = ptab[(L, n)]
            r0, r1 = o // 2048, (o + 128 * nco) // 2048
            S.dma("pool", key, lambda e, r0=r0, r1=r1: e.dma_start(out=wbf_d[r0:r1, :], in_=wsrc_d[r0:r1, :]),
                  writes=[cvB[key]])

    order = []
    for s in range(nseq):
        for g in range(NG):
            for L in layers:
                for (n, nco) in layer_piece_list(L):
                    if (n[:2] in ("up", "dn")) and "ffn" not in parts:
                        continue
                    if (n[:2] not in ("up", "dn")) and "mixer" not in parts:
                        continue
                    order.append((L, n))
    k.w_issue = 0
    k.w_use = 0

    def w_issue_upto(i):
        while k.w_issue <= i and k.w_issue < len(order):
            j = k.w_issue
            L, n = order[j]
            o, nco = ptab[(L, n)]
            slot = j % NSLOT
            key = "cv%d%s" % (L, n[:2])
            src = wbf_d.rearrange("r c -> (r c)")[o:o + 128 * nco].rearrange("(p c) -> p c", p=128)
            S.dma("sp", "w%d" % slot, lambda e, slot=slot, nco=nco, src=src: e.dma_start(out=ring[slot][:, 0:nco], in_=src),
                  reads=[cvB[key]], writes=[ringB[slot]])
            k.w_issue += 1

    def w_get(L, n):
        i = k.w_use
        assert order[i] == (L, n), (order[i], L, n)
        w_issue_upto(i + NSLOT - 1)
        k.w_use += 1
        return ring[i % NSLOT], ringB[i % NSLOT]

    def rstd_tile(t):
        S.op("act", lambda e: e.activation(out=rstd[:, t:t + 1], in_=ss4[:, t:t + 1], func=AF.Sqrt, scale=1.0 / D, bias=EPS),
             reads=[ss4B[t]], writes=[rstdB[t]])
        S.op("dve", lambda e: e.reciprocal(out=rstd[:, t:t + 1], in_=rstd[:, t:t + 1]), reads=[rstdB[t]], writes=[rstdB[t]])

    def norm_tile(hb, hbB, nwT, t):
        junk, junkB = next_junk()
        S.op("act", lambda e: e.activation(out=junk[:], in_=hb[:, t, :], func=AF.Square, accum_out=ss4[:, t:t + 1]),
             reads=[hbB[t]], writes=[ss4B[t], junkB])
        rstd_tile(t)
        y, yB = yn[t % 2], ynB[t % 2]
        S.op("dve", lambda e: e.tensor_scalar(out=y[:], in0=hb[:, t, :], scalar1=rstd[:, t:t + 1], scalar2=None, op0=ALU.mult),
             reads=[hbB[t], rstdB[t]], writes=[yB])
        p, pB = bank()
        pb = p[:].bitcast(BF16)
        for kc in range(KC):
            S.op("pe", lambda e, kc=kc: e.transpose(out=pb[:, kc * 128:(kc + 1) * 128], in_=y[:, kc * 128:(kc + 1) * 128], identity=ident[:]),
                 reads=[yB, identB], writes=[pB], inc=(kc == KC - 1))
        S.op("dve", lambda e: e.tensor_tensor(out=yT[:, :, t * 128:(t + 1) * 128], in0=pb.rearrange("p (k t) -> p k t", k=KC),
                                              in1=nwT.unsqueeze(2).broadcast_to([128, KC, 128]), op=ALU.mult),
             reads=[pB, cstB], writes=[yTB[t]])

    def norm_transpose(hb, hbB, nwT):
        for t in range(TPG):
            norm_tile(hb, hbB, nwT, t)

    def post_tile(hb, hbB, t):
        S.op("dve", lambda e: e.tensor_tensor(out=ss4[:, t:t + 1], in0=ssp[:, t, 0:1], in1=ssp[:, t, 1:2], op=ALU.add), reads=[sspB[t]], writes=[ss4B[t]])
        rstd_tile(t)
        S.op("dve", lambda e: e.scalar_tensor_tensor(out=hb[:, t, :], in0=tmpo[:, t, :], scalar=rstd[:, t:t + 1], op0=ALU.mult, in1=hb[:, t, :], op1=ALU.add),
             reads=[tmpoB[t], rstdB[t], hbB[t]], writes=[hbB[t]])

    def post_norm_residual(hb, hbB, nwR_name, produce, stage=9):
        for cg in range(2):
            banks = produce(cg)
            for t in range(TPG):
                p, pB = banks[t]
                junk, junkB = next_junk()
                S.op("act", lambda e, t=t, cg=cg, p=p, junk=junk: e.activation(out=junk[:, 0:512], in_=p[:], func=AF.Square, accum_out=ssp[:, t, cg:cg + 1]),
                     reads=[pB], writes=[sspB[t], junkB])
                S.op("dve", lambda e, t=t, cg=cg, p=p: e.tensor_tensor(out=tmpo[:, t, cg * 512:(cg + 1) * 512], in0=p[:], in1=cs(nwR_name, cg * 512, (cg + 1) * 512), op=ALU.mult),
                     reads=[pB, cstB], writes=[tmpoB[t]])
                if cg == 1:
                    post_tile(hb, hbB, t)

    def evac(i, out, in_, reads, writes, scale=None):
        if i % 2 == 0:
            if scale is None:
                S.op("act", lambda e: e.activation(out=out, in_=in_, func=AF.Identity), reads=reads, writes=writes)
            else:
                S.op("act", lambda e: e.activation(out=out, in_=in_, func=AF.Identity, scale=scale), reads=reads, writes=writes)
        else:
            if scale is None:
                S.op("dve", lambda e: e.tensor_copy(out=out, in_=in_), reads=reads, writes=writes)
            else:
                S.op("dve", lambda e: e.tensor_scalar(out=out, in0=in_, scalar1=scale, scalar2=None, op0=ALU.mult), reads=reads, writes=writes)

    def proj_fm(wv, wB, c, dst, dstB, ei, scale=None):
        p, pB = bank()
        for kc in range(KC):
            S.op("pe", lambda e, kc=kc: e.matmul(p[:], lhsT=wv[:, kc, c * 128:(c + 1) * 128], rhs=yT[:, kc, :], start=(kc == 0), stop=(kc == KC - 1)),
                 reads=[wB] + yTB, writes=[pB], inc=(kc == KC - 1))
        evac(ei, dst, p[:], [pB], [dstB], scale)

    def otok_to_yT(qt):
        p, pB = bank()
        pb = p[:].bitcast(BF16)
        for kc in range(KC):
            S.op("pe", lambda e, kc=kc: e.transpose(out=pb[:, kc * 128:(kc + 1) * 128], in_=otok[:, qt, kc * 128:(kc + 1) * 128], identity=ident[:]),
                 reads=[otokB, identB], writes=[pB], inc=(kc == KC - 1))
        evac(qt, yT[:, :, qt * 128:(qt + 1) * 128], pb.rearrange("p (k t) -> p k t", k=KC), [pB], [yTB[qt]])

    def out_proj_post(L, hb, hbB, nwR_name):
        def produce(cg):
            wt, wB = w_get(L, "ow%d" % cg)
            wv = wt[:, 0:4096].rearrange("p (k c) -> p k c", k=KC)
            banks = [bank() for _ in range(TPG)]
            for t in range(TPG):
                p, pB = banks[t]
                for kc in range(KC):
                    S.op("pe", lambda e, t=t, kc=kc, p=p: e.matmul(p[:], lhsT=yT[:, kc, t * 128:(t + 1) * 128], rhs=wv[:, kc, :], start=(kc == 0), stop=(kc == KC - 1)),
                         reads=[wB, yTB[t]], writes=[pB], inc=(kc == KC - 1))
            return banks
        post_norm_residual(hb, hbB, nwR_name, produce)

    def odd_mixer(L, hb, hbB, g):
        alias_fence(mixBs, [gTB])
        norm_transpose(hb, hbB, cs("nwT%d_0" % L))
        ks = g % 2
        ei = 0
        for i in range(2):
            wt, wB = w_get(L, "ik%d" % i)
            wv = wt[:, 0:4096].rearrange("p (k c) -> p k c", k=KC)
            for c in range(4):
                proj_fm(wv, wB, c, kT[:, 4 * i + c, ks * G:(ks + 1) * G], kTB[ks], ei)
                ei += 1
        for i in range(2):
            wt, wB = w_get(L, "iv%d" % i)
            wv = wt[:, 0:4096].rearrange("p (k c) -> p k c", k=KC)
            for t in range(TPG):
                p, pB = bank()
                for kc in range(KC):
                    S.op("pe", lambda e, t=t, kc=kc, p=p, wv=wv: e.matmul(p[:], lhsT=yT[:, kc, t * 128:(t + 1) * 128], rhs=wv[:, kc, :], start=(kc == 0), stop=(kc == KC - 1)),
                         reads=[wB, yTB[t]], writes=[pB], inc=(kc == KC - 1))
                evac(ei, Vr[:, ks * 4 + t, 8 * i:8 * i + 8, 0:64], p[:].rearrange("p (h d) -> p h d", d=64), [pB], [VB[ks]])
                ei += 1
        for i in range(2):
            wt, wB = w_get(L, "iq%d" % i)
            wv = wt[:, 0:4096].rearrange("p (k c) -> p k c", k=KC)
            for c in range(4):
                proj_fm(wv, wB, c, qT[:, 4 * i + c, :], qTB, ei, scale=0.125)
                ei += 1
        k.bank_n = 5
        alias_fence(PTB3f, PTB)
        units = [(qt, hp) for qt in range(TPG) for hp in range(8)]
        U = {}

        def st_phase(u):
            qt, hp = units[u]
            T = g * 4 + qt
            kts = [kt for kt in range(T - 4, T + 1) if kt >= 0]
            sbk = [bank(), bank(), bank()]
            pt, ptBs = PT[u % 2], PTB3[u % 2]
            stops = []
            fixes = []
            seq_ = [(kt, 0) for kt in kts] + [(None, None)] + [(kt, 1) for kt in kts]
            for (kt, e_) in seq_:
                if kt is None:
                    stops.append((lambda e, sbk=sbk: e.matmul(sbk[2][0][:, 256:258], lhsT=ident[:], rhs=ident[:, 0:2], start=True, stop=True),
                                  [identB], [sbk[2][1]]))
                    continue
                hh = 2 * hp + e_
                lo, hi = e_ * 64, (e_ + 1) * 64
                j = kt - (T - 4)
                if j < 4:
                    reg, regB = sbk[e_][0][:, j * 128:(j + 1) * 128], sbk[e_][1]
                    pcol, part = e_ * 512 + j * 128, e_
                elif len(kts) == 1 and e_ == 1:
                    reg, regB = sbk[1][0][:, 0:128], sbk[1][1]
                    pcol, part = 1024 + e_ * 128, 2
                else:
                    reg, regB = sbk[2][0][:, e_ * 128:(e_ + 1) * 128], sbk[2][1]
                    pcol, part = 1024 + e_ * 128, 2
                kslot = (kt // 4) % 2
                k0 = kslot * G + (kt % 4) * 128
                delta = T - kt
                stops.append((lambda e, reg=reg, lo=lo, hi=hi, k0=k0, hp=hp, qt=qt: e.matmul(reg, lhsT=kT[lo:hi, hp, k0:k0 + 128], rhs=qT[lo:hi, hp, qt * 128:(qt + 1) * 128], start=True, stop=True),
                              [kTB[kslot], qTB], [regB]))
                if delta in (0, 1, 4):
                    fac = bt[:, hh, 0, :] if delta == 0 else (bt[:, hh, 1, :] if delta == 1 else m4[:])
                    fixes.append((pcol, part, fac))
            for si, (fn_, rd_, wr_) in enumerate(stops):
                S.op("pe", fn_, reads=rd_, writes=wr_, inc=(si == len(stops) - 1))
            U[u] = dict(sbk=sbk, pt=pt, ptBs=ptBs, fixes=fixes, kts=kts, T=T)


        def ex_phase(u):
            d = U[u]
            sbk, pt, ptBs, kts = d["sbk"], d["pt"], d["ptBs"], d["kts"]
            c0 = (5 - len(kts)) * 128
            if c0 < 512:
                for e_ in range(2):
                    S.op("act", lambda e, e_=e_: e.activation(out=pt[:, e_ * 512 + c0:(e_ + 1) * 512], in_=sbk[e_][0][:, c0:512], func=AF.Exp),
                         reads=[sbk[e_][1]], writes=[ptBs[e_]])
            if len(kts) == 1:
                S.op("act", lambda e: e.activation(out=pt[:, 1024:1152], in_=sbk[2][0][:, 0:128], func=AF.Exp), reads=[sbk[2][1]], writes=[ptBs[2]])
                S.op("act", lambda e: e.activation(out=pt[:, 1152:1280], in_=sbk[1][0][:, 0:128], func=AF.Exp), reads=[sbk[1][1]], writes=[ptBs[2]])
            else:
                S.op("act", lambda e: e.activation(out=pt[:, 1024:1280], in_=sbk[2][0][:, 0:256], func=AF.Exp), reads=[sbk[2][1]], writes=[ptBs[2]])
            for (pcol, part, fac) in d["fixes"]:
                S.op("dve", lambda e, pcol=pcol, fac=fac: e.tensor_tensor(out=pt[:, pcol:pcol + 128], in0=pt[:, pcol:pcol + 128], in1=fac, op=ALU.mult),
                     reads=[ptBs[part], btB, m4B], writes=[ptBs[part]])

        def pv_phase(u):
            d = U[u]
            pt, ptBs, kts, T = d["pt"], d["ptBs"], d["kts"], d["T"]
            qt, hp = units[u]
            for e_ in range(2):
                hh = 2 * hp + e_
                ob, obB = psum[5 + hh // 6], psumB[5 + hh // 6]
                col = (hh % 6) * 65
                for idx, kt in enumerate(kts):
                    j = kt - (T - 4)
                    pc = pt[:, e_ * 512 + j * 128:e_ * 512 + (j + 1) * 128] if j < 4 else pt[:, 1024 + e_ * 128:1024 + (e_ + 1) * 128]
                    last = (idx == len(kts) - 1)
                    S.op("pe", lambda e, pc=pc, kt=kt, hh=hh, idx=idx, last=last, ob=ob, col=col: e.matmul(ob[:, col:col + 65], lhsT=pc, rhs=Vr[:, kt % 8, hh, :], start=(idx == 0), stop=last),
                         reads=[ptBs[e_], ptBs[2], VB[(kt // 4) % 2]], writes=[obB], inc=(last and e_ == 1))

        def fin_qt(qt):
            for b in range(3):
                nh = 6 if b < 2 else 4
                ob, obB = psum[5 + b], psumB[5 + b]
                view = ob[:, 0:nh * 65].rearrange("p (h c) -> p h c", c=65)
                S.op("dve", lambda e, view=view, nh=nh: e.reciprocal(out=rden[:, 0:nh], in_=view[:, :, 64]), reads=[obB], writes=[rdenB])
                S.op("dve", lambda e, view=view, nh=nh, b=b: e.tensor_tensor(out=otok[:, qt, b * 384:b * 384 + nh * 64].rearrange("p (h d) -> p h d", d=64),
                                                                            in0=view[:, :, 0:64], in1=rden[:, 0:nh].unsqueeze(2).broadcast_to([128, nh, 64]), op=ALU.mult),
                     reads=[obB, rdenB], writes=[otokB])
            otok_to_yT(qt)

        st_phase(0)
        for u in range(len(units)):
            ex_phase(u)
            if u + 1 < len(units):
                st_phase(u + 1)
            pv_phase(u)
            if units[u][1] == 7:
                fin_qt(units[u][0])
        k.bank_n = 8
        out_proj_post(L, hb, hbB, "nwR%d_1" % L)

    def mm_group(p, pB, lhs_fn, rhs_fn, reads, n=KC):
        for kc in range(n):
            S.op("pe", lambda e, kc=kc: e.matmul(p, lhsT=lhs_fn(kc), rhs=rhs_fn(kc), start=(kc == 0), stop=(kc == n - 1)),
                 reads=reads, writes=[pB], inc=(kc == n - 1))

    def rope_chunk(wv, wB, c, dst, dstB):
        pa, paB = bank()
        mm_group(pa[:], paB, lambda kc: wv[:, kc, c * 128:(c + 1) * 128], lambda kc: yT[:, kc, :], [wB] + yTB)
        pr, prB = bank()
        mm_group(pr[:], prB, lambda kc: wv[:, kc, (c + 1) * 128:(c + 2) * 128], lambda kc: yT[:, kc, :], [wB] + yTB)
        t1, t1B = csb[0], csbB[0]
        t2, t2B = csb[1], csbB[1]
        S.op("dve", lambda e: e.tensor_tensor(out=t1[:], in0=pa[:], in1=ropeT[:, 0, :], op=ALU.mult), reads=[paB, ropeB], writes=[t1B])
        S.op("dve", lambda e: e.tensor_tensor(out=t2[:], in0=pr[:], in1=ropeT[:, 1, :], op=ALU.mult), reads=[prB, ropeB], writes=[t2B])
        S.op("pool", lambda e: e.tensor_tensor(out=dst, in0=t1[:], in1=t2[:], op=ALU.add), reads=[t1B, t2B], writes=[dstB])

    def hgrn_head(hd, wvq, wBq, cq, wvf, wBf, cf, g, first):
        i2 = hd % 2
        pq, pqB = bank()
        mm_group(pq[:], pqB, lambda kc: wvq[:, kc, cq * 128:(cq + 1) * 128], lambda kc: yT[:, kc, :], [wBq] + yTB)
        pf, pfB = bank()
        mm_group(pf[:], pfB, lambda kc: wvf[:, kc, cf * 128:(cf + 1) * 128], lambda kc: yT[:, kc, :], [wBf] + yTB)
        yield
        if i2 == 0:
            fa, faB = csb[0], csbB[0]
            lc, lcB = csb[1], csbB[1]
            e1, e1B = gsb[0], gsbB[0]
            e2, e2B = gsb[1], gsbB[1]
        else:
            fa, faB = htmp[0], htmpB[0]
            lc, lcB = htmp[1], htmpB[1]
            e1, e1B = htmp[2], htmpB[2]
            e2, e2B = htmp[3], htmpB[3]
        hv, hvB = hvec[i2], hvecB[i2]
        S.op("act", lambda e: e.activation(out=fa[:], in_=pf[:], func=AF.Sigmoid), reads=[pfB], writes=[faB])
        yield
        S.op("dve", lambda e: e.tensor_scalar(out=fa[:], in0=fa[:], scalar1=lbv[:, 4 + hd:5 + hd], scalar2=lbv[:, hd:hd + 1], op0=ALU.mult, op1=ALU.add),
             reads=[faB, evB], writes=[faB])
        yield
        S.op("act", lambda e: e.activation(out=e1[:], in_=fa[:], func=AF.Ln), reads=[faB], writes=[e1B])
        yield
        for t in range(TPG):
            S.op("dve", lambda e, t=t: e.tensor_tensor_scan(out=lc[:, t * 128:(t + 1) * 128], data0=ones[:], data1=e1[:, t * 128:(t + 1) * 128], initial=0.0, op0=ALU.mult, op1=ALU.add),
                 reads=[e1B, evB], writes=[lcB])
        yield
        lc3 = lc[:].rearrange("p (t m) -> p t m", t=TPG)
        S.op("dve", lambda e: e.tensor_copy(out=hv[:, :, 0], in_=lc3[:, :, 63]), reads=[lcB], writes=[hvB])
        S.op("act", lambda e: e.activation(out=hv[:, :, 1], in_=lc3[:, :, 63], func=AF.Exp), reads=[lcB], writes=[hvB])
        S.op("act", lambda e: e.activation(out=hv[:, :, 2], in_=lc3[:, :, 127], func=AF.Exp), reads=[lcB], writes=[hvB])
        yield
        S.op("dve", lambda e: e.tensor_tensor(out=lc3, in0=lc3, in1=hv[:, :, 0:1].broadcast_to([128, TPG, 128]), op=ALU.subtract), reads=[lcB, hvB], writes=[lcB])
        yield
        S.op("act", lambda e: e.activation(out=hv[:, :, 3], in_=lc3[:, :, 127], func=AF.Exp), reads=[lcB], writes=[hvB])
        S.op("act", lambda e: e.activation(out=e1[:], in_=lc[:], func=AF.Exp), reads=[lcB], writes=[e1B])
        S.op("act", lambda e: e.activation(out=e2[:], in_=lc[:], func=AF.Exp, scale=-1.0), reads=[lcB], writes=[e2B])
        yield
        qd, qdB = qdT[i2], qdTB[i2]
        kd, kdB = kdT[i2], kdTB[i2]
        S.op("dve", lambda e: e.tensor_tensor(out=qd[:], in0=pq[:], in1=e1[:], op=ALU.mult), reads=[pqB, e1B], writes=[qdB])
        S.op("act", lambda e: e.activation(out=fa[:], in_=pf[:], func=AF.Sigmoid, scale=-1.0), reads=[pfB, faB], writes=[faB])
        S.op("dve", lambda e: e.scalar_tensor_tensor(out=kd[:], in0=fa[:], scalar=lbv[:, 4 + hd:5 + hd], op0=ALU.mult, in1=e2[:], op1=ALU.mult), reads=[faB, e2B, evB], writes=[kdB])
        yield
        kk, kkB = kdTok[i2], kdTokB[i2]
        pt_, ptB_ = bank()
        ptb = pt_[:].bitcast(BF16)
        for t in range(TPG):
            S.op("pe", lambda e, t=t: e.transpose(out=ptb[:, t * 128:(t + 1) * 128], in_=kd[:, t * 128:(t + 1) * 128], identity=ident[:]),
                 reads=[kdB, identB], writes=[ptB_], inc=(t == TPG - 1))
        evac(hd, kk[:].rearrange("p t d -> p (t d)"), ptb[:, 0:512], [ptB_], [kkB])

    def hgrn_tile(hd, t, g, first):
        i2 = hd % 2
        hv, hvB = hvec[i2], hvecB[i2]
        qd, qdB = qdT[i2], qdTB[i2]
        kd, kdB = kdT[i2], kdTB[i2]
        kk, kkB = kdTok[i2], kdTokB[i2]
        tile0 = first and t == 0
        sl = slice(t * 128, (t + 1) * 128)
        vv = iTok[:, t, hd * 128:(hd + 1) * 128]
        bi = i2 * 2 + (t % 2)
        ps_, psB_ = bank()
        S.op("pe", lambda e: e.matmul(ps_[:, 0:128], lhsT=kd[:, sl], rhs=qd[:, sl], start=True, stop=True), reads=[kdB, qdB], writes=[psB_])
        sm, smB = sTm[bi], sTmB[bi]
        S.op("dve", lambda e: e.tensor_tensor(out=sm[:], in0=ps_[:, 0:128], in1=mcb[:], op=ALU.mult), reads=[psB_, evB], writes=[smB])
        if not tile0:
            S.op("act", lambda e: e.activation(out=stS[:, hd, :], in_=state[:, hd, :], func=AF.Identity, scale=hv[:, t, 1:2]),
                 reads=[stateB[hd], hvB], writes=[stSB[hd]])
        po, poB = bank()
        S.op("pe", lambda e: e.matmul(po[:, 0:128], lhsT=sm[:], rhs=vv, start=True, stop=tile0), reads=[smB, iTokB], writes=[poB], inc=tile0)
        if not tile0:
            S.op("pe", lambda e: e.matmul(po[:, 0:128], lhsT=qd[:, sl], rhs=stS[:, hd, :], start=False, stop=True), reads=[qdB, stSB[hd]], writes=[poB])
        if not (g == NG - 1 and t == TPG - 1):
            pd, pdB = bank()
            S.op("pe", lambda e: e.matmul(pd[:, 0:128], lhsT=kk[:, t, :], rhs=vv, start=True, stop=True), reads=[kkB, iTokB], writes=[pdB])
            if tile0:
                S.op("dve", lambda e: e.tensor_scalar(out=state[:, hd, :], in0=pd[:, 0:128], scalar1=hv[:, t, 3:4], scalar2=None, op0=ALU.mult),
                     reads=[pdB, hvB], writes=[stateB[hd]])
            else:
                td_, tdB_ = tmpd[bi], tmpdB[bi]
                S.op("dve", lambda e: e.tensor_scalar(out=td_[:], in0=pd[:, 0:128], scalar1=hv[:, t, 3:4], scalar2=None, op0=ALU.mult),
                     reads=[pdB, hvB], writes=[tdB_])
                S.op("dve", lambda e: e.scalar_tensor_tensor(out=state[:, hd, :], in0=state[:, hd, :], scalar=hv[:, t, 2:3], op0=ALU.mult, in1=td_[:], op1=ALU.add),
                     reads=[tdB_, hvB, stateB[hd]], writes=[stateB[hd]])
        junk, junkB = next_junk()
        S.op("act", lambda e: e.activation(out=junk[:, 0:128], in_=po[:, 0:128], func=AF.Square, accum_out=ssh[:, t * 4 + hd:t * 4 + hd + 1]),
             reads=[poB], writes=[sshB[t * 4 + hd], junkB])
        S.op("dve", lambda e: e.tensor_copy(out=tmpo[:, t, hd * 128:(hd + 1) * 128], in_=po[:, 0:128]), reads=[poB], writes=[tmpoB[t]])

    def even_mixer(L, hb, hbB, g):
        first = (g == 0)
        alias_fence(mixBs, [gTB])
        r0 = g * G
        S.dma("sp", "rope", lambda e: e.dma_start(out=ropeT[:], in_=rope_d[:, :, r0:r0 + G]), writes=[ropeB])
        norm_transpose(hb, hbB, cs("nwT%d_0" % L))
        ks = g % 2
        def piece(i):
            wt, wB = w_get(L, "e%d" % i)
            n = len(EVEN_FM[i])
            return wt[:, 0:KC * 128 * n].rearrange("p (k c) -> p k c", k=KC), wB
        wt5, wB5 = w_get(L, "e5")
        w5 = wt5[:, 0:KC * 640].rearrange("p (k c) -> p k c", k=KC)
        for t in range(TPG):
            p, pB = bank()
            mm_group(p[:, 0:128], pB, lambda kc, t=t: yT[:, kc, t * 128:(t + 1) * 128], lambda kc: w5[:, kc, 0:128], [wB5, yTB[t]])
            evac(t, Va[:, ks * 4 + t, :, 0:64], p[:, 0:128].rearrange("p (h d) -> p h d", d=64), [pB], [VaB[ks]])
            p, pB = bank()
            mm_group(p[:], pB, lambda kc, t=t: yT[:, kc, t * 128:(t + 1) * 128], lambda kc: w5[:, kc, 128:640], [wB5, yTB[t]])
            evac(t + 1, iTok[:, t, :], p[:], [pB], [iTokB])
        wt6, wB6 = w_get(L, "e6")
        w6 = wt6[:, 0:KC * 512].rearrange("p (k c) -> p k c", k=KC)
        for t in range(TPG):
            p, pB = bank()
            mm_group(p[:], pB, lambda kc, t=t: yT[:, kc, t * 128:(t + 1) * 128], lambda kc: w6[:, kc, :], [wB6, yTB[t]])
            S.op("act", lambda e, p=p, t=t: e.activation(out=sg[:, t, :], in_=p[:], func=AF.Silu), reads=[pB], writes=[sgB])
            S.op("pool", lambda e, t=t: e.tensor_tensor(out=sg[:, t, :], in0=sg[:, t, :], in1=cs("gw"), op=ALU.mult), reads=[sgB, cstB], writes=[sgB])
        wv, wB = piece(0)
        rope_chunk(wv, wB, 0, kaT[:, ks * G:(ks + 1) * G], kaTB[ks])
        rope_chunk(wv, wB, 2, qaT[:, 0, :], qTB)
        wv, wB = piece(1)
        rope_chunk(wv, wB, 0, qaT[:, 1, :], qTB)
        rope_chunk(wv, wB, 2, qaT[:, 2, :], qTB)
        wv2, wB2 = piece(2)
        rope_chunk(wv2, wB2, 0, qaT[:, 3, :], qTB)
        k.bank_n = 6
        alias_fence(PTB, PTB3f)
        units = [(qt, c) for qt in range(TPG) for c in range(4)]
        U = {}

        def st_phase(u):
            qt, c = units[u]
            T = g * 4 + qt
            kts = [kt for kt in (T - 1, T) if kt >= 0]
            sb2 = [bank(), bank()]
            ops_ = []
            for e_ in range(2):
                if e_ == 1:
                    ops_.append((lambda e, sb2=sb2: e.matmul(sb2[1][0][:, 256:258], lhsT=ident[:], rhs=ident[:, 0:2], start=True, stop=True),
                                 [identB], [sb2[1][1]]))
                for kt in kts:
                    lo, hi = e_ * 64, (e_ + 1) * 64
                    j = kt - (T - 1)
                    reg = sb2[e_][0][:, j * 128:(j + 1) * 128]
                    kslot = (kt // 4) % 2
                    k0 = kslot * G + (kt % 4) * 128
                    ops_.append((lambda e, reg=reg, lo=lo, hi=hi, k0=k0, c=c, qt=qt: e.matmul(reg, lhsT=kaT[lo:hi, k0:k0 + 128], rhs=qaT[lo:hi, c, qt * 128:(qt + 1) * 128], start=True, stop=True),
                                 [kaTB[kslot], qTB], [sb2[e_][1]]))
            for si, (fn_, rd_, wr_) in enumerate(ops_):
                S.op("pe", fn_, reads=rd_, writes=wr_, inc=(si == len(ops_) - 1))
            U[u] = dict(sb2=sb2, kts=kts, T=T)

        def ex_phase(u):
            d = U[u]
            sb2, kts = d["sb2"], d["kts"]
            pt, ptB = PTa[u % 2], PTB[u % 2]
            j0 = 2 - len(kts)
            for e_ in range(2):
                S.op("act", lambda e, e_=e_: e.activation(out=pt[:, e_ * 256 + j0 * 128:(e_ + 1) * 256], in_=sb2[e_][0][:, j0 * 128:256], func=AF.Exp, scale=0.125),
                     reads=[sb2[e_][1]], writes=[ptB])
            S.op("dve", lambda e: e.tensor_tensor(out=pt.rearrange("p (e j q) -> p e j q", e=2, j=2)[:, :, j0:2, :],
                                                  in0=pt.rearrange("p (e j q) -> p e j q", e=2, j=2)[:, :, j0:2, :], in1=mfull[:, :, j0:2, :], op=ALU.mult),
                 reads=[ptB, evB], writes=[ptB])

        def pv_phase(u):
            d = U[u]
            kts, T = d["kts"], d["T"]
            qt, c = units[u]
            pt, ptB = PTa[u % 2], PTB[u % 2]
            for e_ in range(2):
                ob, obB = psum[6 + e_], psumB[6 + e_]
                col = c * 65
                for idx, kt in enumerate(kts):
                    j = kt - (T - 1)
                    pc = pt[:, e_ * 256 + j * 128:e_ * 256 + (j + 1) * 128]
                    last = (idx == len(kts) - 1)
                    S.op("pe", lambda e, pc=pc, kt=kt, e_=e_, idx=idx, last=last, ob=ob, col=col: e.matmul(ob[:, col:col + 65], lhsT=pc, rhs=Va[:, kt % 8, e_, :], start=(idx == 0), stop=last),
                         reads=[ptB, VaB[(kt // 4) % 2]], writes=[obB], inc=(last and e_ == 1))

        def fin_qt(qt):
            for b in range(2):
                ob, obB = psum[6 + b], psumB[6 + b]
                view = ob[:, 0:260].rearrange("p (h c) -> p h c", c=65)
                S.op("dve", lambda e, view=view, b=b: e.tensor_tensor(out=rden[:, 0:4], in0=view[:, :, 64], in1=esink[:, 4 * b:4 * b + 4], op=ALU.add), reads=[obB, evB], writes=[rdenB])
                S.op("dve", lambda e: e.reciprocal(out=rden[:, 0:4], in_=rden[:, 0:4]), reads=[rdenB], writes=[rdenB])
                S.op("dve", lambda e, view=view, b=b: e.tensor_tensor(out=otok[:, qt, b * 256:(b + 1) * 256].rearrange("p (h d) -> p h d", d=64),
                                                                      in0=view[:, :, 0:64], in1=rden[:, 0:4].unsqueeze(2).broadcast_to([128, 4, 64]), op=ALU.mult),
                     reads=[obB, rdenB], writes=[otokB])

        st_phase(0)
        for u in range(len(units)):
            ex_phase(u)
            if u + 1 < len(units):
                st_phase(u + 1)
            pv_phase(u)
            if units[u][1] == 3:
                fin_qt(units[u][0])
        k.bank_n = 8
        def run_pair(ga, gb):
            alive = [ga, gb]
            while alive:
                for gn in list(alive):
                    try:
                        next(gn)
                    except StopIteration:
                        alive.remove(gn)
        g0 = hgrn_head(0, wv2, wB2, 2, wv2, wB2, 3, g, first)
        next(g0)
        wv3, wB3 = piece(3)
        g1 = hgrn_head(1, wv3, wB3, 0, wv3, wB3, 1, g, first)
        next(g1)
        run_pair(g0, g1)
        for t in range(TPG):
            hgrn_tile(0, t, g, first)
            hgrn_tile(1, t, g, first)
        g2 = hgrn_head(2, wv3, wB3, 2, wv3, wB3, 3, g, first)
        next(g2)
        wv4, wB4 = piece(4)
        g3 = hgrn_head(3, wv4, wB4, 0, wv4, wB4, 1, g, first)
        next(g3)
        run_pair(g2, g3)
        for t in range(TPG):
            hgrn_tile(2, t, g, first)
            hgrn_tile(3, t, g, first)
        S.op("act", lambda e: e.activation(out=rsh[:], in_=ssh[:], func=AF.Sqrt, scale=1.0 / 128, bias=EPS), reads=sshB, writes=[rshB])
        S.op("dve", lambda e: e.reciprocal(out=rsh[:], in_=rsh[:]), reads=[rshB], writes=[rshB])
        for t in range(TPG):
            o3 = tmpo[:, t, 0:512].rearrange("p (h d) -> p h d", h=4)
            S.op("dve", lambda e, t=t, o3=o3: e.tensor_tensor(out=o3, in0=o3, in1=rsh[:, t * 4:(t + 1) * 4].unsqueeze(2).broadcast_to([128, 4, 128]), op=ALU.mult),
                 reads=[tmpoB[t], rshB], writes=[tmpoB[t]])
            S.op("dve", lambda e, t=t: e.tensor_tensor(out=otok[:, t, 512:1024], in0=tmpo[:, t, 0:512], in1=sg[:, t, :], op=ALU.mult), reads=[tmpoB[t], sgB], writes=[otokB])
        for qt in range(TPG):
            otok_to_yT(qt)
        out_proj_post(L, hb, hbB, "nwR%d_1" % L)

    def ffn_block(L, hb, hbB, first, last):
        import os
        stage = int(os.environ.get("FFN_STAGE", "9"))
        alias_fence([gTB], mixBs)
        norm_transpose(hb, hbB, cs("nwT%d_2" % L))
        if stage < 2:
            return
        cw = cs("cw%d" % L).rearrange("p (f j) -> p f j", j=3)
        cb = cs("cb%d" % L)
        cidx = 0
        for j in range(11):
            wt, wB = w_get(L, "up%d" % j)
            wv = wt[:, 0:4096].rearrange("p (k c) -> p k c", k=KC)
            for i in range(2):
                fc = 2 * j + i
                pu, puB = bank()
                for kc in range(KC):
                    S.op("pe", lambda e, kc=kc, i=i, pu=pu, wv=wv: e.matmul(pu[:], lhsT=wv[:, kc, i * 128:(i + 1) * 128], rhs=yT[:, kc, :], start=(kc == 0), stop=(kc == KC - 1)),
                         reads=[wB] + yTB, writes=[puB], inc=(kc == KC - 1))
                pv, pvB = bank()
                for kc in range(KC):
                    S.op("pe", lambda e, kc=kc, i=i, pv=pv, wv=wv: e.matmul(pv[:], lhsT=wv[:, kc, 256 + i * 128:256 + (i + 1) * 128], rhs=yT[:, kc, :], start=(kc == 0), stop=(kc == KC - 1)),
                         reads=[wB] + yTB, writes=[pvB], inc=(kc == KC - 1))
                c, cB = csb[cidx % 2], csbB[cidx % 2]
                gg, gB = gsb[cidx % 2], gsbB[cidx % 2]
                cidx += 1
                if stage < 3:
                    continue
                S.op("act", lambda e, fc=fc, c=c, pu=pu: e.activation(out=c[:], in_=pu[:], func=AF.Identity, scale=cw[:, fc, 2:3], bias=cb[:, fc:fc + 1]),
                     reads=[puB, cstB], writes=[cB])
                S.op("dve", lambda e, fc=fc, c=c, pu=pu: e.scalar_tensor_tensor(out=c[:, 1:G], in0=pu[:, 0:G - 1], scalar=cw[:, fc, 1:2], op0=ALU.mult, in1=c[:, 1:G], op1=ALU.add),
                     reads=[puB, cstB, cB], writes=[cB])
                S.op("dve", lambda e, fc=fc, c=c, pu=pu: e.scalar_tensor_tensor(out=c[:, 2:G], in0=pu[:, 0:G - 2], scalar=cw[:, fc, 0:1], op0=ALU.mult, in1=c[:, 2:G], op1=ALU.add),
                     reads=[puB, cstB, cB], writes=[cB])
                if stage < 4:
                    continue
                if not first:
                    S.op("dve", lambda e, fc=fc, c=c: e.scalar_tensor_tensor(out=c[:, 0:1], in0=carry[L][:, fc, 1:2], scalar=cw[:, fc, 1:2], op0=ALU.mult, in1=c[:, 0:1], op1=ALU.add),
                         reads=[carryB[L], cstB, cB], writes=[cB])
                    S.op("dve", lambda e, fc=fc, c=c: e.scalar_tensor_tensor(out=c[:, 0:2], in0=carry[L][:, fc, 0:2], scalar=cw[:, fc, 0:1], op0=ALU.mult, in1=c[:, 0:2], op1=ALU.add),
                         reads=[carryB[L], cstB, cB], writes=[cB])
                if not last:
                    S.op("dve", lambda e, fc=fc, pu=pu: e.tensor_copy(out=carry[L][:, fc, :], in_=pu[:, G - 2:G]),
                         reads=[puB], writes=[carryB[L]])
                S.op("act", lambda e, c=c, gg=gg: e.activation(out=gg[:], in_=c[:], func=AF.Gelu), reads=[cB], writes=[gB])
                S.op("dve", lambda e, fc=fc, gg=gg, pv=pv: e.tensor_tensor(out=gT[:, fc, :], in0=pv[:], in1=gg[:], op=ALU.mult),
                     reads=[pvB, gB], writes=[gTB])

        if stage < 5:
            for _ in range(4):
                w_get(L, order[k.w_use][1])
            return

        def produce(cg):
            banks = [bank() for _ in range(TPG)]
            for hf in range(2):
                wt, wB = w_get(L, "dn%d_%d" % (cg, hf))
                wv = wt[:, 0:5632].rearrange("p (f c) -> p f c", f=11)
                for t in range(TPG):
                    p, pB = banks[t]
                    for f in range(11):
                        fc = hf * 11 + f
                        S.op("pe", lambda e, t=t, f=f, fc=fc, p=p, wv=wv: e.matmul(p[:], lhsT=gT[:, fc, t * 128:(t + 1) * 128], rhs=wv[:, f, :], start=(fc == 0), stop=(fc == FC - 1)),
                             reads=[wB, gTB], writes=[pB], inc=(f == 10))
            return banks
        post_norm_residual(hb, hbB, "nwR%d_3" % L, produce)

    k.ffn_block = ffn_block
    k.extra = {}

    gi = 0
    groups = [(s_, g_) for s_ in range(nseq) for g_ in range(NG)]

    def load_tile(gidx, t):
        s_, g_ = groups[gidx]
        r0 = s_ * seq + g_ * G + t * 128
        hb_, hbB_ = h[gidx % NH], hB[gidx % NH]
        S.dma("pool", "ldx%d_%d" % (gidx % NH, t), lambda e: e.dma_start(out=hb_[:, t, :], in_=x_d[r0:r0 + 128, :]), writes=[hbB_[t]])

    def store_tile(gidx, t):
        s_, g_ = groups[gidx]
        r0 = s_ * seq + g_ * G + t * 128
        hb_, hbB_ = h[gidx % NH], hB[gidx % NH]
        S.dma("pool", "stx%d_%d" % (gidx % NH, t), lambda e: e.dma_start(out=out_d[r0:r0 + 128, :], in_=hb_[:, t, :]), reads=[hbB_[t]])

    for t in range(TPG):
        load_tile(0, t)
    for gi, (s, g) in enumerate(groups):
        hb, hbB = h[gi % NH], hB[gi % NH]
        for L in layers:
            if "mixer" in parts:
                if L == 1:
                    odd_mixer(L, hb, hbB, g)
                else:
                    even_mixer(L, hb, hbB, g)
            if "ffn" in parts:
                ffn_block(L, hb, hbB, first=(g == 0), last=(g == NG - 1))
        for t in range(TPG):
            store_tile(gi, t)
            if gi + 1 < len(groups):
                load_tile(gi + 1, t)
    S.wait_all("pool", [b for hl in hB for b in hl])
    S.emit()
    return nc


def host_prep(inp):
    return {"wsrc": weights_host(inp).reshape(-1, 2048), "cst": consts_host(inp), "relb": relb_host(inp),
            "rope": rope_host(int(inp["x"].shape[1]))}


_NC_CACHE = {}


def kernel(**inputs):
    inp = {k_: np.asarray(v) for k_, v in inputs.items()}
    x = inp["x"]
    B, Sq, _ = x.shape
    ncores = 8
    nseq = B // ncores
    key = (nseq, Sq)
    if key not in _NC_CACHE:
        _NC_CACHE[key] = build(nseq, Sq)
    nc = _NC_CACHE[key]
    host = host_prep(inp)
    in_maps = []
    for c in range(ncores):
        m = dict(host)
        m["x"] = np.ascontiguousarray(x[c * nseq:(c + 1) * nseq].reshape(nseq * Sq, D))
        in_maps.append(m)
    res = run_bass_kernel_spmd(nc, in_maps, core_ids=list(range(ncores)))
    out = np.stack([np.asarray(r["out"]).reshape(nseq, Sq, D) for r in res.results])
    return out.reshape(B, Sq, D).astype(np.float32)
```

```python
import numpy as np
import concourse.bass as bass
import concourse.mybir as mybir
from concourse.bass_utils import run_bass_kernel_spmd

F32 = mybir.dt.float32
BF16 = mybir.dt.bfloat16
AF = mybir.ActivationFunctionType
ALU = mybir.AluOpType
AX = mybir.AxisListType

ENG_ATTR = {"pe": "tensor", "act": "scalar", "dve": "vector", "pool": "gpsimd", "sp": "sync"}


class Buf:
    __slots__ = ("name", "w", "r", "excl")

    def __init__(self, name, excl=False):
        self.name = name
        self.w = None
        self.r = []
        self.excl = excl


class Sched:
    def __init__(self, nc, same_engine_sync=True):
        self.nc = nc
        self.same = same_engine_sync
        self.ops = {k: [] for k in ENG_ATTR}
        self.cnt = {k: 0 for k in ENG_ATTR}
        self.seen = {k: {} for k in ENG_ATTR}
        self.sems = {}
        self.dma_total = {}
        for k in ENG_ATTR:
            self.sems[k] = nc.alloc_semaphore("s_" + k)

    def dma_sem(self, key):
        if key not in self.sems:
            self.sems[key] = self.nc.alloc_semaphore("d_" + key)
            self.dma_total[key] = 0
        return key

    def _deps(self, eng, reads, writes):
        deps = {}
        for b in reads:
            if b.w is not None:
                k, v = b.w
                deps[k] = max(deps.get(k, 0), v)
            if b.excl:
                for (k, v) in b.r:
                    if k != eng:
                        deps[k] = max(deps.get(k, 0), v)
        for b in writes:
            if b.w is not None:
                k, v = b.w
                deps[k] = max(deps.get(k, 0), v)
            for (k, v) in b.r:
                deps[k] = max(deps.get(k, 0), v)
        seen = self.seen[eng]
        for k, v in deps.items():
            if k == eng and (eng == "pe" or not self.same):
                continue
            if seen.get(k, 0) < v:
                seen[k] = v
                sem = self.sems[k]
                self.ops[eng].append(lambda e, sem=sem, v=v: e.wait_ge(sem, v))

    def op(self, eng, fn, reads=(), writes=(), inc=True):
        self._deps(eng, reads, writes)
        if inc:
            self.cnt[eng] += 1
            tok = (eng, self.cnt[eng])
            sem = self.sems[eng]
            self.ops[eng].append(lambda e, fn=fn, sem=sem: fn(e).then_inc(sem, 1))
        else:
            tok = (eng, self.cnt[eng] + 1)
            self.ops[eng].append(lambda e, fn=fn: fn(e))
        for b in reads:
            b.r.append(tok)
        for b in writes:
            b.w = tok
            b.r = []
        return tok

    def dma(self, eng, semkey, fn, reads=(), writes=()):
        self.dma_sem(semkey)
        self._deps(eng, reads, writes)
        self.dma_total[semkey] += 16
        tok = (semkey, self.dma_total[semkey])
        sem = self.sems[semkey]
        self.ops[eng].append(lambda e, fn=fn, sem=sem: fn(e).then_inc(sem, 16))
        for b in reads:
            b.r.append(tok)
        for b in writes:
            b.w = tok
            b.r = []
        return tok

    def wait_all(self, eng, bufs):
        self._deps(eng, (), bufs)

    def emit(self):
        nc = self.nc
        with nc.Block() as block:
            for k, attr in ENG_ATTR.items():
                ops = self.ops[k]
                if not ops:
                    continue

                def body(e, ops=ops):
                    for f in ops:
                        f(e)
                getattr(block, attr)(body)


D = 1024
KC = 8
DFF = 2816
FC = 22
G = 512
TPG = 4
EPS = 1e-6
SLOT = 5632
NSLOT = 3
NEG = -30000.0


def ffn_piece_list():
    out = [("up%d" % j, 4096) for j in range(11)]
    out += [("dn%d_%d" % (cg, hf), 5632) for cg in range(2) for hf in range(2)]
    return out


def ffn_pieces_host(w_up, w_down):
    res = []
    wu = w_up.reshape(KC, 128, 2 * DFF)
    for j in range(11):
        cols = np.concatenate([np.arange(2 * j * 128, (2 * j + 2) * 128),
                               DFF + np.arange(2 * j * 128, (2 * j + 2) * 128)])
        pc = wu[:, :, cols].transpose(1, 0, 2).reshape(128, KC * 512)
        res.append(("up%d" % j, pc))
    wd = w_down.reshape(FC, 128, D)
    for cg in range(2):
        for hf in range(2):
            pc = wd[hf * 11:(hf + 1) * 11, :, cg * 512:(cg + 1) * 512].transpose(1, 0, 2).reshape(128, 11 * 512)
            res.append(("dn%d_%d" % (cg, hf), pc))
    return res


def const_layout():
    lay = {}
    off = 0

    def add(name, w):
        nonlocal off
        lay[name] = (off, w)
        off += w
    for L in range(2):
        add("nwT%d_0" % L, KC)
        add("nwT%d_2" % L, KC)
        add("nwR%d_1" % L, D)
        add("nwR%d_3" % L, D)
        add("cw%d" % L, FC * 3)
        add("cb%d" % L, FC)
    add("ident", 128)
    add("lbl", 8)
    add("sinks", 8)
    add("gw", 512)
    add("mc", 128)
    add("crep", 16)
    add("m0", 128)
    add("m4", 128)
    lay["_total"] = (off, 0)
    return lay


def consts_host(inp):
    lay = const_layout()
    c = np.zeros((128, lay["_total"][0]), np.float32)

    def put(name, arr):
        o, w = lay[name]
        c[:, o:o + w] = arr.reshape(128, w) if arr.shape[0] == 128 else np.broadcast_to(arr.reshape(1, w), (128, w))
    nw = inp["norm_w"]
    for L in range(2):
        put("nwT%d_0" % L, nw[L, 0].reshape(KC, 128).T)
        put("nwT%d_2" % L, nw[L, 2].reshape(KC, 128).T)
        put("nwR%d_1" % L, nw[L, 1])
        put("nwR%d_3" % L, nw[L, 3])
        cw = inp["ffn_conv_w"][L]
        put("cw%d" % L, cw.reshape(3, FC, 128).transpose(2, 1, 0).reshape(128, FC * 3))
        put("cb%d" % L, inp["ffn_conv_b"][L].reshape(FC, 128).T)
    put("ident", np.eye(128, dtype=np.float32))
    put("lbl", inp["hgrn_lb_logits"][0:2].reshape(2, 4, 128).transpose(2, 0, 1).reshape(128, 8))
    put("sinks", inp["even_sinks"][0])
    put("gw", inp["hgrn_norm_w"][0])
    put("mc", (np.arange(128)[:, None] <= np.arange(128)[None, :]).astype(np.float32))
    put("crep", inp["odd_rel_bias"][0][:, 256])
    jj = np.arange(128)[:, None]
    qq = np.arange(128)[None, :]
    put("m0", np.where((jj >= 64) & (qq < 64), NEG, 0.0).astype(np.float32))
    put("m4", np.where((jj < 64) & (qq >= 64), NEG, 0.0).astype(np.float32))
    return c


def relb_host(inp):
    tab = inp["odd_rel_bias"][0]
    jj = np.arange(128)[:, None]
    qq = np.arange(128)[None, :]
    i0 = (qq - jj) + 128
    i1 = np.minimum(128 + qq - jj, 128) + 128
    b0 = tab[:, i0]
    b1 = tab[:, i1]
    r = np.stack([b0, b1], axis=1)
    return np.ascontiguousarray(r.transpose(2, 0, 1, 3).reshape(128, 16 * 2 * 128).astype(np.float32))


def odd_piece_list():
    return [(n, 4096) for n in ("ik0", "ik1", "iv0", "iv1", "iq0", "iq1", "ow0", "ow1")]


def odd_pieces_host(w_in, w_out):
    wi = w_in.reshape(KC, 128, 3 * D)
    wo = w_out.reshape(KC, 128, D)
    res = []

    def pc(w, c0):
        return w[:, :, c0:c0 + 512].transpose(1, 0, 2).reshape(128, KC * 512)
    for i in range(2):
        res.append(("ik%d" % i, pc(wi, D + i * 512)))
    for i in range(2):
        res.append(("iv%d" % i, pc(wi, 2 * D + i * 512)))
    for i in range(2):
        res.append(("iq%d" % i, pc(wi, i * 512)))
    for i in range(2):
        res.append(("ow%d" % i, pc(wo, i * 512)))
    return res


def layer_piece_list(L):
    if L == 1:
        return odd_piece_list() + ffn_piece_list()
    return even_piece_list() + ffn_piece_list()


EVEN_FM = [["ka", "rka", "qa0", "rqa0"], ["qa1", "rqa1", "qa2", "rqa2"], ["qa3", "rqa3", "qb0", "fb0"],
           ["qb1", "fb1", "qb2", "fb2"], ["qb3", "fb3"]]


def even_piece_list():
    out = [("e5", KC * 640), ("e6", KC * 512)]
    out += [("e%d" % i, KC * 128 * len(ch)) for i, ch in enumerate(EVEN_FM)]
    out += [("ow0", 4096), ("ow1", 4096)]
    return out


def even_cols():
    rot = (np.arange(64) + 32) % 64
    cols = {}
    cols["ka"] = 512 + np.arange(128)
    cols["rka"] = 512 + np.concatenate([rot, 64 + rot])
    for c in range(4):
        h0, h1 = c, 4 + c
        cols["qa%d" % c] = np.concatenate([h0 * 64 + np.arange(64), h1 * 64 + np.arange(64)])
        cols["rqa%d" % c] = np.concatenate([h0 * 64 + rot, h1 * 64 + rot])
        cols["qb%d" % c] = 768 + c * 128 + np.arange(128)
        cols["fb%d" % c] = 1280 + c * 128 + np.arange(128)
    return cols


def even_pieces_host(inp):
    wi = inp["even_w_in"][0].reshape(KC, 128, 2816)
    wo = inp["even_w_out"][0].reshape(KC, 128, D)
    cols = even_cols()
    res = []
    cc = np.concatenate([640 + np.arange(128), 1792 + np.arange(512)])
    res.append(("e5", wi[:, :, cc].transpose(1, 0, 2).reshape(128, KC * 640)))
    cc = 2304 + np.arange(512)
    res.append(("e6", wi[:, :, cc].transpose(1, 0, 2).reshape(128, KC * 512)))
    for i, ch in enumerate(EVEN_FM):
        cc = np.concatenate([cols[n] for n in ch])
        res.append(("e%d" % i, wi[:, :, cc].transpose(1, 0, 2).reshape(128, KC * len(cc))))
    for i in range(2):
        res.append(("ow%d" % i, wo[:, :, i * 512:(i + 1) * 512].transpose(1, 0, 2).reshape(128, KC * 512)))
    return res


def rope_host(seq):
    inv = (10000.0 ** (-np.arange(0, 64, 2, dtype=np.float32) / 64)).astype(np.float32)
    ang = np.arange(seq, dtype=np.float32)[None, :] * inv[:, None]
    cos = np.cos(ang).astype(np.float32)
    sin = np.sin(ang).astype(np.float32)
    c64 = np.concatenate([cos, cos], 0)
    s64 = np.concatenate([-sin, sin], 0)
    t = np.stack([np.concatenate([c64, c64], 0), np.concatenate([s64, s64], 0)], axis=1)
    return np.ascontiguousarray(t.astype(np.float32))


def weights_host(inp):
    chunks = []
    for L in range(2):
        pcs = (odd_pieces_host(inp["odd_w_in"][0], inp["odd_w_out"][0]) if L == 1 else even_pieces_host(inp))
        pcs = pcs + ffn_pieces_host(inp["ffn_w_up"][L], inp["ffn_w_down"][L])
        want = layer_piece_list(L)
        assert [p[0] for p in pcs] == [w[0] for w in want]
        for (n, a), (_, nco) in zip(pcs, want):
            assert a.shape == (128, nco), (n, a.shape, nco)
            chunks.append(np.ascontiguousarray(a, dtype=np.float32).reshape(-1))
    return np.concatenate(chunks)


class K:
    pass


def build(nseq, seq, layers=(0, 1), parts=("mixer", "ffn")):
    nc = bass.Bass("TRN2", target_bir_lowering=False)
    NG = seq // G
    NTOK = nseq * seq
    lay = const_layout()
    NCST = lay["_total"][0]
    ptab = {}
    off = 0
    for L in range(2):
        for (n, nco) in layer_piece_list(L):
            ptab[(L, n)] = (off, nco)
            off += 128 * nco
    WTOT = off

    x_d = nc.dram_tensor("x", [NTOK, D], F32, kind="ExternalInput").ap()
    out_d = nc.dram_tensor("out", [NTOK, D], F32, kind="ExternalOutput").ap()
    wsrc_d = nc.dram_tensor("wsrc", [WTOT // 2048, 2048], F32, kind="ExternalInput").ap()
    cst_d = nc.dram_tensor("cst", [128, NCST], F32, kind="ExternalInput").ap()
    wbf_d = nc.dram_tensor("wbf", [WTOT // 2048, 2048], BF16, kind="Internal").ap()
    relb_d = nc.dram_tensor("relb", [128, 4096], F32, kind="ExternalInput").ap()
    rope_d = nc.dram_tensor("rope", [128, 2, seq], F32, kind="ExternalInput").ap()

    S = Sched(nc)
    k = K()
    k.nc, k.S = nc, S

    def sb(name, shape, dt):
        return nc.alloc_sbuf_tensor("s_" + name, shape, dt)

    cst = sb("cst", [128, NCST], F32)
    cstB = Buf("cst")
    ident = sb("ident", [128, 128], BF16)
    identB = Buf("ident")
    NH = 1
    h = [sb("h%d" % i, [128, TPG, D], F32) for i in range(NH)]
    hB = [[Buf("h%d_%d" % (i, t)) for t in range(TPG)] for i in range(NH)]
    ring = [sb("ring%d" % i, [128, SLOT], BF16) for i in range(NSLOT)]
    ringB = [Buf("ring%d" % i) for i in range(NSLOT)]
    yn = [sb("yn%d" % i, [128, D], BF16) for i in range(2)]
    ynB = [Buf("yn%d" % i) for i in range(2)]
    yT = sb("yT", [128, KC, G], BF16)
    yTB = [Buf("yT%d" % t) for t in range(TPG)]
    arena = sb("arena", [128, FC * G], BF16)
    gT = arena[:].rearrange("p (f t) -> p f t", f=FC)
    gTB = Buf("gT")
    qT = arena[:, 0:4096].rearrange("p (k t) -> p k t", k=KC)
    qTB = Buf("qT")
    otok = arena[:, 4096:8192].rearrange("p (t d) -> p t d", t=TPG)
    otokB = Buf("otok")
    PT = [arena[:, 8192 + i * 1280:8192 + (i + 1) * 1280] for i in range(2)]
    PTB = [Buf("PT%d" % i) for i in range(2)]
    PTB3 = [[Buf("PT%d_%d" % (i, j)) for j in range(3)] for i in range(2)]
    PTB3f = [b for l in PTB3 for b in l]
    mixBs = [qTB, otokB] + PTB + PTB3f

    def alias_fence(dsts, srcs):
        for d_ in dsts:
            for s_ in srcs:
                if s_.w is not None:
                    d_.r.append(s_.w)
                d_.r.extend(s_.r)
    kT = sb("kT", [128, KC, 2 * G], BF16)
    kTB = [Buf("kT%d" % i) for i in range(2)]
    Vr = sb("Vr", [128, 8, 16, 65], BF16)
    VB = [Buf("V%d" % i) for i in range(2)]
    bt = sb("bt", [128, 16, 2, 128], BF16)
    btB = Buf("bt")
    m4 = sb("m4", [128, 128], BF16)
    m4B = Buf("m4")
    rden = sb("rden", [128, 8], F32)
    rdenB = Buf("rden")
    qaT = arena[:, 0:2048].rearrange("p (k t) -> p k t", k=4)
    iTok = arena[:, 2048:4096].rearrange("p (t d) -> p t d", t=TPG)
    iTokB = Buf("iTok")
    PTa = [arena[:, 8192 + i * 512:8192 + (i + 1) * 512] for i in range(2)]
    sg = arena[:, 9216:11264].rearrange("p (t d) -> p t d", t=TPG)
    sgB = Buf("sg")
    mixBs.extend([iTokB, sgB])
    ropeT = sb("ropeT", [128, 2, G], F32)
    ropeB = Buf("rope")
    kaT = sb("kaT", [128, 2 * G], BF16)
    kaTB = [Buf("kaT%d" % i) for i in range(2)]
    Va = sb("Va", [128, 8, 2, 65], BF16)
    VaB = [Buf("Va%d" % i) for i in range(2)]
    esink = sb("esink", [128, 8], F32)
    lbv = sb("lbv", [128, 8], F32)
    evB = Buf("evconst")
    mcb = sb("mcb", [128, 128], BF16)
    m0b = sb("m0b", [128, 128], BF16)
    ones = sb("ones", [128, 128], F32)
    htmp = [sb("htmp%d" % i, [128, G], F32) for i in range(4)]
    htmpB = [Buf("htmp%d" % i) for i in range(4)]
    mfull = sb("mfull", [128, 2, 2, 128], BF16)
    qdT = [sb("qdT%d" % i, [128, G], BF16) for i in range(2)]
    qdTB = [Buf("qdT%d" % i) for i in range(2)]
    kdT = [sb("kdT%d" % i, [128, G], BF16) for i in range(2)]
    kdTB = [Buf("kdT%d" % i) for i in range(2)]
    kdTok = [sb("kdTok%d" % i, [128, TPG, 128], BF16) for i in range(2)]
    kdTokB = [Buf("kdTok%d" % i) for i in range(2)]
    sTm = [sb("sTm%d" % i, [128, 128], BF16) for i in range(4)]
    sTmB = [Buf("sTm%d" % i) for i in range(4)]
    state = sb("state", [128, 4, 128], F32)
    stS = sb("stS", [128, 4, 128], BF16)
    stateB = [Buf("state%d" % i) for i in range(4)]
    stSB = [Buf("stS%d" % i) for i in range(4)]
    tmpd = [sb("tmpd%d" % i, [128, 128], F32) for i in range(4)]
    tmpdB = [Buf("tmpd%d" % i) for i in range(4)]
    hvec = [sb("hvec%d" % i, [128, 4, 4], F32) for i in range(2)]
    hvecB = [Buf("hvec%d" % i) for i in range(2)]
    ssh = sb("ssh", [128, 16], F32)
    sshB = [Buf("ssh%d" % i) for i in range(16)]
    rsh = sb("rsh", [128, 16], F32)
    rshB = Buf("rsh")
    tmpo = sb("tmpo", [128, TPG, D], F32)
    tmpoB = [Buf("tmpo%d" % t) for t in range(TPG)]
    junks = [sb("junk%d" % i, [128, D], BF16) for i in range(2)]
    junkBs = [Buf("junk%d" % i) for i in range(2)]
    k.junk_i = 0

    def next_junk():
        k.junk_i += 1
        return junks[k.junk_i % 2], junkBs[k.junk_i % 2]
    csb = [sb("csb%d" % i, [128, G], F32) for i in range(2)]
    csbB = [Buf("csb%d" % i) for i in range(2)]
    gsb = [sb("gsb%d" % i, [128, G], F32) for i in range(2)]
    gsbB = [Buf("gsb%d" % i) for i in range(2)]
    carry = [sb("carry%d" % L, [128, FC, 2], F32) for L in range(2)]
    carryB = [Buf("carry%d" % L) for L in range(2)]
    ss4 = sb("ss4", [128, 4], F32)
    ss4B = [Buf("ss4_%d" % t) for t in range(TPG)]
    ssp = sb("ssp", [128, 4, 2], F32)
    sspB = [Buf("ssp%d" % t) for t in range(TPG)]
    rstd = sb("rstd", [128, 4], F32)
    rstdB = [Buf("rstd%d" % t) for t in range(TPG)]
    psum = [nc.alloc_psum_tensor("ps%d" % i, [128, 512], F32) for i in range(8)]
    psumB = [Buf("ps%d" % i, excl=True) for i in range(8)]
    k.bank_i = 0
    k.bank_n = 8

    def bank():
        i = k.bank_i % k.bank_n
        k.bank_i += 1
        return psum[i], psumB[i]

    def cs(name, a=None, b=None):
        o, w = lay[name]
        if a is None:
            return cst[:, o:o + w]
        return cst[:, o + a:o + b]

    S.dma("sp", "cst", lambda e: e.dma_start(out=cst[:], in_=cst_d), writes=[cstB])
    S.op("dve", lambda e: e.tensor_copy(out=ident[:], in_=cs("ident")), reads=[cstB], writes=[identB])
    if 1 in layers and "mixer" in parts:
        S.dma("sp", "relb", lambda e: e.dma_start(out=tmpo[:].rearrange("p t d -> p (t d)"), in_=relb_d), writes=tmpoB)
        S.op("pool", lambda e: e.memset(Vr[:], 1.0), writes=VB)
        S.op("act", lambda e: e.activation(out=m4[:], in_=cs("m4"), func=AF.Exp), reads=[cstB], writes=[m4B])
        rb = tmpo[:].rearrange("p t d -> p (t d)").rearrange("p (h k q) -> p h k q", h=16, k=2)
        for hh in range(16):
            S.op("dve", lambda e, hh=hh: e.scalar_tensor_tensor(out=rb[:, hh, 0, :], in0=rb[:, hh, 0, :], scalar=cs("crep", hh, hh + 1), op0=ALU.subtract, in1=cs("m0"), op1=ALU.add),
                 reads=tmpoB + [cstB], writes=tmpoB)
            S.op("dve", lambda e, hh=hh: e.tensor_scalar(out=rb[:, hh, 1, :], in0=rb[:, hh, 1, :], scalar1=cs("crep", hh, hh + 1), scalar2=None, op0=ALU.subtract),
                 reads=tmpoB + [cstB], writes=tmpoB)
            S.op("act", lambda e, hh=hh: e.activation(out=bt[:, hh, :, :], in_=rb[:, hh, :, :], func=AF.Exp), reads=tmpoB, writes=[btB])
    if 0 in layers and "mixer" in parts:
        S.op("pool", lambda e: e.memset(Va[:], 1.0), writes=VaB)
        S.op("pool", lambda e: e.memset(ones[:], 1.0), writes=[evB])
        S.op("dve", lambda e: e.tensor_copy(out=mcb[:], in_=cs("mc")), reads=[cstB], writes=[evB])
        if not (1 in layers and "mixer" in parts):
            S.op("act", lambda e: e.activation(out=m4[:], in_=cs("m4"), func=AF.Exp), reads=[cstB], writes=[m4B])
        S.op("act", lambda e: e.activation(out=m0b[:], in_=cs("m0"), func=AF.Exp), reads=[cstB], writes=[evB])
        for e_ in range(2):
            S.op("dve", lambda e, e_=e_: e.tensor_copy(out=mfull[:, e_, 0, :], in_=m4[:]), reads=[m4B], writes=[evB])
            S.op("dve", lambda e, e_=e_: e.tensor_copy(out=mfull[:, e_, 1, :], in_=m0b[:]), reads=[evB], writes=[evB])
        S.op("act", lambda e: e.activation(out=esink[:], in_=cs("sinks"), func=AF.Exp), reads=[cstB], writes=[evB])
        S.op("dve", lambda e: e.tensor_tensor(out=lbv[:, 4:8], in0=cs("lbl", 0, 4), in1=cs("lbl", 4, 8), op=ALU.subtract), reads=[cstB], writes=[evB])
        S.op("act", lambda e: e.activation(out=lbv[:, 0:4], in_=lbv[:, 4:8], func=AF.Sigmoid), reads=[evB], writes=[evB])
        S.op("dve", lambda e: e.tensor_scalar(out=lbv[:, 4:8], in0=lbv[:, 0:4], scalar1=-1.0, scalar2=1.0, op0=ALU.mult, op1=ALU.add), reads=[evB], writes=[evB])
    cvB = {}
    for L in layers:
        for (n, nco) in layer_piece_list(L):
            sec = n[:2]
            key = "cv%d%s" % (L, sec)
            cvB.setdefault(key, Buf(key))
            o, _ = ptab[(L, n)]
            r0, r1 = o // 2048, (o + 128 * nco) // 2048
            S.dma("pool", key, lambda e, r0=r0, r1=r1: e.dma_start(out=wbf_d[r0:r1, :], in_=wsrc_d[r0:r1, :]),
                  writes=[cvB[key]])

    order = []
    for s in range(nseq):
        for g in range(NG):
            for L in layers:
                for (n, nco) in layer_piece_list(L):
                    if (n[:2] in ("up", "dn")) and "ffn" not in parts:
                        continue
                    if (n[:2] not in ("up", "dn")) and "mixer" not in parts:
                        continue
                    order.append((L, n))
    k.w_issue = 0
    k.w_use = 0

    def w_issue_upto(i):
        while k.w_issue <= i and k.w_issue < len(order):
            j = k.w_issue
            L, n = order[j]
            o, nco = ptab[(L, n)]
            slot = j % NSLOT
            key = "cv%d%s" % (L, n[:2])
            src = wbf_d.rearrange("r c -> (r c)")[o:o + 128 * nco].rearrange("(p c) -> p c", p=128)
            S.dma("sp", "w%d" % slot, lambda e, slot=slot, nco=nco, src=src: e.dma_start(out=ring[slot][:, 0:nco], in_=src),
                  reads=[cvB[key]], writes=[ringB[slot]])
            k.w_issue += 1

    def w_get(L, n):
        i = k.w_use
        assert order[i] == (L, n), (order[i], L, n)
        w_issue_upto(i + NSLOT - 1)
        k.w_use += 1
        return ring[i % NSLOT], ringB[i % NSLOT]

    def rstd_tile(t):
        S.op("act", lambda e: e.activation(out=rstd[:, t:t + 1], in_=ss4[:, t:t + 1], func=AF.Sqrt, scale=1.0 / D, bias=EPS),
             reads=[ss4B[t]], writes=[rstdB[t]])
        S.op("dve", lambda e: e.reciprocal(out=rstd[:, t:t + 1], in_=rstd[:, t:t + 1]), reads=[rstdB[t]], writes=[rstdB[t]])

    def norm_tile(hb, hbB, nwT, t):
        junk, junkB = next_junk()
        S.op("act", lambda e: e.activation(out=junk[:], in_=hb[:, t, :], func=AF.Square, accum_out=ss4[:, t:t + 1]),
             reads=[hbB[t]], writes=[ss4B[t], junkB])
        rstd_tile(t)
        y, yB = yn[t % 2], ynB[t % 2]
        S.op("dve", lambda e: e.tensor_scalar(out=y[:], in0=hb[:, t, :], scalar1=rstd[:, t:t + 1], scalar2=None, op0=ALU.mult),
             reads=[hbB[t], rstdB[t]], writes=[yB])
        p, pB = bank()
        pb = p[:].bitcast(BF16)
        for kc in range(KC):
            S.op("pe", lambda e, kc=kc: e.transpose(out=pb[:, kc * 128:(kc + 1) * 128], in_=y[:, kc * 128:(kc + 1) * 128], identity=ident[:]),
                 reads=[yB, identB], writes=[pB], inc=(kc == KC - 1))
        S.op("dve", lambda e: e.tensor_tensor(out=yT[:, :, t * 128:(t + 1) * 128], in0=pb.rearrange("p (k t) -> p k t", k=KC),
                                              in1=nwT.unsqueeze(2).broadcast_to([128, KC, 128]), op=ALU.mult),
             reads=[pB, cstB], writes=[yTB[t]])

    def norm_transpose(hb, hbB, nwT):
        for t in range(TPG):
            norm_tile(hb, hbB, nwT, t)

    def post_tile(hb, hbB, t):
        S.op("dve", lambda e: e.tensor_tensor(out=ss4[:, t:t + 1], in0=ssp[:, t, 0:1], in1=ssp[:, t, 1:2], op=ALU.add), reads=[sspB[t]], writes=[ss4B[t]])
        rstd_tile(t)
        S.op("dve", lambda e: e.scalar_tensor_tensor(out=hb[:, t, :], in0=tmpo[:, t, :], scalar=rstd[:, t:t + 1], op0=ALU.mult, in1=hb[:, t, :], op1=ALU.add),
             reads=[tmpoB[t], rstdB[t], hbB[t]], writes=[hbB[t]])

    def post_norm_residual(hb, hbB, nwR_name, produce, stage=9):
        for cg in range(2):
            banks = produce(cg)
            for t in range(TPG):
                p, pB = banks[t]
                junk, junkB = next_junk()
                S.op("act", lambda e, t=t, cg=cg, p=p, junk=junk: e.activation(out=junk[:, 0:512], in_=p[:], func=AF.Square, accum_out=ssp[:, t, cg:cg + 1]),
                     reads=[pB], writes=[sspB[t], junkB])
                S.op("dve", lambda e, t=t, cg=cg, p=p: e.tensor_tensor(out=tmpo[:, t, cg * 512:(cg + 1) * 512], in0=p[:], in1=cs(nwR_name, cg * 512, (cg + 1) * 512), op=ALU.mult),
                     reads=[pB, cstB], writes=[tmpoB[t]])
                if cg == 1:
                    post_tile(hb, hbB, t)

    def evac(i, out, in_, reads, writes, scale=None):
        if i % 2 == 0:
            if scale is None:
                S.op("act", lambda e: e.activation(out=out, in_=in_, func=AF.Identity), reads=reads, writes=writes)
            else:
                S.op("act", lambda e: e.activation(out=out, in_=in_, func=AF.Identity, scale=scale), reads=reads, writes=writes)
        else:
            if scale is None:
                S.op("dve", lambda e: e.tensor_copy(out=out, in_=in_), reads=reads, writes=writes)
            else:
                S.op("dve", lambda e: e.tensor_scalar(out=out, in0=in_, scalar1=scale, scalar2=None, op0=ALU.mult), reads=reads, writes=writes)

    def proj_fm(wv, wB, c, dst, dstB, ei, scale=None):
        p, pB = bank()
        for kc in range(KC):
            S.op("pe", lambda e, kc=kc: e.matmul(p[:], lhsT=wv[:, kc, c * 128:(c + 1) * 128], rhs=yT[:, kc, :], start=(kc == 0), stop=(kc == KC - 1)),
                 reads=[wB] + yTB, writes=[pB], inc=(kc == KC - 1))
        evac(ei, dst, p[:], [pB], [dstB], scale)

    def otok_to_yT(qt):
        p, pB = bank()
        pb = p[:].bitcast(BF16)
        for kc in range(KC):
            S.op("pe", lambda e, kc=kc: e.transpose(out=pb[:, kc * 128:(kc + 1) * 128], in_=otok[:, qt, kc * 128:(kc + 1) * 128], identity=ident[:]),
                 reads=[otokB, identB], writes=[pB], inc=(kc == KC - 1))
        evac(qt, yT[:, :, qt * 128:(qt + 1) * 128], pb.rearrange("p (k t) -> p k t", k=KC), [pB], [yTB[qt]])

    def out_proj_post(L, hb, hbB, nwR_name):
        def produce(cg):
            wt, wB = w_get(L, "ow%d" % cg)
            wv = wt[:, 0:4096].rearrange("p (k c) -> p k c", k=KC)
            banks = [bank() for _ in range(TPG)]
            for t in range(TPG):
                p, pB = banks[t]
                for kc in range(KC):
                    S.op("pe", lambda e, t=t, kc=kc, p=p: e.matmul(p[:], lhsT=yT[:, kc, t * 128:(t + 1) * 128], rhs=wv[:, kc, :], start=(kc == 0), stop=(kc == KC - 1)),
                         reads=[wB, yTB[t]], writes=[pB], inc=(kc == KC - 1))
            return banks
        post_norm_residual(hb, hbB, nwR_name, produce)

    def odd_mixer(L, hb, hbB, g):
        alias_fence(mixBs, [gTB])
        norm_transpose(hb, hbB, cs("nwT%d_0" % L))
        ks = g % 2
        ei = 0
        for i in range(2):
            wt, wB = w_get(L, "ik%d" % i)
            wv = wt[:, 0:4096].rearrange("p (k c) -> p k c", k=KC)
            for c in range(4):
                proj_fm(wv, wB, c, kT[:, 4 * i + c, ks * G:(ks + 1) * G], kTB[ks], ei)
                ei += 1
        for i in range(2):
            wt, wB = w_get(L, "iv%d" % i)
            wv = wt[:, 0:4096].rearrange("p (k c) -> p k c", k=KC)
            for t in range(TPG):
                p, pB = bank()
                for kc in range(KC):
                    S.op("pe", lambda e, t=t, kc=kc, p=p, wv=wv: e.matmul(p[:], lhsT=yT[:, kc, t * 128:(t + 1) * 128], rhs=wv[:, kc, :], start=(kc == 0), stop=(kc == KC - 1)),
                         reads=[wB, yTB[t]], writes=[pB], inc=(kc == KC - 1))
                evac(ei, Vr[:, ks * 4 + t, 8 * i:8 * i + 8, 0:64], p[:].rearrange("p (h d) -> p h d", d=64), [pB], [VB[ks]])
                ei += 1
        for i in range(2):
            wt, wB = w_get(L, "iq%d" % i)
            wv = wt[:, 0:4096].rearrange("p (k c) -> p k c", k=KC)
            for c in range(4):
                proj_fm(wv, wB, c, qT[:, 4 * i + c, :], qTB, ei, scale=0.125)
                ei += 1
        k.bank_n = 5
        alias_fence(PTB3f, PTB)
        units = [(qt, hp) for qt in range(TPG) for hp in range(8)]
        U = {}

        def st_phase(u):
            qt, hp = units[u]
            T = g * 4 + qt
            kts = [kt for kt in range(T - 4, T + 1) if kt >= 0]
            sbk = [bank(), bank(), bank()]
            pt, ptBs = PT[u % 2], PTB3[u % 2]
            stops = []
            fixes = []
            seq_ = [(kt, 0) for kt in kts] + [(None, None)] + [(kt, 1) for kt in kts]
            for (kt, e_) in seq_:
                if kt is None:
                    stops.append((lambda e, sbk=sbk: e.matmul(sbk[2][0][:, 256:258], lhsT=ident[:], rhs=ident[:, 0:2], start=True, stop=True),
                                  [identB], [sbk[2][1]]))
                    continue
                hh = 2 * hp + e_
                lo, hi = e_ * 64, (e_ + 1) * 64
                j = kt - (T - 4)
                if j < 4:
                    reg, regB = sbk[e_][0][:, j * 128:(j + 1) * 128], sbk[e_][1]
                    pcol, part = e_ * 512 + j * 128, e_
                elif len(kts) == 1 and e_ == 1:
                    reg, regB = sbk[1][0][:, 0:128], sbk[1][1]
                    pcol, part = 1024 + e_ * 128, 2
                else:
                    reg, regB = sbk[2][0][:, e_ * 128:(e_ + 1) * 128], sbk[2][1]
                    pcol, part = 1024 + e_ * 128, 2
                kslot = (kt // 4) % 2
                k0 = kslot * G + (kt % 4) * 128
                delta = T - kt
                stops.append((lambda e, reg=reg, lo=lo, hi=hi, k0=k0, hp=hp, qt=qt: e.matmul(reg, lhsT=kT[lo:hi, hp, k0:k0 + 128], rhs=qT[lo:hi, hp, qt * 128:(qt + 1) * 128], start=True, stop=True),
                              [kTB[kslot], qTB], [regB]))
                if delta in (0, 1, 4):
                    fac = bt[:, hh, 0, :] if delta == 0 else (bt[:, hh, 1, :] if delta == 1 else m4[:])
                    fixes.append((pcol, part, fac))
            for si, (fn_, rd_, wr_) in enumerate(stops):
                S.op("pe", fn_, reads=rd_, writes=wr_, inc=(si == len(stops) - 1))
            U[u] = dict(sbk=sbk, pt=pt, ptBs=ptBs, fixes=fixes, kts=kts, T=T)


        def ex_phase(u):
            d = U[u]
            sbk, pt, ptBs, kts = d["sbk"], d["pt"], d["ptBs"], d["kts"]
            c0 = (5 - len(kts)) * 128
            if c0 < 512:
                for e_ in range(2):
                    S.op("act", lambda e, e_=e_: e.activation(out=pt[:, e_ * 512 + c0:(e_ + 1) * 512], in_=sbk[e_][0][:, c0:512], func=AF.Exp),
                         reads=[sbk[e_][1]], writes=[ptBs[e_]])
            if len(kts) == 1:
                S.op("act", lambda e: e.activation(out=pt[:, 1024:1152], in_=sbk[2][0][:, 0:128], func=AF.Exp), reads=[sbk[2][1]], writes=[ptBs[2]])
                S.op("act", lambda e: e.activation(out=pt[:, 1152:1280], in_=sbk[1][0][:, 0:128], func=AF.Exp), reads=[sbk[1][1]], writes=[ptBs[2]])
            else:
                S.op("act", lambda e: e.activation(out=pt[:, 1024:1280], in_=sbk[2][0][:, 0:256], func=AF.Exp), reads=[sbk[2][1]], writes=[ptBs[2]])
            for (pcol, part, fac) in d["fixes"]:
                S.op("dve", lambda e, pcol=pcol, fac=fac: e.tensor_tensor(out=pt[:, pcol:pcol + 128], in0=pt[:, pcol:pcol + 128], in1=fac, op=ALU.mult),
                     reads=[ptBs[part], btB, m4B], writes=[ptBs[part]])

        def pv_phase(u):
            d = U[u]
            pt, ptBs, kts, T = d["pt"], d["ptBs"], d["kts"], d["T"]
            qt, hp = units[u]
            for e_ in range(2):
                hh = 2 * hp + e_
                ob, obB = psum[5 + hh // 6], psumB[5 + hh // 6]
                col = (hh % 6) * 65
                for idx, kt in enumerate(kts):
                    j = kt - (T - 4)
                    pc = pt[:, e_ * 512 + j * 128:e_ * 512 + (j + 1) * 128] if j < 4 else pt[:, 1024 + e_ * 128:1024 + (e_ + 1) * 128]
                    last = (idx == len(kts) - 1)
                    S.op("pe", lambda e, pc=pc, kt=kt, hh=hh, idx=idx, last=last, ob=ob, col=col: e.matmul(ob[:, col:col + 65], lhsT=pc, rhs=Vr[:, kt % 8, hh, :], start=(idx == 0), stop=last),
                         reads=[ptBs[e_], ptBs[2], VB[(kt // 4) % 2]], writes=[obB], inc=(last and e_ == 1))

        def fin_qt(qt):
            for b in range(3):
                nh = 6 if b < 2 else 4
                ob, obB = psum[5 + b], psumB[5 + b]
                view = ob[:, 0:nh * 65].rearrange("p (h c) -> p h c", c=65)
                S.op("dve", lambda e, view=view, nh=nh: e.reciprocal(out=rden[:, 0:nh], in_=view[:, :, 64]), reads=[obB], writes=[rdenB])
                S.op("dve", lambda e, view=view, nh=nh, b=b: e.tensor_tensor(out=otok[:, qt, b * 384:b * 384 + nh * 64].rearrange("p (h d) -> p h d", d=64),
                                                                            in0=view[:, :, 0:64], in1=rden[:, 0:nh].unsqueeze(2).broadcast_to([128, nh, 64]), op=ALU.mult),
                     reads=[obB, rdenB], writes=[otokB])
            otok_to_yT(qt)

        st_phase(0)
        for u in range(len(units)):
            ex_phase(u)
            if u + 1 < len(units):
                st_phase(u + 1)
            pv_phase(u)
            if units[u][1] == 7:
                fin_qt(units[u][0])
        k.bank_n = 8
        out_proj_post(L, hb, hbB, "nwR%d_1" % L)

    def mm_group(p, pB, lhs_fn, rhs_fn, reads, n=KC):
        for kc in range(n):
            S.op("pe", lambda e, kc=kc: e.matmul(p, lhsT=lhs_fn(kc), rhs=rhs_fn(kc), start=(kc == 0), stop=(kc == n - 1)),
                 reads=reads, writes=[pB], inc=(kc == n - 1))

    def rope_chunk(wv, wB, c, dst, dstB):
        pa, paB = bank()
        mm_group(pa[:], paB, lambda kc: wv[:, kc, c * 128:(c + 1) * 128], lambda kc: yT[:, kc, :], [wB] + yTB)
        pr, prB = bank()
        mm_group(pr[:], prB, lambda kc: wv[:, kc, (c + 1) * 128:(c + 2) * 128], lambda kc: yT[:, kc, :], [wB] + yTB)
        t1, t1B = csb[0], csbB[0]
        t2, t2B = csb[1], csbB[1]
        S.op("dve", lambda e: e.tensor_tensor(out=t1[:], in0=pa[:], in1=ropeT[:, 0, :], op=ALU.mult), reads=[paB, ropeB], writes=[t1B])
        S.op("dve", lambda e: e.tensor_tensor(out=t2[:], in0=pr[:], in1=ropeT[:, 1, :], op=ALU.mult), reads=[prB, ropeB], writes=[t2B])
        S.op("pool", lambda e: e.tensor_tensor(out=dst, in0=t1[:], in1=t2[:], op=ALU.add), reads=[t1B, t2B], writes=[dstB])

    def hgrn_head(hd, wvq, wBq, cq, wvf, wBf, cf, g, first):
        i2 = hd % 2
        pq, pqB = bank()
        mm_group(pq[:], pqB, lambda kc: wvq[:, kc, cq * 128:(cq + 1) * 128], lambda kc: yT[:, kc, :], [wBq] + yTB)
        pf, pfB = bank()
        mm_group(pf[:], pfB, lambda kc: wvf[:, kc, cf * 128:(cf + 1) * 128], lambda kc: yT[:, kc, :], [wBf] + yTB)
        yield
        if i2 == 0:
            fa, faB = csb[0], csbB[0]
            lc, lcB = csb[1], csbB[1]
            e1, e1B = gsb[0], gsbB[0]
            e2, e2B = gsb[1], gsbB[1]
        else:
            fa, faB = htmp[0], htmpB[0]
            lc, lcB = htmp[1], htmpB[1]
            e1, e1B = htmp[2], htmpB[2]
            e2, e2B = htmp[3], htmpB[3]
        hv, hvB = hvec[i2], hvecB[i2]
        S.op("act", lambda e: e.activation(out=fa[:], in_=pf[:], func=AF.Sigmoid), reads=[pfB], writes=[faB])
        yield
        S.op("dve", lambda e: e.tensor_scalar(out=fa[:], in0=fa[:], scalar1=lbv[:, 4 + hd:5 + hd], scalar2=lbv[:, hd:hd + 1], op0=ALU.mult, op1=ALU.add),
             reads=[faB, evB], writes=[faB])
        yield
        S.op("act", lambda e: e.activation(out=e1[:], in_=fa[:], func=AF.Ln), reads=[faB], writes=[e1B])
        yield
        for t in range(TPG):
            S.op("dve", lambda e, t=t: e.tensor_tensor_scan(out=lc[:, t * 128:(t + 1) * 128], data0=ones[:], data1=e1[:, t * 128:(t + 1) * 128], initial=0.0, op0=ALU.mult, op1=ALU.add),
                 reads=[e1B, evB], writes=[lcB])
        yield
        lc3 = lc[:].rearrange("p (t m) -> p t m", t=TPG)
        S.op("dve", lambda e: e.tensor_copy(out=hv[:, :, 0], in_=lc3[:, :, 63]), reads=[lcB], writes=[hvB])
        S.op("act", lambda e: e.activation(out=hv[:, :, 1], in_=lc3[:, :, 63], func=AF.Exp), reads=[lcB], writes=[hvB])
        S.op("act", lambda e: e.activation(out=hv[:, :, 2], in_=lc3[:, :, 127], func=AF.Exp), reads=[lcB], writes=[hvB])
        yield
        S.op("dve", lambda e: e.tensor_tensor(out=lc3, in0=lc3, in1=hv[:, :, 0:1].broadcast_to([128, TPG, 128]), op=ALU.subtract), reads=[lcB, hvB], writes=[lcB])
        yield
        S.op("act", lambda e: e.activation(out=hv[:, :, 3], in_=lc3[:, :, 127], func=AF.Exp), reads=[lcB], writes=[hvB])
        S.op("act", lambda e: e.activation(out=e1[:], in_=lc[:], func=AF.Exp), reads=[lcB], writes=[e1B])
        S.op("act", lambda e: e.activation(out=e2[:], in_=lc[:], func=AF.Exp, scale=-1.0), reads=[lcB], writes=[e2B])
        yield
        qd, qdB = qdT[i2], qdTB[i2]
        kd, kdB = kdT[i2], kdTB[i2]
        S.op("dve", lambda e: e.tensor_tensor(out=qd[:], in0=pq[:], in1=e1[:], op=ALU.mult), reads=[pqB, e1B], writes=[qdB])
        S.op("act", lambda e: e.activation(out=fa[:], in_=pf[:], func=AF.Sigmoid, scale=-1.0), reads=[pfB, faB], writes=[faB])
        S.op("dve", lambda e: e.scalar_tensor_tensor(out=kd[:], in0=fa[:], scalar=lbv[:, 4 + hd:5 + hd], op0=ALU.mult, in1=e2[:], op1=ALU.mult), reads=[faB, e2B, evB], writes=[kdB])
        yield
        kk, kkB = kdTok[i2], kdTokB[i2]
        pt_, ptB_ = bank()
        ptb = pt_[:].bitcast(BF16)
        for t in range(TPG):
            S.op("pe", lambda e, t=t: e.transpose(out=ptb[:, t * 128:(t + 1) * 128], in_=kd[:, t * 128:(t + 1) * 128], identity=ident[:]),
                 reads=[kdB, identB], writes=[ptB_], inc=(t == TPG - 1))
        evac(hd, kk[:].rearrange("p t d -> p (t d)"), ptb[:, 0:512], [ptB_], [kkB])

    def hgrn_tile(hd, t, g, first):
        i2 = hd % 2
        hv, hvB = hvec[i2], hvecB[i2]
        qd, qdB = qdT[i2], qdTB[i2]
        kd, kdB = kdT[i2], kdTB[i2]
        kk, kkB = kdTok[i2], kdTokB[i2]
        tile0 = first and t == 0
        sl = slice(t * 128, (t + 1) * 128)
        vv = iTok[:, t, hd * 128:(hd + 1) * 128]
        bi = i2 * 2 + (t % 2)
        ps_, psB_ = bank()
        S.op("pe", lambda e: e.matmul(ps_[:, 0:128], lhsT=kd[:, sl], rhs=qd[:, sl], start=True, stop=True), reads=[kdB, qdB], writes=[psB_])
        sm, smB = sTm[bi], sTmB[bi]
        S.op("dve", lambda e: e.tensor_tensor(out=sm[:], in0=ps_[:, 0:128], in1=mcb[:], op=ALU.mult), reads=[psB_, evB], writes=[smB])
        if not tile0:
            S.op("act", lambda e: e.activation(out=stS[:, hd, :], in_=state[:, hd, :], func=AF.Identity, scale=hv[:, t, 1:2]),
                 reads=[stateB[hd], hvB], writes=[stSB[hd]])
        po, poB = bank()
        S.op("pe", lambda e: e.matmul(po[:, 0:128], lhsT=sm[:], rhs=vv, start=True, stop=tile0), reads=[smB, iTokB], writes=[poB], inc=tile0)
        if not tile0:
            S.op("pe", lambda e: e.matmul(po[:, 0:128], lhsT=qd[:, sl], rhs=stS[:, hd, :], start=False, stop=True), reads=[qdB, stSB[hd]], writes=[poB])
        if not (g == NG - 1 and t == TPG - 1):
            pd, pdB = bank()
            S.op("pe", lambda e: e.matmul(pd[:, 0:128], lhsT=kk[:, t, :], rhs=vv, start=True, stop=True), reads=[kkB, iTokB], writes=[pdB])
            if tile0:
                S.op("dve", lambda e: e.tensor_scalar(out=state[:, hd, :], in0=pd[:, 0:128], scalar1=hv[:, t, 3:4], scalar2=None, op0=ALU.mult),
                     reads=[pdB, hvB], writes=[stateB[hd]])
            else:
                td_, tdB_ = tmpd[bi], tmpdB[bi]
                S.op("dve", lambda e: e.tensor_scalar(out=td_[:], in0=pd[:, 0:128], scalar1=hv[:, t, 3:4], scalar2=None, op0=ALU.mult),
                     reads=[pdB, hvB], writes=[tdB_])
                S.op("dve", lambda e: e.scalar_tensor_tensor(out=state[:, hd, :], in0=state[:, hd, :], scalar=hv[:, t, 2:3], op0=ALU.mult, in1=td_[:], op1=ALU.add),
                     reads=[tdB_, hvB, stateB[hd]], writes=[stateB[hd]])
        junk, junkB = next_junk()
        S.op("act", lambda e: e.activation(out=junk[:, 0:128], in_=po[:, 0:128], func=AF.Square, accum_out=ssh[:, t * 4 + hd:t * 4 + hd + 1]),
             reads=[poB], writes=[sshB[t * 4 + hd], junkB])
        S.op("dve", lambda e: e.tensor_copy(out=tmpo[:, t, hd * 128:(hd + 1) * 128], in_=po[:, 0:128]), reads=[poB], writes=[tmpoB[t]])

    def even_mixer(L, hb, hbB, g):
        first = (g == 0)
        alias_fence(mixBs, [gTB])
        r0 = g * G
        S.dma("sp", "rope", lambda e: e.dma_start(out=ropeT[:], in_=rope_d[:, :, r0:r0 + G]), writes=[ropeB])
        norm_transpose(hb, hbB, cs("nwT%d_0" % L))
        ks = g % 2
        def piece(i):
            wt, wB = w_get(L, "e%d" % i)
            n = len(EVEN_FM[i])
            return wt[:, 0:KC * 128 * n].rearrange("p (k c) -> p k c", k=KC), wB
        wt5, wB5 = w_get(L, "e5")
        w5 = wt5[:, 0:KC * 640].rearrange("p (k c) -> p k c", k=KC)
        for t in range(TPG):
            p, pB = bank()
            mm_group(p[:, 0:128], pB, lambda kc, t=t: yT[:, kc, t * 128:(t + 1) * 128], lambda kc: w5[:, kc, 0:128], [wB5, yTB[t]])
            evac(t, Va[:, ks * 4 + t, :, 0:64], p[:, 0:128].rearrange("p (h d) -> p h d", d=64), [pB], [VaB[ks]])
            p, pB = bank()
            mm_group(p[:], pB, lambda kc, t=t: yT[:, kc, t * 128:(t + 1) * 128], lambda kc: w5[:, kc, 128:640], [wB5, yTB[t]])
            evac(t + 1, iTok[:, t, :], p[:], [pB], [iTokB])
        wt6, wB6 = w_get(L, "e6")
        w6 = wt6[:, 0:KC * 512].rearrange("p (k c) -> p k c", k=KC)
        for t in range(TPG):
            p, pB = bank()
            mm_group(p[:], pB, lambda kc, t=t: yT[:, kc, t * 128:(t + 1) * 128], lambda kc: w6[:, kc, :], [wB6, yTB[t]])
            S.op("act", lambda e, p=p, t=t: e.activation(out=sg[:, t, :], in_=p[:], func=AF.Silu), reads=[pB], writes=[sgB])
            S.op("pool", lambda e, t=t: e.tensor_tensor(out=sg[:, t, :], in0=sg[:, t, :], in1=cs("gw"), op=ALU.mult), reads=[sgB, cstB], writes=[sgB])
        wv, wB = piece(0)
        rope_chunk(wv, wB, 0, kaT[:, ks * G:(ks + 1) * G], kaTB[ks])
        rope_chunk(wv, wB, 2, qaT[:, 0, :], qTB)
        wv, wB = piece(1)
        rope_chunk(wv, wB, 0, qaT[:, 1, :], qTB)
        rope_chunk(wv, wB, 2, qaT[:, 2, :], qTB)
        wv2, wB2 = piece(2)
        rope_chunk(wv2, wB2, 0, qaT[:, 3, :], qTB)
        k.bank_n = 6
        alias_fence(PTB, PTB3f)
        units = [(qt, c) for qt in range(TPG) for c in range(4)]
        U = {}

        def st_phase(u):
            qt, c = units[u]
            T = g * 4 + qt
            kts = [kt for kt in (T - 1, T) if kt >= 0]
            sb2 = [bank(), bank()]
            ops_ = []
            for e_ in range(2):
                if e_ == 1:
                    ops_.append((lambda e, sb2=sb2: e.matmul(sb2[1][0][:, 256:258], lhsT=ident[:], rhs=ident[:, 0:2], start=True, stop=True),
                                 [identB], [sb2[1][1]]))
                for kt in kts:
                    lo, hi = e_ * 64, (e_ + 1) * 64
                    j = kt - (T - 1)
                    reg = sb2[e_][0][:, j * 128:(j + 1) * 128]
                    kslot = (kt // 4) % 2
                    k0 = kslot * G + (kt % 4) * 128
                    ops_.append((lambda e, reg=reg, lo=lo, hi=hi, k0=k0, c=c, qt=qt: e.matmul(reg, lhsT=kaT[lo:hi, k0:k0 + 128], rhs=qaT[lo:hi, c, qt * 128:(qt + 1) * 128], start=True, stop=True),
                                 [kaTB[kslot], qTB], [sb2[e_][1]]))
            for si, (fn_, rd_, wr_) in enumerate(ops_):
                S.op("pe", fn_, reads=rd_, writes=wr_, inc=(si == len(ops_) - 1))
            U[u] = dict(sb2=sb2, kts=kts, T=T)

        def ex_phase(u):
            d = U[u]
            sb2, kts = d["sb2"], d["kts"]
            pt, ptB = PTa[u % 2], PTB[u % 2]
            j0 = 2 - len(kts)
            for e_ in range(2):
                S.op("act", lambda e, e_=e_: e.activation(out=pt[:, e_ * 256 + j0 * 128:(e_ + 1) * 256], in_=sb2[e_][0][:, j0 * 128:256], func=AF.Exp, scale=0.125),
                     reads=[sb2[e_][1]], writes=[ptB])
            S.op("dve", lambda e: e.tensor_tensor(out=pt.rearrange("p (e j q) -> p e j q", e=2, j=2)[:, :, j0:2, :],
                                                  in0=pt.rearrange("p (e j q) -> p e j q", e=2, j=2)[:, :, j0:2, :], in1=mfull[:, :, j0:2, :], op=ALU.mult),
                 reads=[ptB, evB], writes=[ptB])

        def pv_phase(u):
            d = U[u]
            kts, T = d["kts"], d["T"]
            qt, c = units[u]
            pt, ptB = PTa[u % 2], PTB[u % 2]
            for e_ in range(2):
                ob, obB = psum[6 + e_], psumB[6 + e_]
                col = c * 65
                for idx, kt in enumerate(kts):
                    j = kt - (T - 1)
                    pc = pt[:, e_ * 256 + j * 128:e_ * 256 + (j + 1) * 128]
                    last = (idx == len(kts) - 1)
                    S.op("pe", lambda e, pc=pc, kt=kt, e_=e_, idx=idx, last=last, ob=ob, col=col: e.matmul(ob[:, col:col + 65], lhsT=pc, rhs=Va[:, kt % 8, e_, :], start=(idx == 0), stop=last),
                         reads=[ptB, VaB[(kt // 4) % 2]], writes=[obB], inc=(last and e_ == 1))

        def fin_qt(qt):
            for b in range(2):
                ob, obB = psum[6 + b], psumB[6 + b]
                view = ob[:, 0:260].rearrange("p (h c) -> p h c", c=65)
                S.op("dve", lambda e, view=view, b=b: e.tensor_tensor(out=rden[:, 0:4], in0=view[:, :, 64], in1=esink[:, 4 * b:4 * b + 4], op=ALU.add), reads=[obB, evB], writes=[rdenB])
                S.op("dve", lambda e: e.reciprocal(out=rden[:, 0:4], in_=rden[:, 0:4]), reads=[rdenB], writes=[rdenB])
                S.op("dve", lambda e, view=view, b=b: e.tensor_tensor(out=otok[:, qt, b * 256:(b + 1) * 256].rearrange("p (h d) -> p h d", d=64),
                                                                      in0=view[:, :, 0:64], in1=rden[:, 0:4].unsqueeze(2).broadcast_to([128, 4, 64]), op=ALU.mult),
                     reads=[obB, rdenB], writes=[otokB])

        st_phase(0)
        for u in range(len(units)):
            ex_phase(u)
            if u + 1 < len(units):
                st_phase(u + 1)
            pv_phase(u)
            if units[u][1] == 3:
                fin_qt(units[u][0])
        k.bank_n = 8
        def run_pair(ga, gb):
            alive = [ga, gb]
            while alive:
                for gn in list(alive):
                    try:
                        next(gn)
                    except StopIteration:
                        alive.remove(gn)
        g0 = hgrn_head(0, wv2, wB2, 2, wv2, wB2, 3, g, first)
        next(g0)
        wv3, wB3 = piece(3)
        g1 = hgrn_head(1, wv3, wB3, 0, wv3, wB3, 1, g, first)
        next(g1)
        run_pair(g0, g1)
        for t in range(TPG):
            hgrn_tile(0, t, g, first)
            hgrn_tile(1, t, g, first)
        g2 = hgrn_head(2, wv3, wB3, 2, wv3, wB3, 3, g, first)
        next(g2)
        wv4, wB4 = piece(4)
        g3 = hgrn_head(3, wv4, wB4, 0, wv4, wB4, 1, g, first)
        next(g3)
        run_pair(g2, g3)
        for t in range(TPG):
            hgrn_tile(2, t, g, first)
            hgrn_tile(3, t, g, first)
        S.op("act", lambda e: e.activation(out=rsh[:], in_=ssh[:], func=AF.Sqrt, scale=1.0 / 128, bias=EPS), reads=sshB, writes=[rshB])
        S.op("dve", lambda e: e.reciprocal(out=rsh[:], in_=rsh[:]), reads=[rshB], writes=[rshB])
        for t in range(TPG):
            o3 = tmpo[:, t, 0:512].rearrange("p (h d) -> p h d", h=4)
            S.op("dve", lambda e, t=t, o3=o3: e.tensor_tensor(out=o3, in0=o3, in1=rsh[:, t * 4:(t + 1) * 4].unsqueeze(2).broadcast_to([128, 4, 128]), op=ALU.mult),
                 reads=[tmpoB[t], rshB], writes=[tmpoB[t]])
            S.op("dve", lambda e, t=t: e.tensor_tensor(out=otok[:, t, 512:1024], in0=tmpo[:, t, 0:512], in1=sg[:, t, :], op=ALU.mult), reads=[tmpoB[t], sgB], writes=[otokB])
        for qt in range(TPG):
            otok_to_yT(qt)
        out_proj_post(L, hb, hbB, "nwR%d_1" % L)

    def ffn_block(L, hb, hbB, first, last):
        import os
        stage = int(os.environ.get("FFN_STAGE", "9"))
        alias_fence([gTB], mixBs)
        norm_transpose(hb, hbB, cs("nwT%d_2" % L))
        if stage < 2:
            return
        cw = cs("cw%d" % L).rearrange("p (f j) -> p f j", j=3)
        cb = cs("cb%d" % L)
        cidx = 0
        for j in range(11):
            wt, wB = w_get(L, "up%d" % j)
            wv = wt[:, 0:4096].rearrange("p (k c) -> p k c", k=KC)
            for i in range(2):
                fc = 2 * j + i
                pu, puB = bank()
                for kc in range(KC):
                    S.op("pe", lambda e, kc=kc, i=i, pu=pu, wv=wv: e.matmul(pu[:], lhsT=wv[:, kc, i * 128:(i + 1) * 128], rhs=yT[:, kc, :], start=(kc == 0), stop=(kc == KC - 1)),
                         reads=[wB] + yTB, writes=[puB], inc=(kc == KC - 1))
                pv, pvB = bank()
                for kc in range(KC):
                    S.op("pe", lambda e, kc=kc, i=i, pv=pv, wv=wv: e.matmul(pv[:], lhsT=wv[:, kc, 256 + i * 128:256 + (i + 1) * 128], rhs=yT[:, kc, :], start=(kc == 0), stop=(kc == KC - 1)),
                         reads=[wB] + yTB, writes=[pvB], inc=(kc == KC - 1))
                c, cB = csb[cidx % 2], csbB[cidx % 2]
                gg, gB = gsb[cidx % 2], gsbB[cidx % 2]
                cidx += 1
                if stage < 3:
                    continue
                S.op("act", lambda e, fc=fc, c=c, pu=pu: e.activation(out=c[:], in_=pu[:], func=AF.Identity, scale=cw[:, fc, 2:3], bias=cb[:, fc:fc + 1]),
                     reads=[puB, cstB], writes=[cB])
                S.op("dve", lambda e, fc=fc, c=c, pu=pu: e.scalar_tensor_tensor(out=c[:, 1:G], in0=pu[:, 0:G - 1], scalar=cw[:, fc, 1:2], op0=ALU.mult, in1=c[:, 1:G], op1=ALU.add),
                     reads=[puB, cstB, cB], writes=[cB])
                S.op("dve", lambda e, fc=fc, c=c, pu=pu: e.scalar_tensor_tensor(out=c[:, 2:G], in0=pu[:, 0:G - 2], scalar=cw[:, fc, 0:1], op0=ALU.mult, in1=c[:, 2:G], op1=ALU.add),
                     reads=[puB, cstB, cB], writes=[cB])
                if stage < 4:
                    continue
                if not first:
                    S.op("dve", lambda e, fc=fc, c=c: e.scalar_tensor_tensor(out=c[:, 0:1], in0=carry[L][:, fc, 1:2], scalar=cw[:, fc, 1:2], op0=ALU.mult, in1=c[:, 0:1], op1=ALU.add),
                         reads=[carryB[L], cstB, cB], writes=[cB])
                    S.op("dve", lambda e, fc=fc, c=c: e.scalar_tensor_tensor(out=c[:, 0:2], in0=carry[L][:, fc, 0:2], scalar=cw[:, fc, 0:1], op0=ALU.mult, in1=c[:, 0:2], op1=ALU.add),
                         reads=[carryB[L], cstB, cB], writes=[cB])
                if not last:
                    S.op("dve", lambda e, fc=fc, pu=pu: e.tensor_copy(out=carry[L][:, fc, :], in_=pu[:, G - 2:G]),
                         reads=[puB], writes=[carryB[L]])
                S.op("act", lambda e, c=c, gg=gg: e.activation(out=gg[:], in_=c[:], func=AF.Gelu), reads=[cB], writes=[gB])
                S.op("dve", lambda e, fc=fc, gg=gg, pv=pv: e.tensor_tensor(out=gT[:, fc, :], in0=pv[:], in1=gg[:], op=ALU.mult),
                     reads=[pvB, gB], writes=[gTB])

        if stage < 5:
            for _ in range(4):
                w_get(L, order[k.w_use][1])
            return

        def produce(cg):
            banks = [bank() for _ in range(TPG)]
            for hf in range(2):
                wt, wB = w_get(L, "dn%d_%d" % (cg, hf))
                wv = wt[:, 0:5632].rearrange("p (f c) -> p f c", f=11)
                for t in range(TPG):
                    p, pB = banks[t]
                    for f in range(11):
                        fc = hf * 11 + f
                        S.op("pe", lambda e, t=t, f=f, fc=fc, p=p, wv=wv: e.matmul(p[:], lhsT=gT[:, fc, t * 128:(t + 1) * 128], rhs=wv[:, f, :], start=(fc == 0), stop=(fc == FC - 1)),
                             reads=[wB, gTB], writes=[pB], inc=(f == 10))
            return banks
        post_norm_residual(hb, hbB, "nwR%d_3" % L, produce)

    k.ffn_block = ffn_block
    k.extra = {}

    gi = 0
    groups = [(s_, g_) for s_ in range(nseq) for g_ in range(NG)]

    def load_tile(gidx, t):
        s_, g_ = groups[gidx]
        r0 = s_ * seq + g_ * G + t * 128
        hb_, hbB_ = h[gidx % NH], hB[gidx % NH]
        S.dma("sp", "ldx%d_%d" % (gidx % NH, t), lambda e: e.dma_start(out=hb_[:, t, :], in_=x_d[r0:r0 + 128, :]), writes=[hbB_[t]])

    def store_tile(gidx, t):
        s_, g_ = groups[gidx]
        r0 = s_ * seq + g_ * G + t * 128
        hb_, hbB_ = h[gidx % NH], hB[gidx % NH]
        S.dma("sp", "stx%d_%d" % (gidx % NH, t), lambda e: e.dma_start(out=out_d[r0:r0 + 128, :], in_=hb_[:, t, :]), reads=[hbB_[t]])

    for t in range(TPG):
        load_tile(0, t)
    for gi, (s, g) in enumerate(groups):
        hb, hbB = h[gi % NH], hB[gi % NH]
        for L in layers:
            if "mixer" in parts:
                if L == 1:
                    odd_mixer(L, hb, hbB, g)
                else:
                    even_mixer(L, hb, hbB, g)
            if "ffn" in parts:
                ffn_block(L, hb, hbB, first=(g == 0), last=(g == NG - 1))
        for t in range(TPG):
            store_tile(gi, t)
            if gi + 1 < len(groups):
                load_tile(gi + 1, t)
    S.wait_all("sp", [b for hl in hB for b in hl])
    S.emit()
    return nc


def host_prep(inp):
    return {"wsrc": weights_host(inp).reshape(-1, 2048), "cst": consts_host(inp), "relb": relb_host(inp),
            "rope": rope_host(int(inp["x"].shape[1]))}


_NC_CACHE = {}


def kernel(**inputs):
    inp = {k_: np.asarray(v) for k_, v in inputs.items()}
    x = inp["x"]
    B, Sq, _ = x.shape
    ncores = 8
    nseq = B // ncores
    key = (nseq, Sq)
    if key not in _NC_CACHE:
        _NC_CACHE[key] = build(nseq, Sq)
    nc = _NC_CACHE[key]
    host = host_prep(inp)
    in_maps = []
    for c in range(ncores):
        m = dict(host)
        m["x"] = np.ascontiguousarray(x[c * nseq:(c + 1) * nseq].reshape(nseq * Sq, D))
        in_maps.append(m)
    res = run_bass_kernel_spmd(nc, in_maps, core_ids=list(range(ncores)))
    out = np.stack([np.asarray(r["out"]).reshape(nseq, Sq, D) for r in res.results])
    return out.reshape(B, Sq, D).astype(np.float32)
```

```python
import numpy as np
import concourse.bass as bass
import concourse.mybir as mybir
from concourse.bass_utils import run_bass_kernel_spmd

F32 = mybir.dt.float32
BF16 = mybir.dt.bfloat16
AF = mybir.ActivationFunctionType
ALU = mybir.AluOpType
AX = mybir.AxisListType

ENG_ATTR = {"pe": "tensor", "act": "scalar", "dve": "vector", "pool": "gpsimd", "sp": "sync"}


class Buf:
    __slots__ = ("name", "w", "r", "excl")

    def __init__(self, name, excl=False):
        self.name = name
        self.w = None
        self.r = []
        self.excl = excl


class Sched:
    def __init__(self, nc, same_engine_sync=True):
        self.nc = nc
        self.same = same_engine_sync
        self.ops = {k: [] for k in ENG_ATTR}
        self.cnt = {k: 0 for k in ENG_ATTR}
        self.seen = {k: {} for k in ENG_ATTR}
        self.sems = {}
        self.dma_total = {}
        for k in ENG_ATTR:
            self.sems[k] = nc.alloc_semaphore("s_" + k)

    def dma_sem(self, key):
        if key not in self.sems:
            self.sems[key] = self.nc.alloc_semaphore("d_" + key)
            self.dma_total[key] = 0
        return key

    def _deps(self, eng, reads, writes):
        deps = {}
        for b in reads:
            if b.w is not None:
                k, v = b.w
                deps[k] = max(deps.get(k, 0), v)
            if b.excl:
                for (k, v) in b.r:
                    if k != eng:
                        deps[k] = max(deps.get(k, 0), v)
        for b in writes:
            if b.w is not None:
                k, v = b.w
                deps[k] = max(deps.get(k, 0), v)
            for (k, v) in b.r:
                deps[k] = max(deps.get(k, 0), v)
        seen = self.seen[eng]
        for k, v in deps.items():
            if k == eng and (eng == "pe" or not self.same):
                continue
            if seen.get(k, 0) < v:
                seen[k] = v
                sem = self.sems[k]
                self.ops[eng].append(lambda e, sem=sem, v=v: e.wait_ge(sem, v))

    def op(self, eng, fn, reads=(), writes=(), inc=True):
        self._deps(eng, reads, writes)
        if inc:
            self.cnt[eng] += 1
            tok = (eng, self.cnt[eng])
            sem = self.sems[eng]
            self.ops[eng].append(lambda e, fn=fn, sem=sem: fn(e).then_inc(sem, 1))
        else:
            tok = (eng, self.cnt[eng] + 1)
            self.ops[eng].append(lambda e, fn=fn: fn(e))
        for b in reads:
            b.r.append(tok)
        for b in writes:
            b.w = tok
            b.r = []
        return tok

    def dma(self, eng, semkey, fn, reads=(), writes=()):
        self.dma_sem(semkey)
        self._deps(eng, reads, writes)
        self.dma_total[semkey] += 16
        tok = (semkey, self.dma_total[semkey])
        sem = self.sems[semkey]
        self.ops[eng].append(lambda e, fn=fn, sem=sem: fn(e).then_inc(sem, 16))
        for b in reads:
            b.r.append(tok)
        for b in writes:
            b.w = tok
            b.r = []
        return tok

    def wait_all(self, eng, bufs):
        self._deps(eng, (), bufs)

    def emit(self):
        nc = self.nc
        with nc.Block() as block:
            for k, attr in ENG_ATTR.items():
                ops = self.ops[k]
                if not ops:
                    continue

                def body(e, ops=ops):
                    for f in ops:
                        f(e)
                getattr(block, attr)(body)


D = 1024
KC = 8
DFF = 2816
FC = 22
G = 512
TPG = 4
EPS = 1e-6
SLOT = 5632
NSLOT = 3
NEG = -30000.0


def ffn_piece_list():
    out = [("up%d" % j, 4096) for j in range(11)]
    out += [("dn%d_%d" % (cg, hf), 5632) for cg in range(2) for hf in range(2)]
    return out


def ffn_pieces_host(w_up, w_down):
    res = []
    wu = w_up.reshape(KC, 128, 2 * DFF)
    for j in range(11):
        cols = np.concatenate([np.arange(2 * j * 128, (2 * j + 2) * 128),
                               DFF + np.arange(2 * j * 128, (2 * j + 2) * 128)])
        pc = wu[:, :, cols].transpose(1, 0, 2).reshape(128, KC * 512)
        res.append(("up%d" % j, pc))
    wd = w_down.reshape(FC, 128, D)
    for cg in range(2):
        for hf in range(2):
            pc = wd[hf * 11:(hf + 1) * 11, :, cg * 512:(cg + 1) * 512].transpose(1, 0, 2).reshape(128, 11 * 512)
            res.append(("dn%d_%d" % (cg, hf), pc))
    return res


def const_layout():
    lay = {}
    off = 0

    def add(name, w):
        nonlocal off
        lay[name] = (off, w)
        off += w
    for L in range(2):
        add("nwT%d_0" % L, KC)
        add("nwT%d_2" % L, KC)
        add("nwR%d_1" % L, D)
        add("nwR%d_3" % L, D)
        add("cw%d" % L, FC * 3)
        add("cb%d" % L, FC)
    add("ident", 128)
    add("lbl", 8)
    add("sinks", 8)
    add("gw", 512)
    add("mc", 128)
    add("crep", 16)
    add("m0", 128)
    add("m4", 128)
    lay["_total"] = (off, 0)
    return lay


def consts_host(inp):
    lay = const_layout()
    c = np.zeros((128, lay["_total"][0]), np.float32)

    def put(name, arr):
        o, w = lay[name]
        c[:, o:o + w] = arr.reshape(128, w) if arr.shape[0] == 128 else np.broadcast_to(arr.reshape(1, w), (128, w))
    nw = inp["norm_w"]
    for L in range(2):
        put("nwT%d_0" % L, nw[L, 0].reshape(KC, 128).T)
        put("nwT%d_2" % L, nw[L, 2].reshape(KC, 128).T)
        put("nwR%d_1" % L, nw[L, 1])
        put("nwR%d_3" % L, nw[L, 3])
        cw = inp["ffn_conv_w"][L]
        put("cw%d" % L, cw.reshape(3, FC, 128).transpose(2, 1, 0).reshape(128, FC * 3))
        put("cb%d" % L, inp["ffn_conv_b"][L].reshape(FC, 128).T)
    put("ident", np.eye(128, dtype=np.float32))
    put("lbl", inp["hgrn_lb_logits"][0:2].reshape(2, 4, 128).transpose(2, 0, 1).reshape(128, 8))
    put("sinks", inp["even_sinks"][0])
    put("gw", inp["hgrn_norm_w"][0])
    put("mc", (np.arange(128)[:, None] <= np.arange(128)[None, :]).astype(np.float32))
    put("crep", inp["odd_rel_bias"][0][:, 256])
    jj = np.arange(128)[:, None]
    qq = np.arange(128)[None, :]
    put("m0", np.where((jj >= 64) & (qq < 64), NEG, 0.0).astype(np.float32))
    put("m4", np.where((jj < 64) & (qq >= 64), NEG, 0.0).astype(np.float32))
    return c


def relb_host(inp):
    tab = inp["odd_rel_bias"][0]
    jj = np.arange(128)[:, None]
    qq = np.arange(128)[None, :]
    i0 = (qq - jj) + 128
    i1 = np.minimum(128 + qq - jj, 128) + 128
    b0 = tab[:, i0]
    b1 = tab[:, i1]
    r = np.stack([b0, b1], axis=1)
    return np.ascontiguousarray(r.transpose(2, 0, 1, 3).reshape(128, 16 * 2 * 128).astype(np.float32))


def odd_piece_list():
    return [(n, 4096) for n in ("ik0", "ik1", "iv0", "iv1", "iq0", "iq1", "ow0", "ow1")]


def odd_pieces_host(w_in, w_out):
    wi = w_in.reshape(KC, 128, 3 * D)
    wo = w_out.reshape(KC, 128, D)
    res = []

    def pc(w, c0):
        return w[:, :, c0:c0 + 512].transpose(1, 0, 2).reshape(128, KC * 512)
    for i in range(2):
        res.append(("ik%d" % i, pc(wi, D + i * 512)))
    for i in range(2):
        res.append(("iv%d" % i, pc(wi, 2 * D + i * 512)))
    for i in range(2):
        res.append(("iq%d" % i, pc(wi, i * 512)))
    for i in range(2):
        res.append(("ow%d" % i, pc(wo, i * 512)))
    return res


def layer_piece_list(L):
    if L == 1:
        return odd_piece_list() + ffn_piece_list()
    return even_piece_list() + ffn_piece_list()


EVEN_FM = [["ka", "rka", "qa0", "rqa0"], ["qa1", "rqa1", "qa2", "rqa2"], ["qa3", "rqa3", "qb0", "fb0"],
           ["qb1", "fb1", "qb2", "fb2"], ["qb3", "fb3"]]


def even_piece_list():
    out = [("e5", KC * 640), ("e6", KC * 512)]
    out += [("e%d" % i, KC * 128 * len(ch)) for i, ch in enumerate(EVEN_FM)]
    out += [("ow0", 4096), ("ow1", 4096)]
    return out


def even_cols():
    rot = (np.arange(64) + 32) % 64
    cols = {}
    cols["ka"] = 512 + np.arange(128)
    cols["rka"] = 512 + np.concatenate([rot, 64 + rot])
    for c in range(4):
        h0, h1 = c, 4 + c
        cols["qa%d" % c] = np.concatenate([h0 * 64 + np.arange(64), h1 * 64 + np.arange(64)])
        cols["rqa%d" % c] = np.concatenate([h0 * 64 + rot, h1 * 64 + rot])
        cols["qb%d" % c] = 768 + c * 128 + np.arange(128)
        cols["fb%d" % c] = 1280 + c * 128 + np.arange(128)
    return cols


def even_pieces_host(inp):
    wi = inp["even_w_in"][0].reshape(KC, 128, 2816)
    wo = inp["even_w_out"][0].reshape(KC, 128, D)
    cols = even_cols()
    res = []
    cc = np.concatenate([640 + np.arange(128), 1792 + np.arange(512)])
    res.append(("e5", wi[:, :, cc].transpose(1, 0, 2).reshape(128, KC * 640)))
    cc = 2304 + np.arange(512)
    res.append(("e6", wi[:, :, cc].transpose(1, 0, 2).reshape(128, KC * 512)))
    for i, ch in enumerate(EVEN_FM):
        cc = np.concatenate([cols[n] for n in ch])
        res.append(("e%d" % i, wi[:, :, cc].transpose(1, 0, 2).reshape(128, KC * len(cc))))
    for i in range(2):
        res.append(("ow%d" % i, wo[:, :, i * 512:(i + 1) * 512].transpose(1, 0, 2).reshape(128, KC * 512)))
    return res


def rope_host(seq):
    inv = (10000.0 ** (-np.arange(0, 64, 2, dtype=np.float32) / 64)).astype(np.float32)
    ang = np.arange(seq, dtype=np.float32)[None, :] * inv[:, None]
    cos = np.cos(ang).astype(np.float32)
    sin = np.sin(ang).astype(np.float32)
    c64 = np.concatenate([cos, cos], 0)
    s64 = np.concatenate([-sin, sin], 0)
    t = np.stack([np.concatenate([c64, c64], 0), np.concatenate([s64, s64], 0)], axis=1)
    return np.ascontiguousarray(t.astype(np.float32))


def weights_host(inp):
    chunks = []
    for L in range(2):
        pcs = (odd_pieces_host(inp["odd_w_in"][0], inp["odd_w_out"][0]) if L == 1 else even_pieces_host(inp))
        pcs = pcs + ffn_pieces_host(inp["ffn_w_up"][L], inp["ffn_w_down"][L])
        want = layer_piece_list(L)
        assert [p[0] for p in pcs] == [w[0] for w in want]
        for (n, a), (_, nco) in zip(pcs, want):
            assert a.shape == (128, nco), (n, a.shape, nco)
            chunks.append(np.ascontiguousarray(a, dtype=np.float32).reshape(-1))
    return np.concatenate(chunks)


class K:
    pass


def build(nseq, seq, layers=(0, 1), parts=("mixer", "ffn")):
    nc = bass.Bass("TRN2", target_bir_lowering=False)
    NG = seq // G
    NTOK = nseq * seq
    lay = const_layout()
    NCST = lay["_total"][0]
    ptab = {}
    off = 0
    for L in range(2):
        for (n, nco) in layer_piece_list(L):
            ptab[(L, n)] = (off, nco)
            off += 128 * nco
    WTOT = off

    x_d = nc.dram_tensor("x", [NTOK, D], F32, kind="ExternalInput").ap()
    out_d = nc.dram_tensor("out", [NTOK, D], F32, kind="ExternalOutput").ap()
    wsrc_d = nc.dram_tensor("wsrc", [WTOT // 2048, 2048], F32, kind="ExternalInput").ap()
    cst_d = nc.dram_tensor("cst", [128, NCST], F32, kind="ExternalInput").ap()
    wbf_d = nc.dram_tensor("wbf", [WTOT // 2048, 2048], BF16, kind="Internal").ap()
    relb_d = nc.dram_tensor("relb", [128, 4096], F32, kind="ExternalInput").ap()
    rope_d = nc.dram_tensor("rope", [128, 2, seq], F32, kind="ExternalInput").ap()

    S = Sched(nc)
    k = K()
    k.nc, k.S = nc, S

    def sb(name, shape, dt):
        return nc.alloc_sbuf_tensor("s_" + name, shape, dt)

    cst = sb("cst", [128, NCST], F32)
    cstB = Buf("cst")
    ident = sb("ident", [128, 128], BF16)
    identB = Buf("ident")
    NH = 1
    h = [sb("h%d" % i, [128, TPG, D], F32) for i in range(NH)]
    hB = [[Buf("h%d_%d" % (i, t)) for t in range(TPG)] for i in range(NH)]
    ring = [sb("ring%d" % i, [128, SLOT], BF16) for i in range(NSLOT)]
    ringB = [Buf("ring%d" % i) for i in range(NSLOT)]
    yn = [sb("yn%d" % i, [128, D], BF16) for i in range(2)]
    ynB = [Buf("yn%d" % i) for i in range(2)]
    yT = sb("yT", [128, KC, G], BF16)
    yTB = [Buf("yT%d" % t) for t in range(TPG)]
    arena = sb("arena", [128, FC * G], BF16)
    gT = arena[:].rearrange("p (f t) -> p f t", f=FC)
    gTB = Buf("gT")
    qT = arena[:, 0:4096].rearrange("p (k t) -> p k t", k=KC)
    qTB = Buf("qT")
    otok = arena[:, 4096:8192].rearrange("p (t d) -> p t d", t=TPG)
    otokB = Buf("otok")
    PT = [arena[:, 8192 + i * 1280:8192 + (i + 1) * 1280] for i in range(2)]
    PTB = [Buf("PT%d" % i) for i in range(2)]
    PTB3 = [[Buf("PT%d_%d" % (i, j)) for j in range(3)] for i in range(2)]
    PTB3f = [b for l in PTB3 for b in l]
    mixBs = [qTB, otokB] + PTB + PTB3f

    def alias_fence(dsts, srcs):
        for d_ in dsts:
            for s_ in srcs:
                if s_.w is not None:
                    d_.r.append(s_.w)
                d_.r.extend(s_.r)
    kT = sb("kT", [128, KC, 2 * G], BF16)
    kTB = [Buf("kT%d" % i) for i in range(2)]
    Vr = sb("Vr", [128, 8, 16, 65], BF16)
    VB = [Buf("V%d" % i) for i in range(2)]
    bt = sb("bt", [128, 16, 2, 128], BF16)
    btB = Buf("bt")
    m4 = sb("m4", [128, 128], BF16)
    m4B = Buf("m4")
    rden = sb("rden", [128, 8], F32)
    rdenB = Buf("rden")
    qaT = arena[:, 0:2048].rearrange("p (k t) -> p k t", k=4)
    iTok = arena[:, 2048:4096].rearrange("p (t d) -> p t d", t=TPG)
    iTokB = Buf("iTok")
    PTa = [arena[:, 8192 + i * 512:8192 + (i + 1) * 512] for i in range(2)]
    sg = arena[:, 9216:11264].rearrange("p (t d) -> p t d", t=TPG)
    sgB = Buf("sg")
    mixBs.extend([iTokB, sgB])
    ropeT = sb("ropeT", [128, 2, G], F32)
    ropeB = Buf("rope")
    kaT = sb("kaT", [128, 2 * G], BF16)
    kaTB = [Buf("kaT%d" % i) for i in range(2)]
    Va = sb("Va", [128, 8, 2, 65], BF16)
    VaB = [Buf("Va%d" % i) for i in range(2)]
    esink = sb("esink", [128, 8], F32)
    lbv = sb("lbv", [128, 8], F32)
    evB = Buf("evconst")
    mcb = sb("mcb", [128, 128], BF16)
    m0b = sb("m0b", [128, 128], BF16)
    ones = sb("ones", [128, 128], F32)
    htmp = [sb("htmp%d" % i, [128, G], F32) for i in range(4)]
    htmpB = [Buf("htmp%d" % i) for i in range(4)]
    mfull = sb("mfull", [128, 2, 2, 128], BF16)
    qdT = [sb("qdT%d" % i, [128, G], BF16) for i in range(2)]
    qdTB = [Buf("qdT%d" % i) for i in range(2)]
    kdT = [sb("kdT%d" % i, [128, G], BF16) for i in range(2)]
    kdTB = [Buf("kdT%d" % i) for i in range(2)]
    kdTok = [sb("kdTok%d" % i, [128, TPG, 128], BF16) for i in range(2)]
    kdTokB = [Buf("kdTok%d" % i) for i in range(2)]
    sTm = [sb("sTm%d" % i, [128, 128], BF16) for i in range(4)]
    sTmB = [Buf("sTm%d" % i) for i in range(4)]
    state = sb("state", [128, 4, 128], F32)
    stS = sb("stS", [128, 4, 128], BF16)
    stateB = [Buf("state%d" % i) for i in range(4)]
    stSB = [Buf("stS%d" % i) for i in range(4)]
    tmpd = [sb("tmpd%d" % i, [128, 128], F32) for i in range(4)]
    tmpdB = [Buf("tmpd%d" % i) for i in range(4)]
    hvec = [sb("hvec%d" % i, [128, 4, 4], F32) for i in range(2)]
    hvecB = [Buf("hvec%d" % i) for i in range(2)]
    ssh = sb("ssh", [128, 16], F32)
    sshB = [Buf("ssh%d" % i) for i in range(16)]
    rsh = sb("rsh", [128, 16], F32)
    rshB = Buf("rsh")
    tmpo = sb("tmpo", [128, TPG, D], F32)
    tmpoB = [Buf("tmpo%d" % t) for t in range(TPG)]
    junks = [sb("junk%d" % i, [128, D], BF16) for i in range(2)]
    junkBs = [Buf("junk%d" % i) for i in range(2)]
    k.junk_i = 0

    def next_junk():
        k.junk_i += 1
        return junks[k.junk_i % 2], junkBs[k.junk_i % 2]
    csb = [sb("csb%d" % i, [128, G], F32) for i in range(2)]
    csbB = [Buf("csb%d" % i) for i in range(2)]
    gsb = [sb("gsb%d" % i, [128, G], F32) for i in range(2)]
    gsbB = [Buf("gsb%d" % i) for i in range(2)]
    carry = [sb("carry%d" % L, [128, FC, 2], F32) for L in range(2)]
    carryB = [Buf("carry%d" % L) for L in range(2)]
    ss4 = sb("ss4", [128, 4], F32)
    ss4B = [Buf("ss4_%d" % t) for t in range(TPG)]
    ssp = sb("ssp", [128, 4, 2], F32)
    sspB = [Buf("ssp%d" % t) for t in range(TPG)]
    rstd = sb("rstd", [128, 4], F32)
    rstdB = [Buf("rstd%d" % t) for t in range(TPG)]
    psum = [nc.alloc_psum_tensor("ps%d" % i, [128, 512], F32) for i in range(8)]
    psumB = [Buf("ps%d" % i, excl=True) for i in range(8)]
    k.bank_i = 0
    k.bank_n = 8

    def bank():
        i = k.bank_i % k.bank_n
        k.bank_i += 1
        return psum[i], psumB[i]

    def cs(name, a=None, b=None):
        o, w = lay[name]
        if a is None:
            return cst[:, o:o + w]
        return cst[:, o + a:o + b]

    S.dma("sp", "cst", lambda e: e.dma_start(out=cst[:], in_=cst_d), writes=[cstB])
    S.op("dve", lambda e: e.tensor_copy(out=ident[:], in_=cs("ident")), reads=[cstB], writes=[identB])
    if 1 in layers and "mixer" in parts:
        S.dma("sp", "relb", lambda e: e.dma_start(out=tmpo[:].rearrange("p t d -> p (t d)"), in_=relb_d), writes=tmpoB)
        S.op("pool", lambda e: e.memset(Vr[:], 1.0), writes=VB)
        S.op("act", lambda e: e.activation(out=m4[:], in_=cs("m4"), func=AF.Exp), reads=[cstB], writes=[m4B])
        rb = tmpo[:].rearrange("p t d -> p (t d)").rearrange("p (h k q) -> p h k q", h=16, k=2)
        for hh in range(16):
            S.op("dve", lambda e, hh=hh: e.scalar_tensor_tensor(out=rb[:, hh, 0, :], in0=rb[:, hh, 0, :], scalar=cs("crep", hh, hh + 1), op0=ALU.subtract, in1=cs("m0"), op1=ALU.add),
                 reads=tmpoB + [cstB], writes=tmpoB)
            S.op("dve", lambda e, hh=hh: e.tensor_scalar(out=rb[:, hh, 1, :], in0=rb[:, hh, 1, :], scalar1=cs("crep", hh, hh + 1), scalar2=None, op0=ALU.subtract),
                 reads=tmpoB + [cstB], writes=tmpoB)
            S.op("act", lambda e, hh=hh: e.activation(out=bt[:, hh, :, :], in_=rb[:, hh, :, :], func=AF.Exp), reads=tmpoB, writes=[btB])
    if 0 in layers and "mixer" in parts:
        S.op("pool", lambda e: e.memset(Va[:], 1.0), writes=VaB)
        S.op("pool", lambda e: e.memset(ones[:], 1.0), writes=[evB])
        S.op("dve", lambda e: e.tensor_copy(out=mcb[:], in_=cs("mc")), reads=[cstB], writes=[evB])
        if not (1 in layers and "mixer" in parts):
            S.op("act", lambda e: e.activation(out=m4[:], in_=cs("m4"), func=AF.Exp), reads=[cstB], writes=[m4B])
        S.op("act", lambda e: e.activation(out=m0b[:], in_=cs("m0"), func=AF.Exp), reads=[cstB], writes=[evB])
        for e_ in range(2):
            S.op("dve", lambda e, e_=e_: e.tensor_copy(out=mfull[:, e_, 0, :], in_=m4[:]), reads=[m4B], writes=[evB])
            S.op("dve", lambda e, e_=e_: e.tensor_copy(out=mfull[:, e_, 1, :], in_=m0b[:]), reads=[evB], writes=[evB])
        S.op("act", lambda e: e.activation(out=esink[:], in_=cs("sinks"), func=AF.Exp), reads=[cstB], writes=[evB])
        S.op("dve", lambda e: e.tensor_tensor(out=lbv[:, 4:8], in0=cs("lbl", 0, 4), in1=cs("lbl", 4, 8), op=ALU.subtract), reads=[cstB], writes=[evB])
        S.op("act", lambda e: e.activation(out=lbv[:, 0:4], in_=lbv[:, 4:8], func=AF.Sigmoid), reads=[evB], writes=[evB])
        S.op("dve", lambda e: e.tensor_scalar(out=lbv[:, 4:8], in0=lbv[:, 0:4], scalar1=-1.0, scalar2=1.0, op0=ALU.mult, op1=ALU.add), reads=[evB], writes=[evB])
    cvB = {}
    for L in layers:
        for (n, nco) in layer_piece_list(L):
            sec = n[:2]
            key = "cv%d%s" % (L, sec)
            cvB.setdefault(key, Buf(key))
            o, _ = ptab[(L, n)]
            r0, r1 = o // 2048, (o + 128 * nco) // 2048
            S.dma("pool", key, lambda e, r0=r0, r1=r1: e.dma_start(out=wbf_d[r0:r1, :], in_=wsrc_d[r0:r1, :]),
                  writes=[cvB[key]])

    order = []
    for s in range(nseq):
        for g in range(NG):
            for L in layers:
                for (n, nco) in layer_piece_list(L):
                    if (n[:2] in ("up", "dn")) and "ffn" not in parts:
                        continue
                    if (n[:2] not in ("up", "dn")) and "mixer" not in parts:
                        continue
                    order.append((L, n))
    k.w_issue = 0
    k.w_use = 0

    def w_issue_upto(i):
        while k.w_issue <= i and k.w_issue < len(order):
            j = k.w_issue
            L, n = order[j]
            o, nco = ptab[(L, n)]
            slot = j % NSLOT
            key = "cv%d%s" % (L, n[:2])
            src = wbf_d.rearrange("r c -> (r c)")[o:o + 128 * nco].rearrange("(p c) -> p c", p=128)
            S.dma("sp", "w%d" % slot, lambda e, slot=slot, nco=nco, src=src: e.dma_start(out=ring[slot][:, 0:nco], in_=src),
                  reads=[cvB[key]], writes=[ringB[slot]])
            k.w_issue += 1

    def w_get(L, n):
        i = k.w_use
        assert order[i] == (L, n), (order[i], L, n)
        w_issue_upto(i + NSLOT - 1)
        k.w_use += 1
        return ring[i % NSLOT], ringB[i % NSLOT]

    def rstd_tile(t):
        S.op("act", lambda e: e.activation(out=rstd[:, t:t + 1], in_=ss4[:, t:t + 1], func=AF.Sqrt, scale=1.0 / D, bias=EPS),
             reads=[ss4B[t]], writes=[rstdB[t]])
        S.op("dve", lambda e: e.reciprocal(out=rstd[:, t:t + 1], in_=rstd[:, t:t + 1]), reads=[rstdB[t]], writes=[rstdB[t]])

    def norm_tile(hb, hbB, nwT, t):
        junk, junkB = next_junk()
        S.op("act", lambda e: e.activation(out=junk[:], in_=hb[:, t, :], func=AF.Square, accum_out=ss4[:, t:t + 1]),
             reads=[hbB[t]], writes=[ss4B[t], junkB])
        rstd_tile(t)
        y, yB = yn[t % 2], ynB[t % 2]
        S.op("act", lambda e: e.activation(out=y[:], in_=hb[:, t, :], func=AF.Identity, scale=rstd[:, t:t + 1]),
             reads=[hbB[t], rstdB[t]], writes=[yB])
        p, pB = bank()
        pb = p[:].bitcast(BF16)
        for kc in range(KC):
            S.op("pe", lambda e, kc=kc: e.transpose(out=pb[:, kc * 128:(kc + 1) * 128], in_=y[:, kc * 128:(kc + 1) * 128], identity=ident[:]),
                 reads=[yB, identB], writes=[pB], inc=(kc == KC - 1))
        S.op("dve", lambda e: e.tensor_tensor(out=yT[:, :, t * 128:(t + 1) * 128], in0=pb.rearrange("p (k t) -> p k t", k=KC),
                                              in1=nwT.unsqueeze(2).broadcast_to([128, KC, 128]), op=ALU.mult),
             reads=[pB, cstB], writes=[yTB[t]])

    def norm_transpose(hb, hbB, nwT):
        for t in range(TPG):
            norm_tile(hb, hbB, nwT, t)

    def post_tile(hb, hbB, t):
        S.op("dve", lambda e: e.tensor_tensor(out=ss4[:, t:t + 1], in0=ssp[:, t, 0:1], in1=ssp[:, t, 1:2], op=ALU.add), reads=[sspB[t]], writes=[ss4B[t]])
        rstd_tile(t)
        S.op("dve", lambda e: e.scalar_tensor_tensor(out=hb[:, t, :], in0=tmpo[:, t, :], scalar=rstd[:, t:t + 1], op0=ALU.mult, in1=hb[:, t, :], op1=ALU.add),
             reads=[tmpoB[t], rstdB[t], hbB[t]], writes=[hbB[t]])

    def post_norm_residual(hb, hbB, nwR_name, produce, stage=9):
        for cg in range(2):
            banks = produce(cg)
            for t in range(TPG):
                p, pB = banks[t]
                junk, junkB = next_junk()
                S.op("act", lambda e, t=t, cg=cg, p=p, junk=junk: e.activation(out=junk[:, 0:512], in_=p[:], func=AF.Square, accum_out=ssp[:, t, cg:cg + 1]),
                     reads=[pB], writes=[sspB[t], junkB])
                S.op("dve", lambda e, t=t, cg=cg, p=p: e.tensor_tensor(out=tmpo[:, t, cg * 512:(cg + 1) * 512], in0=p[:], in1=cs(nwR_name, cg * 512, (cg + 1) * 512), op=ALU.mult),
                     reads=[pB, cstB], writes=[tmpoB[t]])
                if cg == 1:
                    post_tile(hb, hbB, t)

    def evac(i, out, in_, reads, writes, scale=None):
        if i % 2 == 0:
            if scale is None:
                S.op("act", lambda e: e.activation(out=out, in_=in_, func=AF.Identity), reads=reads, writes=writes)
            else:
                S.op("act", lambda e: e.activation(out=out, in_=in_, func=AF.Identity, scale=scale), reads=reads, writes=writes)
        else:
            if scale is None:
                S.op("dve", lambda e: e.tensor_copy(out=out, in_=in_), reads=reads, writes=writes)
            else:
                S.op("dve", lambda e: e.tensor_scalar(out=out, in0=in_, scalar1=scale, scalar2=None, op0=ALU.mult), reads=reads, writes=writes)

    def proj_fm(wv, wB, c, dst, dstB, ei, scale=None):
        p, pB = bank()
        for kc in range(KC):
            S.op("pe", lambda e, kc=kc: e.matmul(p[:], lhsT=wv[:, kc, c * 128:(c + 1) * 128], rhs=yT[:, kc, :], start=(kc == 0), stop=(kc == KC - 1)),
                 reads=[wB] + yTB, writes=[pB], inc=(kc == KC - 1))
        evac(ei, dst, p[:], [pB], [dstB], scale)

    def otok_to_yT(qt):
        p, pB = bank()
        pb = p[:].bitcast(BF16)
        for kc in range(KC):
            S.op("pe", lambda e, kc=kc: e.transpose(out=pb[:, kc * 128:(kc + 1) * 128], in_=otok[:, qt, kc * 128:(kc + 1) * 128], identity=ident[:]),
                 reads=[otokB, identB], writes=[pB], inc=(kc == KC - 1))
        evac(qt, yT[:, :, qt * 128:(qt + 1) * 128], pb.rearrange("p (k t) -> p k t", k=KC), [pB], [yTB[qt]])

    def out_proj_post(L, hb, hbB, nwR_name):
        def produce(cg):
            wt, wB = w_get(L, "ow%d" % cg)
            wv = wt[:, 0:4096].rearrange("p (k c) -> p k c", k=KC)
            banks = [bank() for _ in range(TPG)]
            for t in range(TPG):
                p, pB = banks[t]
                for kc in range(KC):
                    S.op("pe", lambda e, t=t, kc=kc, p=p: e.matmul(p[:], lhsT=yT[:, kc, t * 128:(t + 1) * 128], rhs=wv[:, kc, :], start=(kc == 0), stop=(kc == KC - 1)),
                         reads=[wB, yTB[t]], writes=[pB], inc=(kc == KC - 1))
            return banks
        post_norm_residual(hb, hbB, nwR_name, produce)

    def odd_mixer(L, hb, hbB, g):
        alias_fence(mixBs, [gTB])
        norm_transpose(hb, hbB, cs("nwT%d_0" % L))
        ks = g % 2
        ei = 0
        for i in range(2):
            wt, wB = w_get(L, "ik%d" % i)
            wv = wt[:, 0:4096].rearrange("p (k c) -> p k c", k=KC)
            for c in range(4):
                proj_fm(wv, wB, c, kT[:, 4 * i + c, ks * G:(ks + 1) * G], kTB[ks], ei)
                ei += 1
        for i in range(2):
            wt, wB = w_get(L, "iv%d" % i)
            wv = wt[:, 0:4096].rearrange("p (k c) -> p k c", k=KC)
            for t in range(TPG):
                p, pB = bank()
                for kc in range(KC):
                    S.op("pe", lambda e, t=t, kc=kc, p=p, wv=wv: e.matmul(p[:], lhsT=yT[:, kc, t * 128:(t + 1) * 128], rhs=wv[:, kc, :], start=(kc == 0), stop=(kc == KC - 1)),
                         reads=[wB, yTB[t]], writes=[pB], inc=(kc == KC - 1))
                evac(ei, Vr[:, ks * 4 + t, 8 * i:8 * i + 8, 0:64], p[:].rearrange("p (h d) -> p h d", d=64), [pB], [VB[ks]])
                ei += 1
        for i in range(2):
            wt, wB = w_get(L, "iq%d" % i)
            wv = wt[:, 0:4096].rearrange("p (k c) -> p k c", k=KC)
            for c in range(4):
                proj_fm(wv, wB, c, qT[:, 4 * i + c, :], qTB, ei, scale=0.125)
                ei += 1
        k.bank_n = 5
        alias_fence(PTB3f, PTB)
        units = [(qt, hp) for qt in range(TPG) for hp in range(8)]
        U = {}

        def st_phase(u):
            qt, hp = units[u]
            T = g * 4 + qt
            kts = [kt for kt in range(T - 4, T + 1) if kt >= 0]
            sbk = [bank(), bank(), bank()]
            pt, ptBs = PT[u % 2], PTB3[u % 2]
            stops = []
            fixes = []
            seq_ = [(kt, 0) for kt in kts] + [(None, None)] + [(kt, 1) for kt in kts]
            for (kt, e_) in seq_:
                if kt is None:
                    stops.append((lambda e, sbk=sbk: e.matmul(sbk[2][0][:, 256:258], lhsT=ident[:], rhs=ident[:, 0:2], start=True, stop=True),
                                  [identB], [sbk[2][1]]))
                    continue
                hh = 2 * hp + e_
                lo, hi = e_ * 64, (e_ + 1) * 64
                j = kt - (T - 4)
                if j < 4:
                    reg, regB = sbk[e_][0][:, j * 128:(j + 1) * 128], sbk[e_][1]
                    pcol, part = e_ * 512 + j * 128, e_
                elif len(kts) == 1 and e_ == 1:
                    reg, regB = sbk[1][0][:, 0:128], sbk[1][1]
                    pcol, part = 1024 + e_ * 128, 2
                else:
                    reg, regB = sbk[2][0][:, e_ * 128:(e_ + 1) * 128], sbk[2][1]
                    pcol, part = 1024 + e_ * 128, 2
                kslot = (kt // 4) % 2
                k0 = kslot * G + (kt % 4) * 128
                delta = T - kt
                stops.append((lambda e, reg=reg, lo=lo, hi=hi, k0=k0, hp=hp, qt=qt: e.matmul(reg, lhsT=kT[lo:hi, hp, k0:k0 + 128], rhs=qT[lo:hi, hp, qt * 128:(qt + 1) * 128], start=True, stop=True),
                              [kTB[kslot], qTB], [regB]))
                if delta in (0, 1, 4):
                    fac = bt[:, hh, 0, :] if delta == 0 else (bt[:, hh, 1, :] if delta == 1 else m4[:])
                    fixes.append((pcol, part, fac))
            for si, (fn_, rd_, wr_) in enumerate(stops):
                S.op("pe", fn_, reads=rd_, writes=wr_, inc=(si == len(stops) - 1))
            U[u] = dict(sbk=sbk, pt=pt, ptBs=ptBs, fixes=fixes, kts=kts, T=T)


        def ex_phase(u):
            d = U[u]
            sbk, pt, ptBs, kts = d["sbk"], d["pt"], d["ptBs"], d["kts"]
            c0 = (5 - len(kts)) * 128
            if c0 < 512:
                for e_ in range(2):
                    S.op("act", lambda e, e_=e_: e.activation(out=pt[:, e_ * 512 + c0:(e_ + 1) * 512], in_=sbk[e_][0][:, c0:512], func=AF.Exp),
                         reads=[sbk[e_][1]], writes=[ptBs[e_]])
            if len(kts) == 1:
                S.op("act", lambda e: e.activation(out=pt[:, 1024:1152], in_=sbk[2][0][:, 0:128], func=AF.Exp), reads=[sbk[2][1]], writes=[ptBs[2]])
                S.op("act", lambda e: e.activation(out=pt[:, 1152:1280], in_=sbk[1][0][:, 0:128], func=AF.Exp), reads=[sbk[1][1]], writes=[ptBs[2]])
            else:
                S.op("act", lambda e: e.activation(out=pt[:, 1024:1280], in_=sbk[2][0][:, 0:256], func=AF.Exp), reads=[sbk[2][1]], writes=[ptBs[2]])
            for (pcol, part, fac) in d["fixes"]:
                S.op("dve", lambda e, pcol=pcol, fac=fac: e.tensor_tensor(out=pt[:, pcol:pcol + 128], in0=pt[:, pcol:pcol + 128], in1=fac, op=ALU.mult),
                     reads=[ptBs[part], btB, m4B], writes=[ptBs[part]])

        def pv_phase(u):
            d = U[u]
            pt, ptBs, kts, T = d["pt"], d["ptBs"], d["kts"], d["T"]
            qt, hp = units[u]
            for e_ in range(2):
                hh = 2 * hp + e_
                ob, obB = psum[5 + hh // 6], psumB[5 + hh // 6]
                col = (hh % 6) * 65
                for idx, kt in enumerate(kts):
                    j = kt - (T - 4)
                    pc = pt[:, e_ * 512 + j * 128:e_ * 512 + (j + 1) * 128] if j < 4 else pt[:, 1024 + e_ * 128:1024 + (e_ + 1) * 128]
                    last = (idx == len(kts) - 1)
                    S.op("pe", lambda e, pc=pc, kt=kt, hh=hh, idx=idx, last=last, ob=ob, col=col: e.matmul(ob[:, col:col + 65], lhsT=pc, rhs=Vr[:, kt % 8, hh, :], start=(idx == 0), stop=last),
                         reads=[ptBs[e_], ptBs[2], VB[(kt // 4) % 2]], writes=[obB], inc=(last and e_ == 1))

        def fin_qt(qt):
            for b in range(3):
                nh = 6 if b < 2 else 4
                ob, obB = psum[5 + b], psumB[5 + b]
                view = ob[:, 0:nh * 65].rearrange("p (h c) -> p h c", c=65)
                S.op("dve", lambda e, view=view, nh=nh: e.reciprocal(out=rden[:, 0:nh], in_=view[:, :, 64]), reads=[obB], writes=[rdenB])
                S.op("dve", lambda e, view=view, nh=nh, b=b: e.tensor_tensor(out=otok[:, qt, b * 384:b * 384 + nh * 64].rearrange("p (h d) -> p h d", d=64),
                                                                            in0=view[:, :, 0:64], in1=rden[:, 0:nh].unsqueeze(2).broadcast_to([128, nh, 64]), op=ALU.mult),
                     reads=[obB, rdenB], writes=[otokB])
            otok_to_yT(qt)

        st_phase(0)
        for u in range(len(units)):
            ex_phase(u)
            if u + 1 < len(units):
                st_phase(u + 1)
            pv_phase(u)
            if units[u][1] == 7:
                fin_qt(units[u][0])
        k.bank_n = 8
        out_proj_post(L, hb, hbB, "nwR%d_1" % L)

    def mm_group(p, pB, lhs_fn, rhs_fn, reads, n=KC):
        for kc in range(n):
            S.op("pe", lambda e, kc=kc: e.matmul(p, lhsT=lhs_fn(kc), rhs=rhs_fn(kc), start=(kc == 0), stop=(kc == n - 1)),
                 reads=reads, writes=[pB], inc=(kc == n - 1))

    def rope_chunk(wv, wB, c, dst, dstB):
        pa, paB = bank()
        mm_group(pa[:], paB, lambda kc: wv[:, kc, c * 128:(c + 1) * 128], lambda kc: yT[:, kc, :], [wB] + yTB)
        pr, prB = bank()
        mm_group(pr[:], prB, lambda kc: wv[:, kc, (c + 1) * 128:(c + 2) * 128], lambda kc: yT[:, kc, :], [wB] + yTB)
        t1, t1B = csb[0], csbB[0]
        t2, t2B = csb[1], csbB[1]
        S.op("dve", lambda e: e.tensor_tensor(out=t1[:], in0=pa[:], in1=ropeT[:, 0, :], op=ALU.mult), reads=[paB, ropeB], writes=[t1B])
        S.op("dve", lambda e: e.tensor_tensor(out=t2[:], in0=pr[:], in1=ropeT[:, 1, :], op=ALU.mult), reads=[prB, ropeB], writes=[t2B])
        S.op("pool", lambda e: e.tensor_tensor(out=dst, in0=t1[:], in1=t2[:], op=ALU.add), reads=[t1B, t2B], writes=[dstB])

    def hgrn_head(hd, wvq, wBq, cq, wvf, wBf, cf, g, first):
        i2 = hd % 2
        pq, pqB = bank()
        mm_group(pq[:], pqB, lambda kc: wvq[:, kc, cq * 128:(cq + 1) * 128], lambda kc: yT[:, kc, :], [wBq] + yTB)
        pf, pfB = bank()
        mm_group(pf[:], pfB, lambda kc: wvf[:, kc, cf * 128:(cf + 1) * 128], lambda kc: yT[:, kc, :], [wBf] + yTB)
        yield
        if i2 == 0:
            fa, faB = csb[0], csbB[0]
            lc, lcB = csb[1], csbB[1]
            e1, e1B = gsb[0], gsbB[0]
            e2, e2B = gsb[1], gsbB[1]
        else:
            fa, faB = htmp[0], htmpB[0]
            lc, lcB = htmp[1], htmpB[1]
            e1, e1B = htmp[2], htmpB[2]
            e2, e2B = htmp[3], htmpB[3]
        hv, hvB = hvec[i2], hvecB[i2]
        S.op("act", lambda e: e.activation(out=fa[:], in_=pf[:], func=AF.Sigmoid), reads=[pfB], writes=[faB])
        yield
        S.op("dve", lambda e: e.tensor_scalar(out=fa[:], in0=fa[:], scalar1=lbv[:, 4 + hd:5 + hd], scalar2=lbv[:, hd:hd + 1], op0=ALU.mult, op1=ALU.add),
             reads=[faB, evB], writes=[faB])
        yield
        S.op("act", lambda e: e.activation(out=e1[:], in_=fa[:], func=AF.Ln), reads=[faB], writes=[e1B])
        yield
        for t in range(TPG):
            S.op("dve", lambda e, t=t: e.tensor_tensor_scan(out=lc[:, t * 128:(t + 1) * 128], data0=ones[:], data1=e1[:, t * 128:(t + 1) * 128], initial=0.0, op0=ALU.mult, op1=ALU.add),
                 reads=[e1B, evB], writes=[lcB])
        yield
        lc3 = lc[:].rearrange("p (t m) -> p t m", t=TPG)
        S.op("dve", lambda e: e.tensor_copy(out=hv[:, :, 0], in_=lc3[:, :, 63]), reads=[lcB], writes=[hvB])
        S.op("act", lambda e: e.activation(out=hv[:, :, 1], in_=lc3[:, :, 63], func=AF.Exp), reads=[lcB], writes=[hvB])
        S.op("act", lambda e: e.activation(out=hv[:, :, 2], in_=lc3[:, :, 127], func=AF.Exp), reads=[lcB], writes=[hvB])
        yield
        S.op("dve", lambda e: e.tensor_tensor(out=lc3, in0=lc3, in1=hv[:, :, 0:1].broadcast_to([128, TPG, 128]), op=ALU.subtract), reads=[lcB, hvB], writes=[lcB])
        yield
        S.op("act", lambda e: e.activation(out=hv[:, :, 3], in_=lc3[:, :, 127], func=AF.Exp), reads=[lcB], writes=[hvB])
        S.op("act", lambda e: e.activation(out=e1[:], in_=lc[:], func=AF.Exp), reads=[lcB], writes=[e1B])
        S.op("act", lambda e: e.activation(out=e2[:], in_=lc[:], func=AF.Exp, scale=-1.0), reads=[lcB], writes=[e2B])
        yield
        qd, qdB = qdT[i2], qdTB[i2]
        kd, kdB = kdT[i2], kdTB[i2]
        S.op("dve", lambda e: e.tensor_tensor(out=qd[:], in0=pq[:], in1=e1[:], op=ALU.mult), reads=[pqB, e1B], writes=[qdB])
        S.op("act", lambda e: e.activation(out=fa[:], in_=pf[:], func=AF.Sigmoid, scale=-1.0), reads=[pfB, faB], writes=[faB])
        S.op("dve", lambda e: e.scalar_tensor_tensor(out=kd[:], in0=fa[:], scalar=lbv[:, 4 + hd:5 + hd], op0=ALU.mult, in1=e2[:], op1=ALU.mult), reads=[faB, e2B, evB], writes=[kdB])
        yield
        kk, kkB = kdTok[i2], kdTokB[i2]
        pt_, ptB_ = bank()
        ptb = pt_[:].bitcast(BF16)
        for t in range(TPG):
            S.op("pe", lambda e, t=t: e.transpose(out=ptb[:, t * 128:(t + 1) * 128], in_=kd[:, t * 128:(t + 1) * 128], identity=ident[:]),
                 reads=[kdB, identB], writes=[ptB_], inc=(t == TPG - 1))
        evac(hd, kk[:].rearrange("p t d -> p (t d)"), ptb[:, 0:512], [ptB_], [kkB])

    def hgrn_tile(hd, t, g, first):
        i2 = hd % 2
        hv, hvB = hvec[i2], hvecB[i2]
        qd, qdB = qdT[i2], qdTB[i2]
        kd, kdB = kdT[i2], kdTB[i2]
        kk, kkB = kdTok[i2], kdTokB[i2]
        tile0 = first and t == 0
        upd = not (g == NG - 1 and t == TPG - 1)
        sl = slice(t * 128, (t + 1) * 128)
        vv = iTok[:, t, hd * 128:(hd + 1) * 128]
        bi = i2 * 2 + (t % 2)
        sm, smB = sTm[bi], sTmB[bi]
        td_, tdB_ = tmpd[bi], tmpdB[bi]
        ps_, psB_ = bank()
        S.op("pe", lambda e: e.matmul(ps_[:, 0:128], lhsT=kd[:, sl], rhs=qd[:, sl], start=True, stop=True), reads=[kdB, qdB], writes=[psB_])
        if upd:
            pd, pdB = bank()
            S.op("pe", lambda e: e.matmul(pd[:, 0:128], lhsT=kk[:, t, :], rhs=vv, start=True, stop=True), reads=[kkB, iTokB], writes=[pdB])
        yield
        if not tile0:
            S.op("act", lambda e: e.activation(out=stS[:, hd, :], in_=state[:, hd, :], func=AF.Identity, scale=hv[:, t, 1:2]),
                 reads=[stateB[hd], hvB], writes=[stSB[hd]])
        S.op("dve", lambda e: e.tensor_tensor(out=sm[:], in0=ps_[:, 0:128], in1=mcb[:], op=ALU.mult), reads=[psB_, evB], writes=[smB])
        if upd and not tile0:
            S.op("dve", lambda e: e.tensor_scalar(out=td_[:], in0=pd[:, 0:128], scalar1=hv[:, t, 3:4], scalar2=None, op0=ALU.mult),
                 reads=[pdB, hvB], writes=[tdB_])
        yield
        po, poB = bank()
        S.op("pe", lambda e: e.matmul(po[:, 0:128], lhsT=sm[:], rhs=vv, start=True, stop=tile0), reads=[smB, iTokB], writes=[poB], inc=tile0)
        if not tile0:
            S.op("pe", lambda e: e.matmul(po[:, 0:128], lhsT=qd[:, sl], rhs=stS[:, hd, :], start=False, stop=True), reads=[qdB, stSB[hd]], writes=[poB])
        if upd:
            if tile0:
                S.op("dve", lambda e: e.tensor_scalar(out=state[:, hd, :], in0=pd[:, 0:128], scalar1=hv[:, t, 3:4], scalar2=None, op0=ALU.mult),
                     reads=[pdB, hvB], writes=[stateB[hd]])
            else:
                S.op("dve", lambda e: e.scalar_tensor_tensor(out=state[:, hd, :], in0=state[:, hd, :], scalar=hv[:, t, 2:3], op0=ALU.mult, in1=td_[:], op1=ALU.add),
                     reads=[tdB_, hvB, stateB[hd]], writes=[stateB[hd]])
        yield
        junk, junkB = next_junk()
        S.op("act", lambda e: e.activation(out=junk[:, 0:128], in_=po[:, 0:128], func=AF.Square, accum_out=ssh[:, t * 4 + hd:t * 4 + hd + 1]),
             reads=[poB], writes=[sshB[t * 4 + hd], junkB])
        S.op("dve", lambda e: e.tensor_copy(out=tmpo[:, t, hd * 128:(hd + 1) * 128], in_=po[:, 0:128]), reads=[poB], writes=[tmpoB[t]])

    def even_mixer(L, hb, hbB, g):
        first = (g == 0)
        alias_fence(mixBs, [gTB])
        r0 = g * G
        S.dma("sp", "rope", lambda e: e.dma_start(out=ropeT[:], in_=rope_d[:, :, r0:r0 + G]), writes=[ropeB])
        norm_transpose(hb, hbB, cs("nwT%d_0" % L))
        ks = g % 2
        def piece(i):
            wt, wB = w_get(L, "e%d" % i)
            n = len(EVEN_FM[i])
            return wt[:, 0:KC * 128 * n].rearrange("p (k c) -> p k c", k=KC), wB
        wt5, wB5 = w_get(L, "e5")
        w5 = wt5[:, 0:KC * 640].rearrange("p (k c) -> p k c", k=KC)
        for t in range(TPG):
            p, pB = bank()
            mm_group(p[:, 0:128], pB, lambda kc, t=t: yT[:, kc, t * 128:(t + 1) * 128], lambda kc: w5[:, kc, 0:128], [wB5, yTB[t]])
            evac(t, Va[:, ks * 4 + t, :, 0:64], p[:, 0:128].rearrange("p (h d) -> p h d", d=64), [pB], [VaB[ks]])
            p, pB = bank()
            mm_group(p[:], pB, lambda kc, t=t: yT[:, kc, t * 128:(t + 1) * 128], lambda kc: w5[:, kc, 128:640], [wB5, yTB[t]])
            evac(t + 1, iTok[:, t, :], p[:], [pB], [iTokB])
        wt6, wB6 = w_get(L, "e6")
        w6 = wt6[:, 0:KC * 512].rearrange("p (k c) -> p k c", k=KC)
        for t in range(TPG):
            p, pB = bank()
            mm_group(p[:], pB, lambda kc, t=t: yT[:, kc, t * 128:(t + 1) * 128], lambda kc: w6[:, kc, :], [wB6, yTB[t]])
            S.op("act", lambda e, p=p, t=t: e.activation(out=sg[:, t, :], in_=p[:], func=AF.Silu), reads=[pB], writes=[sgB])
            S.op("pool", lambda e, t=t: e.tensor_tensor(out=sg[:, t, :], in0=sg[:, t, :], in1=cs("gw"), op=ALU.mult), reads=[sgB, cstB], writes=[sgB])
        wv, wB = piece(0)
        rope_chunk(wv, wB, 0, kaT[:, ks * G:(ks + 1) * G], kaTB[ks])
        rope_chunk(wv, wB, 2, qaT[:, 0, :], qTB)
        wv, wB = piece(1)
        rope_chunk(wv, wB, 0, qaT[:, 1, :], qTB)
        rope_chunk(wv, wB, 2, qaT[:, 2, :], qTB)
        wv2, wB2 = piece(2)
        rope_chunk(wv2, wB2, 0, qaT[:, 3, :], qTB)
        k.bank_n = 6
        alias_fence(PTB, PTB3f)
        units = [(qt, c) for qt in range(TPG) for c in range(4)]
        U = {}

        def st_phase(u):
            qt, c = units[u]
            T = g * 4 + qt
            kts = [kt for kt in (T - 1, T) if kt >= 0]
            sb2 = [bank(), bank()]
            ops_ = []
            for e_ in range(2):
                if e_ == 1:
                    ops_.append((lambda e, sb2=sb2: e.matmul(sb2[1][0][:, 256:258], lhsT=ident[:], rhs=ident[:, 0:2], start=True, stop=True),
                                 [identB], [sb2[1][1]]))
                for kt in kts:
                    lo, hi = e_ * 64, (e_ + 1) * 64
                    j = kt - (T - 1)
                    reg = sb2[e_][0][:, j * 128:(j + 1) * 128]
                    kslot = (kt // 4) % 2
                    k0 = kslot * G + (kt % 4) * 128
                    ops_.append((lambda e, reg=reg, lo=lo, hi=hi, k0=k0, c=c, qt=qt: e.matmul(reg, lhsT=kaT[lo:hi, k0:k0 + 128], rhs=qaT[lo:hi, c, qt * 128:(qt + 1) * 128], start=True, stop=True),
                                 [kaTB[kslot], qTB], [sb2[e_][1]]))
            for si, (fn_, rd_, wr_) in enumerate(ops_):
                S.op("pe", fn_, reads=rd_, writes=wr_, inc=(si == len(ops_) - 1))
            U[u] = dict(sb2=sb2, kts=kts, T=T)

        def ex_phase(u):
            d = U[u]
            sb2, kts = d["sb2"], d["kts"]
            pt, ptB = PTa[u % 2], PTB[u % 2]
            j0 = 2 - len(kts)
            for e_ in range(2):
                S.op("act", lambda e, e_=e_: e.activation(out=pt[:, e_ * 256 + j0 * 128:(e_ + 1) * 256], in_=sb2[e_][0][:, j0 * 128:256], func=AF.Exp, scale=0.125),
                     reads=[sb2[e_][1]], writes=[ptB])
            S.op("dve", lambda e: e.tensor_tensor(out=pt.rearrange("p (e j q) -> p e j q", e=2, j=2)[:, :, j0:2, :],
                                                  in0=pt.rearrange("p (e j q) -> p e j q", e=2, j=2)[:, :, j0:2, :], in1=mfull[:, :, j0:2, :], op=ALU.mult),
                 reads=[ptB, evB], writes=[ptB])

        def pv_phase(u):
            d = U[u]
            kts, T = d["kts"], d["T"]
            qt, c = units[u]
            pt, ptB = PTa[u % 2], PTB[u % 2]
            for e_ in range(2):
                ob, obB = psum[6 + e_], psumB[6 + e_]
                col = c * 65
                for idx, kt in enumerate(kts):
                    j = kt - (T - 1)
                    pc = pt[:, e_ * 256 + j * 128:e_ * 256 + (j + 1) * 128]
                    last = (idx == len(kts) - 1)
                    S.op("pe", lambda e, pc=pc, kt=kt, e_=e_, idx=idx, last=last, ob=ob, col=col: e.matmul(ob[:, col:col + 65], lhsT=pc, rhs=Va[:, kt % 8, e_, :], start=(idx == 0), stop=last),
                         reads=[ptB, VaB[(kt // 4) % 2]], writes=[obB], inc=(last and e_ == 1))

        def fin_qt(qt):
            for b in range(2):
                ob, obB = psum[6 + b], psumB[6 + b]
                view = ob[:, 0:260].rearrange("p (h c) -> p h c", c=65)
                S.op("dve", lambda e, view=view, b=b: e.tensor_tensor(out=rden[:, 0:4], in0=view[:, :, 64], in1=esink[:, 4 * b:4 * b + 4], op=ALU.add), reads=[obB, evB], writes=[rdenB])
                S.op("dve", lambda e: e.reciprocal(out=rden[:, 0:4], in_=rden[:, 0:4]), reads=[rdenB], writes=[rdenB])
                S.op("dve", lambda e, view=view, b=b: e.tensor_tensor(out=otok[:, qt, b * 256:(b + 1) * 256].rearrange("p (h d) -> p h d", d=64),
                                                                      in0=view[:, :, 0:64], in1=rden[:, 0:4].unsqueeze(2).broadcast_to([128, 4, 64]), op=ALU.mult),
                     reads=[obB, rdenB], writes=[otokB])

        st_phase(0)
        for u in range(len(units)):
            ex_phase(u)
            if u + 1 < len(units):
                st_phase(u + 1)
            pv_phase(u)
            if units[u][1] == 3:
                fin_qt(units[u][0])
        k.bank_n = 8
        def run_pair(ga, gb):
            alive = [ga, gb]
            while alive:
                for gn in list(alive):
                    try:
                        next(gn)
                    except StopIteration:
                        alive.remove(gn)
        g0 = hgrn_head(0, wv2, wB2, 2, wv2, wB2, 3, g, first)
        next(g0)
        wv3, wB3 = piece(3)
        g1 = hgrn_head(1, wv3, wB3, 0, wv3, wB3, 1, g, first)
        next(g1)
        run_pair(g0, g1)
        for t in range(TPG):
            run_pair(hgrn_tile(0, t, g, first), hgrn_tile(1, t, g, first))
        g2 = hgrn_head(2, wv3, wB3, 2, wv3, wB3, 3, g, first)
        next(g2)
        wv4, wB4 = piece(4)
        g3 = hgrn_head(3, wv4, wB4, 0, wv4, wB4, 1, g, first)
        next(g3)
        run_pair(g2, g3)
        for t in range(TPG):
            run_pair(hgrn_tile(2, t, g, first), hgrn_tile(3, t, g, first))
        S.op("act", lambda e: e.activation(out=rsh[:], in_=ssh[:], func=AF.Sqrt, scale=1.0 / 128, bias=EPS), reads=sshB, writes=[rshB])
        S.op("dve", lambda e: e.reciprocal(out=rsh[:], in_=rsh[:]), reads=[rshB], writes=[rshB])
        for t in range(TPG):
            o3 = tmpo[:, t, 0:512].rearrange("p (h d) -> p h d", h=4)
            S.op("dve", lambda e, t=t, o3=o3: e.tensor_tensor(out=o3, in0=o3, in1=rsh[:, t * 4:(t + 1) * 4].unsqueeze(2).broadcast_to([128, 4, 128]), op=ALU.mult),
                 reads=[tmpoB[t], rshB], writes=[tmpoB[t]])
            S.op("dve", lambda e, t=t: e.tensor_tensor(out=otok[:, t, 512:1024], in0=tmpo[:, t, 0:512], in1=sg[:, t, :], op=ALU.mult), reads=[tmpoB[t], sgB], writes=[otokB])
        for qt in range(TPG):
            otok_to_yT(qt)
        out_proj_post(L, hb, hbB, "nwR%d_1" % L)

    def ffn_block(L, hb, hbB, first, last):
        import os
        stage = int(os.environ.get("FFN_STAGE", "9"))
        alias_fence([gTB], mixBs)
        norm_transpose(hb, hbB, cs("nwT%d_2" % L))
        if stage < 2:
            return
        cw = cs("cw%d" % L).rearrange("p (f j) -> p f j", j=3)
        cb = cs("cb%d" % L)
        cidx = 0
        for j in range(11):
            wt, wB = w_get(L, "up%d" % j)
            wv = wt[:, 0:4096].rearrange("p (k c) -> p k c", k=KC)
            for i in range(2):
                fc = 2 * j + i
                pu, puB = bank()
                for kc in range(KC):
                    S.op("pe", lambda e, kc=kc, i=i, pu=pu, wv=wv: e.matmul(pu[:], lhsT=wv[:, kc, i * 128:(i + 1) * 128], rhs=yT[:, kc, :], start=(kc == 0), stop=(kc == KC - 1)),
                         reads=[wB] + yTB, writes=[puB], inc=(kc == KC - 1))
                pv, pvB = bank()
                for kc in range(KC):
                    S.op("pe", lambda e, kc=kc, i=i, pv=pv, wv=wv: e.matmul(pv[:], lhsT=wv[:, kc, 256 + i * 128:256 + (i + 1) * 128], rhs=yT[:, kc, :], start=(kc == 0), stop=(kc == KC - 1)),
                         reads=[wB] + yTB, writes=[pvB], inc=(kc == KC - 1))
                c, cB = csb[cidx % 2], csbB[cidx % 2]
                gg, gB = gsb[cidx % 2], gsbB[cidx % 2]
                cidx += 1
                if stage < 3:
                    continue
                S.op("act", lambda e, fc=fc, c=c, pu=pu: e.activation(out=c[:], in_=pu[:], func=AF.Identity, scale=cw[:, fc, 2:3], bias=cb[:, fc:fc + 1]),
                     reads=[puB, cstB], writes=[cB])
                S.op("dve", lambda e, fc=fc, c=c, pu=pu: e.scalar_tensor_tensor(out=c[:, 1:G], in0=pu[:, 0:G - 1], scalar=cw[:, fc, 1:2], op0=ALU.mult, in1=c[:, 1:G], op1=ALU.add),
                     reads=[puB, cstB, cB], writes=[cB])
                S.op("dve", lambda e, fc=fc, c=c, pu=pu: e.scalar_tensor_tensor(out=c[:, 2:G], in0=pu[:, 0:G - 2], scalar=cw[:, fc, 0:1], op0=ALU.mult, in1=c[:, 2:G], op1=ALU.add),
                     reads=[puB, cstB, cB], writes=[cB])
                if stage < 4:
                    continue
                if not first:
                    S.op("dve", lambda e, fc=fc, c=c: e.scalar_tensor_tensor(out=c[:, 0:1], in0=carry[L][:, fc, 1:2], scalar=cw[:, fc, 1:2], op0=ALU.mult, in1=c[:, 0:1], op1=ALU.add),
                         reads=[carryB[L], cstB, cB], writes=[cB])
                    S.op("dve", lambda e, fc=fc, c=c: e.scalar_tensor_tensor(out=c[:, 0:2], in0=carry[L][:, fc, 0:2], scalar=cw[:, fc, 0:1], op0=ALU.mult, in1=c[:, 0:2], op1=ALU.add),
                         reads=[carryB[L], cstB, cB], writes=[cB])
                if not last:
                    S.op("dve", lambda e, fc=fc, pu=pu: e.tensor_copy(out=carry[L][:, fc, :], in_=pu[:, G - 2:G]),
                         reads=[puB], writes=[carryB[L]])
                S.op("act", lambda e, c=c, gg=gg: e.activation(out=gg[:], in_=c[:], func=AF.Gelu), reads=[cB], writes=[gB])
                S.op("dve", lambda e, fc=fc, gg=gg, pv=pv: e.tensor_tensor(out=gT[:, fc, :], in0=pv[:], in1=gg[:], op=ALU.mult),
                     reads=[pvB, gB], writes=[gTB])

        if stage < 5:
            for _ in range(4):
                w_get(L, order[k.w_use][1])
            return

        def produce(cg):
            banks = [bank() for _ in range(TPG)]
            for hf in range(2):
                wt, wB = w_get(L, "dn%d_%d" % (cg, hf))
                wv = wt[:, 0:5632].rearrange("p (f c) -> p f c", f=11)
                for t in range(TPG):
                    p, pB = banks[t]
                    for f in range(11):
                        fc = hf * 11 + f
                        S.op("pe", lambda e, t=t, f=f, fc=fc, p=p, wv=wv: e.matmul(p[:], lhsT=gT[:, fc, t * 128:(t + 1) * 128], rhs=wv[:, f, :], start=(fc == 0), stop=(fc == FC - 1)),
                             reads=[wB, gTB], writes=[pB], inc=(f == 10))
            return banks
        post_norm_residual(hb, hbB, "nwR%d_3" % L, produce)

    k.ffn_block = ffn_block
    k.extra = {}

    gi = 0
    groups = [(s_, g_) for s_ in range(nseq) for g_ in range(NG)]

    def load_tile(gidx, t):
        s_, g_ = groups[gidx]
        r0 = s_ * seq + g_ * G + t * 128
        hb_, hbB_ = h[gidx % NH], hB[gidx % NH]
        S.dma("sp", "ldx%d_%d" % (gidx % NH, t), lambda e: e.dma_start(out=hb_[:, t, :], in_=x_d[r0:r0 + 128, :]), writes=[hbB_[t]])

    def store_tile(gidx, t):
        s_, g_ = groups[gidx]
        r0 = s_ * seq + g_ * G + t * 128
        hb_, hbB_ = h[gidx % NH], hB[gidx % NH]
        S.dma("sp", "stx%d_%d" % (gidx % NH, t), lambda e: e.dma_start(out=out_d[r0:r0 + 128, :], in_=hb_[:, t, :]), reads=[hbB_[t]])

    for t in range(TPG):
        load_tile(0, t)
    for gi, (s, g) in enumerate(groups):
        hb, hbB = h[gi % NH], hB[gi % NH]
        for L in layers:
            if "mixer" in parts:
                if L == 1:
                    odd_mixer(L, hb, hbB, g)
                else:
                    even_mixer(L, hb, hbB, g)
            if "ffn" in parts:
                ffn_block(L, hb, hbB, first=(g == 0), last=(g == NG - 1))
        for t in range(TPG):
            store_tile(gi, t)
            if gi + 1 < len(groups):
                load_tile(gi + 1, t)
    S.wait_all("sp", [b for hl in hB for b in hl])
    S.emit()
    return nc


def host_prep(inp):
    return {"wsrc": weights_host(inp).reshape(-1, 2048), "cst": consts_host(inp), "relb": relb_host(inp),
            "rope": rope_host(int(inp["x"].shape[1]))}


_NC_CACHE = {}


def kernel(**inputs):
    inp = {k_: np.asarray(v) for k_, v in inputs.items()}
    x = inp["x"]
    B, Sq, _ = x.shape
    ncores = 8
    nseq = B // ncores
    key = (nseq, Sq)
    if key not in _NC_CACHE:
        _NC_CACHE[key] = build(nseq, Sq)
    nc = _NC_CACHE[key]
    host = host_prep(inp)
    in_maps = []
    for c in range(ncores):
        m = dict(host)
        m["x"] = np.ascontiguousarray(x[c * nseq:(c + 1) * nseq].reshape(nseq * Sq, D))
        in_maps.append(m)
    res = run_bass_kernel_spmd(nc, in_maps, core_ids=list(range(ncores)))
    out = np.stack([np.asarray(r["out"]).reshape(nseq, Sq, D) for r in res.results])
    return out.reshape(B, Sq, D).astype(np.float32)
```
